# Optimizing a Trainium2 kernel written in Bass

```python
import math
import jax, jax.numpy as jnp
from jax import lax
import numpy as np

D_MODEL = 2048
BATCH = 2
SEQ = 4096
DEPTH = 1

N_Q_HEADS = 32
N_KV_HEADS = 4
HEAD_DIM = 64
WINDOW = 128
ROPE_THETA = 500000.0
ROT_DIM = HEAD_DIM // 4
SGU_GROUPS = 8
SGU_CH = 128
CHUNK = 128
N_EXPERTS = 64
N_EXPERT_GROUPS = 8
TOPK_GROUPS = 4
TOP_K = 8
D_EXPERT = 512
D_SHARED = 512
ROUTED_SCALE = 2.5
EXPERT_BLOCK = 128

ATTN_W = N_Q_HEADS * HEAD_DIM
KV_W = N_KV_HEADS * HEAD_DIM
SGU_W = SGU_GROUPS * SGU_CH
IN_W = ATTN_W + 2 * KV_W + 2 * SGU_W + 2 * D_MODEL
ALPHA = (2 * DEPTH) ** 0.25
BETA = (8 * DEPTH) ** -0.25
LN_EPS = 1e-5

kernel_name = "hybrid_swa_sgu_moe_deepnorm"


def layer_norm(x, g, b):
    xf = x.astype(jnp.float32)
    mu = jnp.mean(xf, axis=-1, keepdims=True)
    var = jnp.mean(jnp.square(xf - mu), axis=-1, keepdims=True)
    y = (xf - mu) * lax.rsqrt(var + LN_EPS)
    return (y * g.astype(jnp.float32) + b.astype(jnp.float32)).astype(x.dtype)


def partial_rope(x, pos):
    half = ROT_DIM // 2
    inv_freq = ROPE_THETA ** (-jnp.arange(0, ROT_DIM, 2, dtype=jnp.float32) / ROT_DIM)
    ang = pos[:, None] * inv_freq[None, :]
    cos = jnp.cos(ang)[None, :, None, :]
    sin = jnp.sin(ang)[None, :, None, :]
    xr = x[..., :ROT_DIM].astype(jnp.float32)
    x1, x2 = xr[..., :half], xr[..., half:]
    rot = jnp.concatenate([x1 * cos - x2 * sin, x2 * cos + x1 * sin], axis=-1)
    return jnp.concatenate([rot.astype(x.dtype), x[..., ROT_DIM:]], axis=-1)


def sliding_window_attention(q, k, v, sinks):
    B, S = q.shape[0], q.shape[1]
    nb = S // WINDOW
    G = N_Q_HEADS // N_KV_HEADS
    qb = q.reshape(B, nb, WINDOW, N_KV_HEADS, G, HEAD_DIM)

    def band(t):
        padded = jnp.pad(t, ((0, 0), (WINDOW, 0), (0, 0), (0, 0)))
        prev = padded[:, :S].reshape(B, nb, WINDOW, N_KV_HEADS, HEAD_DIM)
        cur = t.reshape(B, nb, WINDOW, N_KV_HEADS, HEAD_DIM)
        return jnp.concatenate([prev, cur], axis=2)

    kb, vb = band(k), band(v)
    s = jnp.einsum('bnqhgd,bnkhd->bnhgqk', qb, kb).astype(jnp.float32) * (HEAD_DIM ** -0.5)
    qi = jnp.arange(WINDOW)[:, None]
    kj = jnp.arange(2 * WINDOW)[None, :]
    in_band = (kj > qi) & (kj <= qi + WINDOW)
    not_pad = (jnp.arange(nb)[:, None, None] > 0) | (kj[None] >= WINDOW)
    valid = in_band[None] & not_pad
    s = jnp.where(valid[None, :, None, None], s, -jnp.inf)
    sink = sinks.astype(jnp.float32).reshape(N_KV_HEADS, G)[None, None, :, :, None, None]
    m = jnp.maximum(jnp.max(s, axis=-1, keepdims=True), sink)
    p = jnp.exp(s - m)
    denom = jnp.sum(p, axis=-1, keepdims=True) + jnp.exp(sink - m)
    p = (p / denom).astype(v.dtype)
    o = jnp.einsum('bnhgqk,bnkhd->bnqhgd', p, vb)
    return o.reshape(B, S, ATTN_W)


def spatial_gating(u, vg, ln_g, ln_b, w_s, b_s):
    B, S = u.shape[0], u.shape[1]
    nc = S // CHUNK
    vn = layer_norm(vg, ln_g, ln_b)
    vc = vn.reshape(B, nc, CHUNK, SGU_GROUPS, SGU_CH)
    causal = jnp.tril(jnp.ones((CHUNK, CHUNK), dtype=bool))
    ws = jnp.where(causal[None], w_s, jnp.zeros_like(w_s))
    sv = jnp.einsum('gts,bnsgc->bntgc', ws, vc) + jnp.transpose(b_s)[None, None, :, :, None]
    return (u * sv.reshape(B, S, SGU_GROUPS, SGU_CH)).reshape(B, S, SGU_W)


def swiglu(x, w1, w3, w2):
    return (jax.nn.silu(x @ w1) * (x @ w3)) @ w2


def route(xf, w_router, router_bias):
    N = xf.shape[0]
    scores = jax.nn.sigmoid((xf @ w_router).astype(jnp.float32))
    biased = scores + router_bias.astype(jnp.float32)
    per_group = N_EXPERTS // N_EXPERT_GROUPS
    grp = biased.reshape(N, N_EXPERT_GROUPS, per_group)
    grp_score = jnp.sum(lax.top_k(grp, 2)[0], axis=-1)
    _, top_g = lax.top_k(grp_score, TOPK_GROUPS)
    gmask = jnp.sum(jax.nn.one_hot(top_g, N_EXPERT_GROUPS, dtype=jnp.float32), axis=1) > 0
    emask = jnp.repeat(gmask, per_group, axis=1)
    masked = jnp.where(emask, biased, -jnp.inf)
    _, idx = lax.top_k(masked, TOP_K)
    w = jnp.take_along_axis(scores, idx, axis=1)
    w = w / (jnp.sum(w, axis=-1, keepdims=True) + 1e-20) * ROUTED_SCALE
    return idx, w


def routed_experts(xf, idx, wts, w1, w3, w2):
    N, D = xf.shape
    A = N * TOP_K
    n_blocks = -(-(A + N_EXPERTS * (EXPERT_BLOCK - 1)) // EXPERT_BLOCK)
    P = n_blocks * EXPERT_BLOCK
    flat_e = idx.reshape(-1)
    flat_tok = jnp.repeat(jnp.arange(N, dtype=jnp.int32), TOP_K)
    flat_w = wts.reshape(-1)
    order = jnp.argsort(flat_e)
    se = flat_e[order]
    counts = jnp.zeros((N_EXPERTS,), jnp.int32).at[flat_e].add(1)
    start = jnp.cumsum(counts) - counts
    padded = (counts + EXPERT_BLOCK - 1) // EXPERT_BLOCK * EXPERT_BLOCK
    pad_end = jnp.cumsum(padded)
    pad_start = pad_end - padded
    dest = pad_start[se] + (jnp.arange(A, dtype=jnp.int32) - start[se])
    tok_buf = jnp.full((P,), N, jnp.int32).at[dest].set(flat_tok[order])
    w_buf = jnp.zeros((P,), jnp.float32).at[dest].set(flat_w[order])
    block_start = jnp.arange(n_blocks, dtype=jnp.int32) * EXPERT_BLOCK
    block_e = jnp.minimum(jnp.searchsorted(pad_end, block_start, side='right'), N_EXPERTS - 1)
    xpad = jnp.concatenate([xf, jnp.zeros((1, D), xf.dtype)], axis=0)

    def one_block(args):
        e, tok, w = args
        xb = xpad[tok]
        return (swiglu(xb, w1[e], w3[e], w2[e]) * w[:, None]).astype(xf.dtype)

    outs = lax.map(one_block, (block_e, tok_buf.reshape(n_blocks, EXPERT_BLOCK),
                               w_buf.reshape(n_blocks, EXPERT_BLOCK)))
    y = jnp.zeros((N + 1, D), xf.dtype).at[tok_buf].add(outs.reshape(P, D))
    return y[:N]


def setup_inputs(seed: int = 0) -> dict:
    key = jax.random.key(seed)
    ks = jax.random.split(key, 24)
    L, D = DEPTH, D_MODEL
    f = jnp.float32
    nrm = lambda k, shape, scale: jax.random.normal(k, shape, f) * scale
    return {
        "x": jax.random.normal(ks[0], (BATCH, SEQ, D), f),
        "w_in": nrm(ks[1], (L, D, IN_W), D ** -0.5),
        "b_in": nrm(ks[2], (L, IN_W), 0.02),
        "sinks": nrm(ks[3], (L, N_Q_HEADS), 0.5),
        "sgu_ln_g": 1.0 + nrm(ks[4], (L, SGU_GROUPS, SGU_CH), 0.05),
        "sgu_ln_b": nrm(ks[5], (L, SGU_GROUPS, SGU_CH), 0.02),
        "w_spatial": nrm(ks[6], (L, SGU_GROUPS, CHUNK, CHUNK), CHUNK ** -0.5),
        "b_spatial": 1.0 + nrm(ks[7], (L, SGU_GROUPS, CHUNK), 0.1),
        "w_branch_attn": nrm(ks[8], (L, ATTN_W, D), ATTN_W ** -0.5),
        "w_branch_sgu": nrm(ks[9], (L, SGU_W, D), SGU_W ** -0.5),
        "w_out": nrm(ks[10], (L, D, D), BETA * D ** -0.5),
        "ln1_g": 1.0 + nrm(ks[11], (L, D), 0.05),
        "ln1_b": nrm(ks[12], (L, D), 0.02),
        "w_router": nrm(ks[13], (L, D, N_EXPERTS), D ** -0.5),
        "router_bias": nrm(ks[14], (L, N_EXPERTS), 0.01),
        "w1": nrm(ks[15], (L, N_EXPERTS, D, D_EXPERT), D ** -0.5),
        "w3": nrm(ks[16], (L, N_EXPERTS, D, D_EXPERT), D ** -0.5),
        "w2": nrm(ks[17], (L, N_EXPERTS, D_EXPERT, D), BETA * D_EXPERT ** -0.5),
        "ws1": nrm(ks[18], (L, D, D_SHARED), D ** -0.5),
        "ws3": nrm(ks[19], (L, D, D_SHARED), D ** -0.5),
        "ws2": nrm(ks[20], (L, D_SHARED, D), BETA * D_SHARED ** -0.5),
        "ln2_g": 1.0 + nrm(ks[21], (L, D), 0.05),
        "ln2_b": nrm(ks[22], (L, D), 0.02),
    }


def reference(x, w_in, b_in, sinks, sgu_ln_g, sgu_ln_b, w_spatial, b_spatial,
              w_branch_attn, w_branch_sgu, w_out, ln1_g, ln1_b, w_router, router_bias,
              w1, w3, w2, ws1, ws3, ws2, ln2_g, ln2_b):
    B, S, D = x.shape
    pos = jnp.arange(S, dtype=jnp.float32)
    splits = list(np.cumsum([ATTN_W, KV_W, KV_W, SGU_W, SGU_W, D_MODEL]))
    for l in range(DEPTH):
        h = x @ w_in[l] + b_in[l]
        q, k, v, u_pre, vg_pre, gate_a, gate_b = jnp.split(h, splits, axis=-1)
        q = partial_rope(q.reshape(B, S, N_Q_HEADS, HEAD_DIM), pos)
        k = partial_rope(k.reshape(B, S, N_KV_HEADS, HEAD_DIM), pos)
        v = v.reshape(B, S, N_KV_HEADS, HEAD_DIM)
        attn = sliding_window_attention(q, k, v, sinks[l])
        u = jax.nn.gelu(u_pre).reshape(B, S, SGU_GROUPS, SGU_CH)
        vg = jax.nn.gelu(vg_pre).reshape(B, S, SGU_GROUPS, SGU_CH)
        sgu = spatial_gating(u, vg, sgu_ln_g[l], sgu_ln_b[l], w_spatial[l], b_spatial[l])
        merged = (jax.nn.sigmoid(gate_a) * (attn @ w_branch_attn[l])
                  + jax.nn.sigmoid(gate_b) * (sgu @ w_branch_sgu[l]))
        x = layer_norm(ALPHA * x + merged @ w_out[l], ln1_g[l], ln1_b[l])
        xf = x.reshape(B * S, D)
        idx, wts = route(xf, w_router[l], router_bias[l])
        ffn = routed_experts(xf, idx, wts, w1[l], w3[l], w2[l]) + swiglu(xf, ws1[l], ws3[l], ws2[l])
        x = layer_norm(ALPHA * x + ffn.reshape(B, S, D), ln2_g[l], ln2_b[l])
    return x
```

```python
import numpy as np
from contextlib import ExitStack
import concourse.bass as bass
import concourse.mybir as mybir
from concourse.bass_utils import run_bass_kernel_spmd

F32 = mybir.dt.float32
BF16 = mybir.dt.bfloat16
I32 = mybir.dt.int32
AF = mybir.ActivationFunctionType
ALU = mybir.AluOpType

NCORES = 8
D = 2048
T = 1024
TH = 1152
NT = 8
IN_W = 8704
NE = 64
CAP = 256
ALPHA = 2.0 ** 0.25
EPS = 1e-5
NEG = -30000.0
COL_K, COL_V, COL_U, COL_VG, COL_GA, COL_GB = 2048, 2304, 2560, 3584, 4608, 6656

ENGS = ("pe", "act", "dve", "pool", "sp")
SAME_ENGINE_SYNC = True


class Buf:
    __slots__ = ("name", "w", "r")

    def __init__(self, name=""):
        self.name = name
        self.w = None
        self.r = []


class DmaSem:
    def __init__(self, sem):
        self.sem = sem
        self.count = 0


class Prog:
    def __init__(self, nc, stack):
        self.nc = nc
        self.stack = stack
        self.ops = {e: [] for e in ENGS}
        self.seen = {e: {} for e in ENGS}
        self.esem = {e: stack.enter_context(nc.semaphore("es_" + e)) for e in ENGS}
        self.ecnt = {e: 0 for e in ENGS}
        self.dsems = []

    def dma_sem(self, name):
        d = DmaSem(self.stack.enter_context(self.nc.semaphore(name)))
        self.dsems.append(d)
        return d

    def _waits(self, eng, reads, writes):
        need = {}

        def add(ev):
            if ev is None:
                return
            sem, val, src = ev
            if src == eng and (eng == "pe" or not SAME_ENGINE_SYNC):
                return
            k = id(sem)
            if self.seen[eng].get(k, 0) >= val:
                return
            if k not in need or need[k][1] < val:
                need[k] = (sem, val)

        for b in reads:
            add(b.w)
        for b in writes:
            add(b.w)
            for ev in b.r:
                add(ev)
        for k, (sem, val) in need.items():
            self.seen[eng][k] = val
        return list(need.values())

    def _post(self, ev, reads, writes):
        for b in reads:
            b.r.append(ev)
            if len(b.r) > 64:
                best = {}
                for e2 in b.r:
                    k = id(e2[0])
                    if k not in best or best[k][1] < e2[1]:
                        best[k] = e2
                b.r = list(best.values())
        for b in writes:
            b.w = ev
            b.r = []

    def op(self, eng, emit, reads=(), writes=()):
        waits = self._waits(eng, reads, writes)
        self.ecnt[eng] += 1
        ev = (self.esem[eng], self.ecnt[eng], eng)
        self.ops[eng].append((waits, emit, (self.esem[eng], 1)))
        self._post(ev, reads, writes)
        return ev

    def dma(self, eng, dsem, emit, reads=(), writes=()):
        waits = self._waits(eng, reads, writes)
        dsem.count += 16
        ev = (dsem.sem, dsem.count, "dma")
        self.ops[eng].append((waits, emit, (dsem.sem, 16)))
        self._post(ev, reads, writes)
        return ev

    def barrier(self):
        for e in ENGS:
            waits = []
            for o in ENGS:
                if (o != e or (e != "pe" and SAME_ENGINE_SYNC)) and self.ecnt[o] > self.seen[e].get(id(self.esem[o]), 0):
                    waits.append((self.esem[o], self.ecnt[o]))
                    self.seen[e][id(self.esem[o])] = self.ecnt[o]
            for d in self.dsems:
                if d.count > self.seen[e].get(id(d.sem), 0):
                    waits.append((d.sem, d.count))
                    self.seen[e][id(d.sem)] = d.count
            self.ops[e].append((waits, None, None))

    def wait_all(self, eng, bufs):
        waits = self._waits(eng, bufs, ())
        self.ops[eng].append((waits, None, None))

    def emit(self):
        nc = self.nc
        with nc.Block() as block:
            def run(engname):
                def f(e):
                    for waits, emit, inc in self.ops[engname]:
                        for sem, val in waits:
                            e.wait_ge(sem, val)
                        if emit is not None:
                            ins = emit(e)
                            ins.then_inc(inc[0], inc[1])
                return f
            block.tensor(run("pe"))
            block.scalar(run("act"))
            block.vector(run("dve"))
            block.gpsimd(run("pool"))
            block.sync(run("sp"))


def mm(P, mms, reads, writes):
    def f(e, mms=mms):
        r = None
        for m in mms:
            r = e.matmul(**m)
        return r
    return P.op("pe", f, reads, writes)


def tr(P, trs, reads, writes):
    def f(e, trs=trs):
        r = None
        for t in trs:
            r = e.transpose(**t)
        return r
    return P.op("pe", f, reads, writes)


def act(P, out, in_, func, reads, writes, bias=None, scale=None):
    kw = {}
    if bias is not None:
        kw["bias"] = bias
    if scale is not None:
        kw["scale"] = scale
    return P.op("act", lambda e: e.activation(out=out, in_=in_, func=func, **kw), reads, writes)


def tt(P, eng, out, in0, in1, op, reads, writes):
    return P.op(eng, lambda e: e.tensor_tensor(out=out, in0=in0, in1=in1, op=op), reads, writes)


def ts(P, eng, out, in0, s1, s2, op0, op1, reads, writes):
    if op1 is None:
        return P.op(eng, lambda e: e.tensor_scalar(out=out, in0=in0, scalar1=s1, scalar2=None, op0=op0), reads, writes)
    return P.op(eng, lambda e: e.tensor_scalar(out=out, in0=in0, scalar1=s1, scalar2=s2, op0=op0, op1=op1), reads, writes)


def stt(P, eng, out, in0, scalar, in1, op0, op1, reads, writes):
    return P.op(eng, lambda e: e.scalar_tensor_tensor(out=out, in0=in0, scalar=scalar, in1=in1, op0=op0, op1=op1), reads, writes)


def cp(P, eng, out, in_, reads, writes):
    if eng == "act":
        return P.op("act", lambda e: e.copy(out=out, in_=in_), reads, writes)
    return P.op(eng, lambda e: e.tensor_copy(out=out, in_=in_), reads, writes)


def dma(P, eng, dsem, out, in_, reads, writes):
    return P.dma(eng, dsem, lambda e: e.dma_start(out=out, in_=in_), reads, writes)


def gather(P, dsem, out, src, idx_ap, reads, writes):
    return P.dma("pool", dsem, lambda e: e.indirect_dma_start(
        out=out, out_offset=None, in_=src,
        in_offset=bass.IndirectOffsetOnAxis(ap=idx_ap, axis=0)), reads, writes)


class Region:
    def __init__(self, arena, base, size):
        self.arena = arena
        self.base = base
        self.size = size
        self.off = 0

    def reset(self):
        self.off = 0

    def alloc(self, shape, dt, parts=128):
        n = 1
        for d in shape[1:]:
            n *= d
        words = n if dt in (F32, I32) else (n + 1) // 2
        assert self.off + words <= self.size, (self.off, words, self.size)
        v = self.arena[0:parts, self.base + self.off:self.base + self.off + words]
        self.off += words
        if dt != F32:
            v = v.bitcast(dt)
        if len(shape) == 3:
            v = v.rearrange("p (a b) -> p a b", a=shape[1])
        elif len(shape) == 4:
            v = v.rearrange("p (a b c) -> p a b c", a=shape[1], b=shape[2])
        return v


NW_ARENA = 52800


def build(debug=()):
    nc = bass.Bass("TRN2", target_bir_lowering=False)
    dbg = {}
    lvl = 8
    for d_ in debug:
        if d_.startswith("lvl"):
            lvl = float(d_[3:])

    def din(name, shape, dt=F32):
        return nc.dram_tensor(name, list(shape), dt, kind="ExternalInput").ap()

    xin = din("xin", [TH, D])
    w_in = din("w_in", [D, IN_W])
    w_a = din("w_a", [D, D])
    w_b = din("w_b", [1024, D])
    w_o = din("w_o", [D, D])
    w_r = din("w_r", [D, NE])
    ne_decl = NE if lvl >= 7 else 1
    w1 = din("w1", [ne_decl, D, 512])
    w3 = din("w3", [ne_decl, D, 512])
    w2 = din("w2", [ne_decl, 512, D])
    ws1 = din("ws1", [D, 512])
    ws3 = din("ws3", [D, 512])
    ws2 = din("ws2", [512, D])
    cP_d = din("cP", [128, CP_W])
    cbf_d = din("cbf", [128, 2048])
    cA_d = din("cA", [128, CA_W])
    cB_d = din("cB", [128, CB_W])
    wsT_d = din("wsT", [128, 8, 128])
    brow_d = din("brow", [1, 1280])
    lnv_d = din("lnv", [4, 128, D])
    out_d = nc.dram_tensor("out", [T, D], F32, kind="ExternalOutput").ap()

    x1bf_d = nc.dram_tensor("x1bf_scr", [T, D], BF16).ap()
    x1f_d = nc.dram_tensor("x1f_scr", [T, D], F32).ap()
    ysc_d = nc.dram_tensor("y_scr", [NE * CAP, D], F32).ap()

    def dbg_out(name, shape, dt=F32):
        if name in debug:
            dbg[name] = nc.dram_tensor("dbg_" + name, list(shape), dt, kind="ExternalOutput").ap()
            return dbg[name]
        return None

    w_in_v = w_in.rearrange("(c p) n -> p c n", p=128)
    w_a_v = w_a.rearrange("(c p) n -> p c n", p=128)
    w_b_v = w_b.rearrange("(c p) n -> p c n", p=128)
    w_o_v = w_o.rearrange("(c p) n -> p c n", p=128)
    w_r_v = w_r.rearrange("(c p) n -> p c n", p=128)
    ws1_v = ws1.rearrange("(c p) n -> p c n", p=128)
    ws3_v = ws3.rearrange("(c p) n -> p c n", p=128)
    ws2_v = ws2.rearrange("(c p) n -> p c n", p=128)

    with ExitStack() as top:
        P = Prog(nc, top)
        arena_t = top.enter_context(nc.sbuf_tensor("arena", [128, NW_ARENA], F32))
        arena = arena_t[:, :]
        oP, oX = 0, 5504
        oQ = oX + 9216
        oU = oQ + 8192
        oK = oU + 4096
        oW = oK + 8192
        oT = oW + 12288
        R_P = Region(arena, oP, oX)
        R_X = Region(arena, oX, 9216)
        R_Q = Region(arena, oQ, 8192)
        R_U = Region(arena, oU, 4096)
        R_K = Region(arena, oK, 8192)
        R_W = Region(arena, oW, 12288)
        R_T = Region(arena, oT, NW_ARENA - oT)
        R_XQU = Region(arena, oX, oK - oX)
        R_WT = Region(arena, oW, NW_ARENA - oW)

        pb = [top.enter_context(nc.psum_tensor(f"pb{i}", [128, 512], F32)) for i in range(8)]
        pbB = [Buf(f"pb{i}") for i in range(8)]
        ptb = [pb[6 + i][:, 0:256].bitcast(BF16).rearrange("p (a b) -> p a b", a=4) for i in range(2)]
        ptbB = [pbB[6], pbB[7]]

        ncs = [0]

        def cs():
            ncs[0] += 1
            return P.dma_sem(f"s_c{ncs[0]}")
        s_w = [P.dma_sem(f"s_w{i}") for i in range(6)]
        s_xp = [P.dma_sem(f"s_xp{i}") for i in range(2)]
        s_x = [P.dma_sem(f"s_x{i}") for i in range(2)]
        s_xbf = P.dma_sem("s_xbf")
        s_xf = P.dma_sem("s_xf")
        s_ysc = P.dma_sem("s_ysc")
        s_g = [P.dma_sem(f"s_g{i}") for i in range(2)]
        s_yg = [P.dma_sem(f"s_yg{i}") for i in range(8)]
        s_out = P.dma_sem("s_out")
        s_dbg = P.dma_sem("s_dbg")
        dbgB = Buf("dbg")
        outB = Buf("out")

        cP = R_P.alloc([128, CP_W], F32)
        cPB = Buf("cP")
        dma(P, "sp", cs(), cP, cP_d, [], [cPB])

        def cpv(name, n):
            return cP[:, CPO[name]:CPO[name] + n]
        ident_f = cpv("ident", 128)
        psw_f = cpv("psw", 128)
        trilT = cpv("trilT", 128)
        bias_pm = cpv("bias_pm", 68)
        bias_k = cpv("bias_k", 4)
        sinks_pm = cpv("sinks_pm", 16)
        rbias = cpv("rbias", 64)
        iota_cap = cpv("iota_cap", CAP)
        ecap1 = cpv("ecap1", 64)
        tokhl = cpv("tokhl", 16)

        cbf = R_P.alloc([128, 2048], BF16)
        cbfB = Buf("cbf")
        dma(P, "pool", cs(), cbf, cbf_d, [], [cbfB])
        ident_bf = cbf[:, 0:128]
        ones_bf = cbf[:, 128:256]
        lstrict_bf = cbf[:, 256:384]
        masks_bf = cbf[:, 512:512 + 1536].rearrange("p (m n) -> p m n", m=3)
        esink = R_P.alloc([128, 16], F32)
        esinkB = Buf("esink")
        act(P, esink, sinks_pm, AF.Exp, [cPB], [esinkB])
        Gall = R_P.alloc([128, 8, 64], F32)
        sel = R_P.alloc([128, 8, 64], F32)
        selb = R_P.alloc([128, 8, 64], BF16)
        pos = R_P.alloc([128, 8, 64], F32)
        GI5 = R_P.alloc([128, 8, 64, 5], BF16)
        addr8 = R_P.alloc([128, 8, 8], F32)
        addr8i = R_P.alloc([128, 8, 8], I32)
        idx_all = R_P.alloc([128, 64, 2], I32)
        gw_all = R_P.alloc([128, 64, 2], F32)

        xT = R_X.alloc([128, 16, TH], BF16)
        xTB = [Buf(f"xT{i}") for i in range(9)]
        qT = R_Q.alloc([128, 16, T], BF16)
        qTB = [[Buf(f"qT{g}_{i}") for i in range(8)] for g in range(4)]
        uT = R_U.alloc([128, 8, T], BF16)
        uTB = [[Buf(f"uT{gb}_{i}") for i in range(8)] for gb in range(2)]
        kT = R_K.alloc([128, 4, TH], BF16)
        kTB = [Buf(f"kT{g}") for g in range(4)]
        Vs = R_K.alloc([128, 9, 256], BF16)
        VB = [Buf(f"V{i}") for i in range(9)]
        vn = R_K.alloc([128, 8, 1024], BF16)
        vnB = [Buf(f"vn{i}") for i in range(8)]

        NSL = 3
        slab = [R_W.alloc([128, 8192], BF16) for i in range(NSL)]
        slabB = [Buf(f"slab{i}") for i in range(NSL)]
        slab_rr = [0]

        def next_slab():
            i = slab_rr[0] % NSL
            slab_rr[0] += 1
            return i

        def load_slab(src_ap, kc, ncols, pieces=2):
            i = next_slab()
            v = slab[i][:, 0:kc * ncols].rearrange("p (c n) -> p c n", c=kc)
            step = kc // pieces
            for h in range(pieces):
                dma(P, "pool", s_w[i], v[:, h * step:(h + 1) * step, :], src_ap[:, h * step:(h + 1) * step, :], [], [slabB[i]])
            return v, slabB[i]

        if lvl >= 1:
            Ru = Region(arena, oU, 4096)
            cqs = Ru.alloc([128, 2 * T], F32)
            R_T.reset()
            cks = R_T.alloc([128, 2 * TH], F32)
            cAB = Buf("cA")
            dma(P, "sp", cs(), cqs, cA_d[:, 0:2 * T], [], [cAB])
            dma(P, "sp", cs(), cks, cA_d[:, 2 * T:2 * T + 2 * TH], [], [cAB])
            cosq = cqs[:, 0:T]
            sinq = cqs[:, T:2 * T]
            cosk = cks[:, 0:TH]
            sink_ = cks[:, TH:2 * TH]
            Rk = Region(arena, oK + 2304, 8192 - 2304)
            xbf = [Rk.alloc([128, D], BF16) for i in range(2)]
            xbfB = [Buf(f"xbf{i}") for i in range(2)]
            qf = [Rk.alloc([128, 512], F32) for i in range(2)]
            qfB = [Buf(f"qf{i}") for i in range(2)]
            t1 = [Rk.alloc([128, 512], F32) for i in range(2)]
            t1B = [Buf(f"t1_{i}") for i in range(2)]
            t2 = [Rk.alloc([128, 512], F32) for i in range(2)]
            t2B = [Buf(f"t2_{i}") for i in range(2)]

            for i in range(9):
                b = i % 2
                dma(P, "pool", s_xp[b], xbf[b], xin[i * 128:(i + 1) * 128, :], [], [xbfB[b]])
                for j in range(4):
                    hb = (i * 4 + j) % 2
                    tr(P, [dict(out=ptb[hb][:, jj, :], in_=xbf[b][:, (4 * j + jj) * 128:(4 * j + jj + 1) * 128], identity=ident_bf)
                           for jj in range(4)], [xbfB[b], cbfB], [ptbB[hb]])
                    cp(P, "dve" if j % 2 == 0 else "act", xT[:, 4 * j:4 * j + 4, i * 128:(i + 1) * 128], ptb[hb], [ptbB[hb]], [xTB[i]])

            it = [0]

            def rope_chunk(bank, bankB, bias_ap, cos_ap, sin_ap, out_ap, n, wr):
                k = it[0] % 2
                it[0] += 1
                act(P, qf[k][:, 0:n], bank[:, 0:n], AF.Identity, [bankB, cPB], [qfB[k]], bias=bias_ap)
                pbk = 4 + k
                mm(P, [dict(out=pb[pbk][:, 0:n], lhsT=psw_f, rhs=qf[k][:, 0:n], start=True, stop=True)], [qfB[k], cPB], [pbB[pbk]])
                tt(P, "pool", t1[k][:, 0:n], qf[k][:, 0:n], cos_ap, ALU.mult, [qfB[k], cAB], [t1B[k]])
                tt(P, "dve", t2[k][:, 0:n], pb[pbk][:, 0:n], sin_ap, ALU.mult, [pbB[pbk], cAB], [t2B[k]])
                tt(P, "dve", out_ap, t1[k][:, 0:n], t2[k][:, 0:n], ALU.add, [t1B[k], t2B[k]], wr)

            bk = 0
            for s in range(4):
                wv, wB = load_slab(w_in_v[:, :, 512 * s:512 * s + 512], 16, 512)
                for cc in range(4):
                    c = 4 * s + cc
                    for half in range(2):
                        tok = slice(128 + 512 * half, 128 + 512 * half + 512)
                        otok = slice(512 * half, 512 * half + 512)
                        bank = bk % 4
                        bk += 1
                        mm(P, [dict(out=pb[bank][:, :], lhsT=wv[:, kc, cc * 128:(cc + 1) * 128], rhs=xT[:, kc, tok],
                                    start=(kc == 0), stop=(kc == 15)) for kc in range(16)],
                           [wB] + xTB[1 + 4 * half:5 + 4 * half], [pbB[bank]])
                        rope_chunk(pb[bank], pbB[bank], bias_pm[:, c:c + 1], cosq[:, otok], sinq[:, otok],
                                   qT[:, c, otok], 512, [qTB[c // 4][4 * half + ii] for ii in range(4)])
            wv, wB = load_slab(w_in_v[:, :, COL_K:COL_K + 256], 16, 256)
            iw = next_slab()
            wkd = slab[iw][:, :].rearrange("p (c g d) -> p c g d", c=16, g=4)
            wkdB = slabB[iw]
            wv4 = wv.rearrange("p c (g d) -> p c g d", g=4)
            cp(P, "pool", wkd[:, :, :, 0:64], wv4, [wB], [wkdB])
            cp(P, "pool", wkd[:, :, :, 64:128], wv4, [wB], [wkdB])
            for g in range(4):
                for (t0_, n) in ((0, 512), (512, 512), (1024, 128)):
                    bank = bk % 4
                    bk += 1
                    mm(P, [dict(out=pb[bank][:, 0:n], lhsT=wkd[:, kc, g, :], rhs=xT[:, kc, t0_:t0_ + n],
                                start=(kc == 0), stop=(kc == 15)) for kc in range(16)],
                       [wkdB] + xTB, [pbB[bank]])
                    rope_chunk(pb[bank], pbB[bank], bias_k[:, g:g + 1], cosk[:, t0_:t0_ + n], sink_[:, t0_:t0_ + n],
                               kT[:, g, t0_:t0_ + n], n, [kTB[g]])
            P.barrier()

        if lvl >= 2:
            R_T.reset()
            cB = R_T.alloc([128, 2048], F32)
            cBB = Buf("cB")
            dma(P, "sp", cs(), cB, cB_d[:, 0:2048], [], [cBB])
            lng = cB[:, 0:1024]
            lnb = cB[:, 1024:2048]
            brow = R_T.alloc([1, 1280], BF16, parts=1)
            browB = Buf("brow")
            dma(P, "pool", cs(), brow, brow_d, [], [browB])
            vgt = [R_T.alloc([128, 1024], F32) for i in range(2)]
            vgtB = [Buf(f"vgt{i}") for i in range(2)]
            stt_ = [R_T.alloc([128, 8, 6], F32) for i in range(2)]
            mv = [R_T.alloc([128, 8, 2], F32) for i in range(2)]
            rstd = [R_T.alloc([128, 8], F32) for i in range(2)]
            lnB = [Buf(f"ln{i}") for i in range(2)]

            wv, wB = load_slab(w_in_v[:, :, COL_V:COL_V + 256], 16, 256)
            for i in range(9 if lvl >= 1.2 else 0):
                bank = i % 4
                mms = [dict(out=pb[bank][:, 0:256], lhsT=xT[:, kc, i * 128:(i + 1) * 128], rhs=wv[:, kc, :],
                            start=(kc == 0), stop=False) for kc in range(16)]
                mms.append(dict(out=pb[bank][:, 0:256], lhsT=ones_bf[0:1, :], rhs=brow[0:1, 0:256], start=False, stop=True))
                mm(P, mms, [wB, xTB[i], cbfB, browB], [pbB[bank]])
                cp(P, "act", Vs[:, i, :], pb[bank][:, 0:256], [pbB[bank]], [VB[i]])
            bk = 0
            for s in range(2 if lvl >= 1.3 else 0):
                wv, wB = load_slab(w_in_v[:, :, COL_U + 512 * s:COL_U + 512 * s + 512], 16, 512)
                for cc in range(4):
                    c = 4 * s + cc
                    for half in range(2):
                        tok = slice(128 + 512 * half, 128 + 512 * half + 512)
                        otok = slice(512 * half, 512 * half + 512)
                        bank = bk % 4
                        bk += 1
                        mm(P, [dict(out=pb[bank][:, :], lhsT=wv[:, kc, cc * 128:(cc + 1) * 128], rhs=xT[:, kc, tok],
                                    start=(kc == 0), stop=(kc == 15)) for kc in range(16)],
                           [wB] + xTB[1 + 4 * half:5 + 4 * half], [pbB[bank]])
                        act(P, uT[:, c, otok], pb[bank][:, :], AF.Gelu_apprx_tanh, [pbB[bank], cPB],
                            [uTB[c // 4][4 * half + ii] for ii in range(4)], bias=bias_pm[:, 20 + c:21 + c])
            wvs = []
            for s in range(2):
                wvs.append(load_slab(w_in_v[:, :, COL_VG + 512 * s:COL_VG + 512 * s + 512], 16, 512))
            for i in range(8 if lvl >= 1.4 else 0):
                k = i % 2
                for s in range(2):
                    bank = 4 + s
                    wv, wB = wvs[s]
                    mms = [dict(out=pb[bank][:, :], lhsT=xT[:, kc, (i + 1) * 128:(i + 2) * 128], rhs=wv[:, kc, :],
                                start=(kc == 0), stop=False) for kc in range(16)]
                    mms.append(dict(out=pb[bank][:, :], lhsT=ones_bf[0:1, :], rhs=brow[0:1, 256 + 512 * s:256 + 512 * s + 512],
                                    start=False, stop=True))
                    mm(P, mms, [wB, xTB[i + 1], cbfB, browB], [pbB[bank]])
                    act(P, vgt[k][:, 512 * s:512 * s + 512], pb[bank][:, :], AF.Gelu_apprx_tanh, [pbB[bank]], [vgtB[k]])
                v3 = vgt[k].rearrange("p (g c) -> p g c", g=8)
                if lvl < 1.5:
                    continue
                for g_ in range(8):
                    P.op("dve", lambda e, o=stt_[k], v=v3, g_=g_: e.bn_stats(out=o[:, g_, :], in_=v[:, g_, :]), [vgtB[k]], [lnB[k]])
                    P.op("dve", lambda e, o=mv[k], s_=stt_[k], g_=g_: e.bn_aggr(out=o[:, g_, :], in_=s_[:, g_, :]), [lnB[k]], [lnB[k]])
                if lvl < 1.6:
                    continue
                act(P, rstd[k], mv[k][:, :, 1], AF.Sqrt, [lnB[k]], [lnB[k]], bias=EPS)
                P.op("dve", lambda e, o=rstd[k]: e.reciprocal(out=o, in_=o), [lnB[k]], [lnB[k]])
                if lvl < 1.7:
                    continue
                tt(P, "dve", v3, v3, mv[k][:, :, 0:1].broadcast_to([128, 8, 128]), ALU.subtract, [vgtB[k], lnB[k]], [vgtB[k]])
                tt(P, "dve", v3, v3, rstd[k].unsqueeze(2).broadcast_to([128, 8, 128]), ALU.mult, [vgtB[k], lnB[k]], [vgtB[k]])
                tt(P, "pool", vgt[k], vgt[k], lng, ALU.mult, [vgtB[k], cBB], [vgtB[k]])
                tt(P, "pool", vn[:, i, :], vgt[k], lnb, ALU.add, [vgtB[k], cBB], [vnB[i]])
            if dbg_out("vn", [128, 8, 1024], BF16) is not None:
                dma(P, "sp", s_dbg, dbg["vn"], vn, vnB, [dbgB])
            if dbg_out("qT", [128, 16, T], BF16) is not None:
                dma(P, "sp", s_dbg, dbg["qT"], qT, [b for r in qTB for b in r], [dbgB])
            if dbg_out("kT", [128, 4, TH], BF16) is not None:
                dma(P, "sp", s_dbg, dbg["kT"], kT, kTB, [dbgB])
            if dbg_out("Vs", [128, 9, 256], BF16) is not None:
                dma(P, "sp", s_dbg, dbg["Vs"], Vs, VB, [dbgB])
            if dbg_out("uT", [128, 8, T], BF16) is not None:
                dma(P, "sp", s_dbg, dbg["uT"], uT, [b for r in uTB for b in r], [dbgB])
            P.barrier()

        if lvl > 2:
            Rw = Region(arena, oW, 12288)
            PT = [Rw.alloc([128, 2, 8, 128], BF16) for i in range(2)]
            PTB = [Buf(f"PT{i}") for i in range(2)]
            dn = [Rw.alloc([128, 512], F32) for i in range(2)]
            dnB = [Buf(f"dn{i}") for i in range(2)]
            wsT_f = Rw.alloc([128, 8, 128], F32)
            wsT_b = Rw.alloc([128, 8, 128], BF16)
            wsB = Buf("wsT")
            bs_bc = Rw.alloc([128, 1024], F32)
            cBB = Buf("cB2")
            dma(P, "sp", cs(), bs_bc, cB_d[:, 2048:3072], [], [cBB])
            dma(P, "sp", cs(), wsT_f, wsT_d, [], [wsB])
            tt(P, "dve", wsT_b, wsT_f, trilT.unsqueeze(1).broadcast_to([128, 8, 128]), ALU.mult, [wsB, cPB], [wsB])
            sgt = [Rw.alloc([128, 512], F32) for i in range(2)]
            sgtB = [Buf(f"sgt{i}") for i in range(2)]
            kz = [Rw.alloc([128, 4, TH], BF16) for r_ in range(2)]
            kzB = Buf("kz")
            P.op("pool", lambda e: e.memset(kz[0][64:128, :, :], 0.0), [], [kzB])
            P.op("pool", lambda e: e.memset(kz[1][0:64, :, :], 0.0), [], [kzB])
            cp(P, "pool", kz[0][0:64, :, :], kT[0:64, :, :], kTB, [kzB])
            cp(P, "pool", kz[1][64:128, :, :], kT[64:128, :, :], kTB, [kzB])

            it = 0
            for i in range(8):
                for g in range(4 if lvl >= 2.2 else 0):
                    k = it % 2
                    it += 1
                    for h in range(2):
                        kt = i + h
                        mk = 2 if h == 1 else (0 if i == 0 else 1)
                        for hb in range(2):
                            bank = 2 * h + hb
                            mms = [dict(out=pb[bank][:, :], lhsT=ident_bf, rhs=masks_bf[:, mk, :], start=True, stop=False)]
                            for sl in range(4):
                                h8 = 4 * hb + sl
                                c = 4 * g + h8 // 2
                                r = h8 % 2
                                mms.append(dict(out=pb[bank][:, sl * 128:(sl + 1) * 128],
                                                lhsT=kz[r][:, g, kt * 128:(kt + 1) * 128],
                                                rhs=qT[:, c, i * 128:(i + 1) * 128],
                                                start=False, stop=(sl == 3)))
                            mm(P, mms, [cbfB, kzB, qTB[g][i]], [pbB[bank]])
                            act(P, PT[k][:, h, 4 * hb:4 * hb + 4, :], pb[bank][:, :].rearrange("p (a b) -> p a b", a=4),
                                AF.Exp, [pbB[bank]], [PTB[k]])
                    if lvl < 2.3:
                        continue
                    mms = []
                    for cl in range(4):
                        for r in range(2):
                            hs = 2 * cl + r
                            tp = dict(tile_position=(0, 64)) if r == 1 else {}
                            for h in range(2):
                                kt = i + h
                                mms.append(dict(out=pb[4][r * 64:(r + 1) * 64, cl * 128:(cl + 1) * 128],
                                                lhsT=Vs[:, kt, g * 64:(g + 1) * 64], rhs=PT[k][:, h, hs, :],
                                                start=(h == 0), stop=(h == 1), **tp))
                            for h in range(2):
                                mms.append(dict(out=pb[5][r * 64:(r + 1) * 64, cl * 128:(cl + 1) * 128],
                                                lhsT=ones_bf[:, 0:64], rhs=PT[k][:, h, hs, :],
                                                start=(h == 0), stop=(h == 1), **tp))
                    mm(P, mms, [PTB[k], VB[i], VB[i + 1], cbfB], [pbB[4], pbB[5]])
                    if lvl < 2.4:
                        continue
                    d3 = dn[k].rearrange("p (a b) -> p a b", a=4)
                    tt(P, "dve", d3, pb[5][:, :].rearrange("p (a b) -> p a b", a=4),
                       esink[:, 4 * g:4 * g + 4].unsqueeze(2).broadcast_to([128, 4, 128]), ALU.add, [pbB[5], esinkB], [dnB[k]])
                    P.op("dve", lambda e, o=dn[k]: e.reciprocal(out=o, in_=o), [dnB[k]], [dnB[k]])
                    tt(P, "dve", qT[:, 4 * g:4 * g + 4, i * 128:(i + 1) * 128], pb[4][:, :].rearrange("p (a b) -> p a b", a=4),
                       d3, ALU.mult, [pbB[4], dnB[k]], [qTB[g][i]])
                for gb in range(2 if (lvl >= 3 or lvl == 2.1) else 0):
                    k2 = (2 * i + gb) % 2
                    mms = []
                    for gl in range(4):
                        g8 = 4 * gb + gl
                        mms.append(dict(out=pb[6][:, gl * 128:(gl + 1) * 128], lhsT=vn[:, i, g8 * 128:(g8 + 1) * 128],
                                        rhs=wsT_b[:, g8, :], start=True, stop=True))
                    mm(P, mms, [vnB[i], wsB], [pbB[6]])
                    tt(P, "dve", sgt[k2], pb[6][:, :], bs_bc[:, 512 * gb:512 * gb + 512], ALU.add, [pbB[6], cBB], [sgtB[k2]])
                    uv = uT[:, 4 * gb:4 * gb + 4, i * 128:(i + 1) * 128]
                    tt(P, "pool", uv, sgt[k2].rearrange("p (a b) -> p a b", a=4), uv, ALU.mult, [sgtB[k2], uTB[gb][i]], [uTB[gb][i]])
            if dbg_out("attnT", [128, 16, T], BF16) is not None:
                dma(P, "sp", s_dbg, dbg["attnT"], qT, [b for r in qTB for b in r], [dbgB])
            if dbg_out("sguT", [128, 8, T], BF16) is not None:
                dma(P, "sp", s_dbg, dbg["sguT"], uT, [b for r in uTB for b in r], [dbgB])
            P.barrier()
        attnT = qT
        sguT = uT
        attnB = [b for r in qTB for b in r]
        sguB = [b for r in uTB for b in r]

        mT = Region(arena, oK, 8192).alloc([128, 16, T], BF16)
        mTB = [Buf(f"mT{i}") for i in range(2)]
        if lvl >= 4:
            R_T.reset()
            sa = [R_T.alloc([128, 512], F32) for i in range(2)]
            sg = [R_T.alloc([128, 512], F32) for i in range(2)]
            ta = [R_T.alloc([128, 512], F32) for i in range(2)]
            tb_ = [R_T.alloc([128, 512], F32) for i in range(2)]
            saB = [Buf() for _ in range(2)]
            sgB = [Buf() for _ in range(2)]
            taB = [Buf() for _ in range(2)]
            tbB = [Buf() for _ in range(2)]
            it = 0
            for op_ in range(8):
                wa, waB = load_slab(w_a_v[:, :, 256 * op_:256 * op_ + 256], 16, 256)
                wga, wgaB = load_slab(w_in_v[:, :, COL_GA + 256 * op_:COL_GA + 256 * op_ + 256], 16, 256)
                i = next_slab()
                wbv = slab[i][:, 0:2048].rearrange("p (c n) -> p c n", c=8)
                wgb = slab[i][:, 2048:2048 + 4096].rearrange("p (c n) -> p c n", c=16)
                dma(P, "pool", s_w[i], wbv, w_b_v[:, :, 256 * op_:256 * op_ + 256], [], [slabB[i]])
                dma(P, "pool", s_w[i], wgb, w_in_v[:, :, COL_GB + 256 * op_:COL_GB + 256 * op_ + 256], [], [slabB[i]])
                wbB = slabB[i]
                for cc in range(2):
                    c = 2 * op_ + cc
                    csl = slice(cc * 128, (cc + 1) * 128)
                    for half in range(2):
                        k = it % 2
                        it += 1
                        otok = slice(512 * half, 512 * half + 512)
                        xtok = slice(128 + 512 * half, 128 + 512 * half + 512)
                        mm(P, [dict(out=pb[0][:, :], lhsT=wa[:, kc, csl], rhs=attnT[:, kc, otok], start=(kc == 0), stop=(kc == 15))
                               for kc in range(16)], [waB] + attnB, [pbB[0]])
                        mm(P, [dict(out=pb[1][:, :], lhsT=wga[:, kc, csl], rhs=xT[:, kc, xtok], start=(kc == 0), stop=(kc == 15))
                               for kc in range(16)], [wgaB] + xTB, [pbB[1]])
                        mm(P, [dict(out=pb[2][:, :], lhsT=wbv[:, kc, csl], rhs=sguT[:, kc, otok], start=(kc == 0), stop=(kc == 7))
                               for kc in range(8)], [wbB] + sguB, [pbB[2]])
                        mm(P, [dict(out=pb[3][:, :], lhsT=wgb[:, kc, csl], rhs=xT[:, kc, xtok], start=(kc == 0), stop=(kc == 15))
                               for kc in range(16)], [wbB] + xTB, [pbB[3]])
                        act(P, sa[k], pb[1][:, :], AF.Sigmoid, [pbB[1], cPB], [saB[k]], bias=bias_pm[:, 36 + c:37 + c])
                        act(P, sg[k], pb[3][:, :], AF.Sigmoid, [pbB[3], cPB], [sgB[k]], bias=bias_pm[:, 52 + c:53 + c])
                        tt(P, "dve", ta[k], pb[0][:, :], sa[k], ALU.mult, [pbB[0], saB[k]], [taB[k]])
                        tt(P, "dve", tb_[k], pb[2][:, :], sg[k], ALU.mult, [pbB[2], sgB[k]], [tbB[k]])
                        tt(P, "pool", mT[:, c, otok], ta[k], tb_[k], ALU.add, [taB[k], tbB[k]], [mTB[half]])
            if dbg_out("mT", [128, 16, T], BF16) is not None:
                dma(P, "sp", s_dbg, dbg["mT"], mT, mTB, [dbgB])
            P.barrier()

        R_XQU.reset()
        r = R_XQU.alloc([128, 8, D], F32)
        rB = [Buf(f"r{i}") for i in range(8)]
        if lvl >= 5:
            Rw = Region(arena, oW, 12288)
            NS2 = 2
            slab2 = [Rw.alloc([128, 16, 512], BF16) for i in range(NS2)]
            slab2B = [Buf() for _ in range(NS2)]
            R_T.reset()
            xt = [R_T.alloc([128, 512], F32) for i in range(2)]
            xtB = [Buf() for _ in range(2)]
            xin_own = xin[128:, :]
            it = 0
            for s in range(4):
                b = s % NS2
                for h in range(2):
                    dma(P, "pool", s_w[b], slab2[b][:, 8 * h:8 * h + 8, :], w_o_v[:, 8 * h:8 * h + 8, 512 * s:512 * s + 512], [], [slab2B[b]])
                for i in range(8):
                    k = it % 2
                    it += 1
                    bank = it % 4
                    dma(P, "sp", s_x[k], xt[k], xin_own[i * 128:(i + 1) * 128, 512 * s:512 * s + 512], [], [xtB[k]])
                    mm(P, [dict(out=pb[bank][:, :], lhsT=mT[:, kc, i * 128:(i + 1) * 128], rhs=slab2[b][:, kc, :],
                                start=(kc == 0), stop=(kc == 15)) for kc in range(16)], [slab2B[b]] + mTB, [pbB[bank]])
                    stt(P, "dve", r[:, i, 512 * s:512 * s + 512], xt[k], ALPHA, pb[bank][:, :], ALU.mult, ALU.add,
                        [xtB[k], pbB[bank]], [rB[i]])
            P.barrier()

        x1T = Region(arena, oK, 8192).alloc([128, 16, T], BF16)
        x1TB = [Buf(f"x1T{i}") for i in range(8)]
        rtB = [Buf(f"rt{i}") for i in range(8)]
        posB = Buf("pos")
        igB = Buf("ig")
        x1bfB = [Buf(f"x1bf_d{i}") for i in range(8)]
        x1fB = [Buf(f"x1f_d{i}") for i in range(8)]
        if lvl > 5:
            Rw = Region(arena, oW, 12288)
            lnv = Rw.alloc([128, 2, D], F32)
            lnvB = Buf("lnv")
            dma(P, "sp", cs(), lnv[:, 0, :], lnv_d[0], [], [lnvB])
            dma(P, "sp", cs(), lnv[:, 1, :], lnv_d[1], [], [lnvB])
            wr_f = Rw.alloc([128, 16, 64], F32)
            wr_hi = Rw.alloc([128, 16, 64], BF16)
            wr_lo = Rw.alloc([128, 16, 64], BF16)
            wrB = Buf("wr")
            dma(P, "sp", cs(), wr_f, w_r_v, [], [wrB])
            cp(P, "dve", wr_hi, wr_f, [wrB], [wrB])
            tt(P, "dve", wr_lo, wr_f, wr_hi, ALU.subtract, [wrB], [wrB])
            x1b = [Rw.alloc([128, D], BF16) for i in range(1)]
            x1bB = [Buf() for _ in range(1)]
            x1lo = [Rw.alloc([128, D], BF16) for i in range(1)]
            x1loB = [Buf() for _ in range(1)]
            x1Tlo = [Rw.alloc([128, 16, 128], BF16) for i in range(2)]
            x1TloB = [Buf() for _ in range(2)]
            R_T.reset()
            st4 = [R_T.alloc([128, 4, 6], F32) for i in range(2)]
            mv2 = [R_T.alloc([128, 2], F32) for i in range(2)]
            rs2 = [R_T.alloc([128, 1], F32) for i in range(2)]
            ln2B = [Buf() for _ in range(2)]
            sc = [R_T.alloc([128, 64], F32) for i in range(2)]
            bi = [R_T.alloc([128, 64], F32) for i in range(2)]
            mx = [R_T.alloc([128, 8, 8], F32) for i in range(2)]
            gs = [R_T.alloc([128, 8], F32) for i in range(2)]
            gm = [R_T.alloc([128, 8], F32) for i in range(2)]
            m8 = [R_T.alloc([128, 8], F32) for i in range(2)]
            mk_ = [R_T.alloc([128, 64], F32) for i in range(2)]
            wsum = [R_T.alloc([128, 1], F32) for i in range(2)]
            rtsB = [Buf() for _ in range(2)]

            def layer_norm(eng2, src, srcB, g_ap, b_ap, gbB, k):
                for a_ in range(4):
                    P.op("dve", lambda e, a_=a_, o=st4[k], s_=src: e.bn_stats(out=o[:, a_, :], in_=s_[:, a_ * 512:(a_ + 1) * 512]), [srcB], [ln2B[k]])
                P.op("dve", lambda e, o=mv2[k], s_=st4[k]: e.bn_aggr(out=o, in_=s_.rearrange("p a b -> p (a b)")), [ln2B[k]], [ln2B[k]])
                act(P, rs2[k], mv2[k][:, 1:2], AF.Sqrt, [ln2B[k]], [ln2B[k]], bias=EPS)
                P.op("dve", lambda e, o=rs2[k]: e.reciprocal(out=o, in_=o), [ln2B[k]], [ln2B[k]])
                ts(P, "dve", src, src, mv2[k][:, 0:1], rs2[k][:, 0:1], ALU.subtract, ALU.mult, [srcB, ln2B[k]], [srcB])
                tt(P, eng2, src, src, g_ap, ALU.mult, [srcB, gbB], [srcB])
                tt(P, eng2, src, src, b_ap, ALU.add, [srcB, gbB], [srcB])

            for i in range(8):
                k = i % 2
                layer_norm("pool", r[:, i, :], rB[i], lnv[:, 0, :], lnv[:, 1, :], lnvB, k)
                cp(P, "act", x1b[0], r[:, i, :], [rB[i]], [x1bB[0]])
                tt(P, "dve", x1lo[0], r[:, i, :], x1b[0], ALU.subtract, [rB[i], x1bB[0]], [x1loB[0]])
                dma(P, "sp", s_xbf, x1bf_d[i * 128:(i + 1) * 128, :], x1b[0], [x1bB[0]], [x1bfB[i]])
                dma(P, "sp", s_xf, x1f_d[i * 128:(i + 1) * 128, :], r[:, i, :], [rB[i]], [x1fB[i]])
                for j in range(4):
                    hb = j % 2
                    tr(P, [dict(out=ptb[hb][:, jj, :], in_=x1b[0][:, (4 * j + jj) * 128:(4 * j + jj + 1) * 128], identity=ident_bf)
                           for jj in range(4)], [x1bB[0], cbfB], [ptbB[hb]])
                    cp(P, "dve" if j % 2 == 0 else "act", x1T[:, 4 * j:4 * j + 4, i * 128:(i + 1) * 128], ptb[hb], [ptbB[hb]], [x1TB[i]])
                for j in range(4):
                    hb = j % 2
                    tr(P, [dict(out=ptb[hb][:, jj, :], in_=x1lo[0][:, (4 * j + jj) * 128:(4 * j + jj + 1) * 128], identity=ident_bf)
                           for jj in range(4)], [x1loB[0], cbfB], [ptbB[hb]])
                    cp(P, "act" if j % 2 == 0 else "dve", x1Tlo[k][:, 4 * j:4 * j + 4, :], ptb[hb], [ptbB[hb]], [x1TloB[k]])
                mms = []
                for kc in range(16):
                    xh = x1T[:, kc, i * 128:(i + 1) * 128]
                    mms.append(dict(out=pb[5][:, 0:64], lhsT=xh, rhs=wr_hi[:, kc, :], start=(kc == 0), stop=False))
                    mms.append(dict(out=pb[5][:, 0:64], lhsT=xh, rhs=wr_lo[:, kc, :], start=False, stop=False))
                    mms.append(dict(out=pb[5][:, 0:64], lhsT=x1Tlo[k][:, kc, :], rhs=wr_hi[:, kc, :], start=False, stop=(kc == 15)))
                mm(P, mms, [x1TB[i], x1TloB[k], wrB], [pbB[5]])
                if lvl < 5.2:
                    continue
                RB = rtsB[k]
                act(P, sc[k], pb[5][:, 0:64], AF.Sigmoid, [pbB[5]], [RB])
                tt(P, "dve", bi[k], sc[k], rbias, ALU.add, [RB, cPB], [RB])
                for g in range(8):
                    P.op("dve", lambda e, o=mx[k][:, g, :], v=bi[k][:, g * 8:(g + 1) * 8]: e.max(out=o, in_=v), [RB], [RB])
                tt(P, "dve", gs[k], mx[k][:, :, 0], mx[k][:, :, 1], ALU.add, [RB], [RB])
                P.op("dve", lambda e, o=m8[k], v=gs[k]: e.max(out=o, in_=v), [RB], [RB])
                ts(P, "dve", gm[k], gs[k], m8[k][:, 3:4], None, ALU.is_ge, None, [RB], [RB])
                stt(P, "dve", mk_[k].rearrange("p (g e) -> p g e", g=8), bi[k].rearrange("p (g e) -> p g e", g=8), 2.0,
                    gm[k].unsqueeze(2).broadcast_to([128, 8, 8]), ALU.add, ALU.mult, [RB], [RB])
                P.op("dve", lambda e, o=m8[k], v=mk_[k]: e.max(out=o, in_=v), [RB], [RB])
                ts(P, "dve", sel[:, i, :], mk_[k], m8[k][:, 7:8], None, ALU.is_ge, None, [RB], [rtB[i]])
                tt(P, "dve", sc[k], sc[k], sel[:, i, :], ALU.mult, [RB, rtB[i]], [RB])
                P.op("dve", lambda e, o=wsum[k], v=sc[k]: e.reduce_sum(out=o, in_=v, axis=mybir.AxisListType.X), [RB], [RB])
                ts(P, "dve", wsum[k], wsum[k], 1e-20, 1.0 / 2.5, ALU.add, ALU.mult, [RB], [RB])
                P.op("dve", lambda e, o=wsum[k]: e.reciprocal(out=o, in_=o), [RB], [RB])
                ts(P, "dve", Gall[:, i, :], sc[k], wsum[k][:, 0:1], None, ALU.mult, None, [RB], [rtB[i]])
                cp(P, "dve", selb[:, i, :], sel[:, i, :], [rtB[i]], [rtB[i]])
            for i in range(8 if lvl >= 5.3 else 0):
                mms = [dict(out=pb[4][:, i * 64:(i + 1) * 64], lhsT=lstrict_bf, rhs=selb[:, i, :], start=True, stop=(i == 0))]
                for j in range(i):
                    mms.append(dict(out=pb[4][:, i * 64:(i + 1) * 64], lhsT=ones_bf, rhs=selb[:, j, :], start=False, stop=(j == i - 1)))
                mm(P, mms, rtB[:i + 1] + [cbfB], [pbB[4]])
            cp(P, "dve", pos.rearrange("p a b -> p (a b)"), pb[4][:, :], [pbB[4]], [posB])
            for i in range(8 if lvl >= 5.3 else 0):
                k = i % 2
                tt(P, "dve", mk_[k], pos[:, i, :], ecap1, ALU.add, [posB, cPB], [rtsB[k]])
                tt(P, "dve", mk_[k], mk_[k], sel[:, i, :], ALU.mult, [rtsB[k], rtB[i]], [rtsB[k]])
                P.op("dve", lambda e, o=addr8[:, i, :], v=mk_[k]: e.max(out=o, in_=v), [rtsB[k]], [posB])
            ts(P, "dve", addr8, addr8, -1.0, None, ALU.add, None, [posB], [posB])
            cp(P, "dve", addr8i, addr8, [posB], [posB])
            P.barrier()
            Rw = Region(arena, oW, 12288)
            gtmp = Rw.alloc([128, 8, 64], F32)
            gB = Buf("gtmp")
            thl = tokhl.rearrange("p (a b) -> p a b", a=8)
            cp(P, "pool", GI5[:, :, :, 0], thl[:, :, 0:1].broadcast_to([128, 8, 64]), [cPB], [igB])
            cp(P, "pool", GI5[:, :, :, 1], thl[:, :, 1:2].broadcast_to([128, 8, 64]), [cPB], [igB])
            cp(P, "dve", GI5[:, :, :, 2], Gall, rtB, [igB])
            tt(P, "dve", gtmp, Gall, GI5[:, :, :, 2], ALU.subtract, rtB + [igB], [gB])
            cp(P, "dve", GI5[:, :, :, 3], gtmp, [gB], [igB])
            tt(P, "dve", gtmp, gtmp, GI5[:, :, :, 3], ALU.subtract, [gB, igB], [gB])
            cp(P, "dve", GI5[:, :, :, 4], gtmp, [gB], [igB])
            S1 = [Rw.alloc([128, 8, CAP], BF16) for i in range(4)]
            S1B = [Buf() for _ in range(4)]
            it = 0
            for e_ in range(NE if lvl >= 5.4 else 0):
                k = e_ % 4
                bank = 2 + e_ // 32
                for i in range(8):
                    it += 1
                    ts(P, "dve" if it % 2 == 0 else "pool", S1[k][:, i, :], iota_cap, pos[:, i, e_:e_ + 1], sel[:, i, e_:e_ + 1],
                       ALU.is_equal, ALU.mult, [posB, rtB[i], cPB], [S1B[k]])
                c0 = ((e_ % 32) * 2) * 5
                mm(P, [dict(out=pb[bank][:, c0 + st * 5:c0 + st * 5 + 5], lhsT=S1[k][:, i, st * 128:(st + 1) * 128],
                            rhs=GI5[:, i, e_, :], start=(i == 0), stop=(i == 7)) for st in range(2) for i in range(8)],
                   [S1B[k], igB], [pbB[bank]])
            igs = Rw.alloc([128, 2, 320], F32)
            igsB = Buf("igs")
            idxf = Rw.alloc([128, 64, 2], F32)
            cp(P, "dve", igs[:, 0, :], pb[2][:, 0:320], [pbB[2]], [igsB])
            cp(P, "act", igs[:, 1, :], pb[3][:, 0:320], [pbB[3]], [igsB])
            for hb_ in range(2):
                v5 = igs[:, hb_, :].rearrange("p (e s c) -> p e s c", e=32, s=2)
                es = slice(32 * hb_, 32 * hb_ + 32)
                stt(P, "dve", idxf[:, es, :], v5[:, :, :, 0], 32.0, v5[:, :, :, 1], ALU.mult, ALU.add, [igsB], [igB])
                tt(P, "dve", gw_all[:, es, :], v5[:, :, :, 2], v5[:, :, :, 3], ALU.add, [igsB], [igB])
                tt(P, "dve", gw_all[:, es, :], gw_all[:, es, :], v5[:, :, :, 4], ALU.add, [igsB, igB], [igB])
            cp(P, "dve", idx_all, idxf, [igB], [igB])
            if dbg_out("x1", [128, 8, D]) is not None:
                dma(P, "sp", s_dbg, dbg["x1"], r, rB, [dbgB])
            if dbg_out("Gall", [128, 8, 64]) is not None:
                dma(P, "sp", s_dbg, dbg["Gall"], Gall, rtB, [dbgB])
            if dbg_out("pos", [128, 8, 64]) is not None:
                dma(P, "sp", s_dbg, dbg["pos"], pos, [posB], [dbgB])
            if dbg_out("idx_all", [128, 64, 2], I32) is not None:
                dma(P, "sp", s_dbg, dbg["idx_all"], idx_all, [igB], [dbgB])
            if dbg_out("gw_all", [128, 64, 2]) is not None:
                dma(P, "sp", s_dbg, dbg["gw_all"], gw_all, [igB], [dbgB])
            if dbg_out("addr8i", [128, 8, 8], I32) is not None:
                dma(P, "sp", s_dbg, dbg["addr8i"], addr8i, [posB], [dbgB])
            P.barrier()

        yscB = Buf("ysc")
        if lvl >= 7:
            R1 = Region(arena, oX, oK - oX)
            R2 = Region(arena, oW, NW_ARENA - oW)
            NW = 2
            w1s = [R1.alloc([128, 16, 512], BF16) for i in range(NW)]
            w3s = [R1.alloc([128, 16, 512], BF16) for i in range(NW)]
            w2s = [R1.alloc([128, 4, D], BF16), R2.alloc([128, 4, D], BF16)]
            hT = [R1.alloc([128, 4, CAP], BF16) for i in range(2)]
            w1B = [Buf() for _ in range(NW)]
            w3B = [Buf() for _ in range(NW)]
            w2B = [Buf() for _ in range(NW)]
            Xg = [R2.alloc([128, 2, D], BF16) for i in range(2)]
            XgB = [Buf() for _ in range(2)]
            XeT = [R2.alloc([128, 16, CAP], BF16) for i in range(2)]
            XeTB = [Buf() for _ in range(2)]
            hTB = [Buf() for _ in range(2)]
            sl_ = [R2.alloc([128, 2, CAP], F32) for i in range(2)]
            slB = [Buf() for _ in range(2)]
            NY = 2
            ys = [R2.alloc([128, D], F32) for i in range(NY)]
            ysB = [Buf() for _ in range(NY)]

            def load_expert(e_):
                b = e_ % NW
                for h in range(2):
                    dma(P, "pool", s_w[b], w1s[b][:, 8 * h:8 * h + 8, :],
                        w1[e_].rearrange("(c p) n -> p c n", p=128)[:, 8 * h:8 * h + 8, :], [], [w1B[b]])
                for h in range(2):
                    dma(P, "pool", s_w[2 + b], w3s[b][:, 8 * h:8 * h + 8, :],
                        w3[e_].rearrange("(c p) n -> p c n", p=128)[:, 8 * h:8 * h + 8, :], [], [w3B[b]])
                for h in range(2):
                    dma(P, "pool", s_w[4 + b], w2s[b][:, 2 * h:2 * h + 2, :],
                        w2[e_].rearrange("(c p) n -> p c n", p=128)[:, 2 * h:2 * h + 2, :], [], [w2B[b]])

            def load_tokens(e_):
                b = e_ % 2
                for st in range(2):
                    gather(P, s_g[b], Xg[b][:, st, :], x1bf_d[:, :], idx_all[:, e_, st:st + 1], [igB] + x1bfB, [XgB[b]])

            load_tokens(0)
            load_expert(0)
            yi = 0
            for e_ in range(NE):
                b = e_ % 2
                wb_ = e_ % NW
                if e_ + 1 < NE:
                    load_tokens(e_ + 1)
                    load_expert(e_ + 1)
                for st in range(2):
                    for j in range(4):
                        hb = j % 2
                        tr(P, [dict(out=ptb[hb][:, jj, :], in_=Xg[b][:, st, (4 * j + jj) * 128:(4 * j + jj + 1) * 128], identity=ident_bf)
                               for jj in range(4)], [XgB[b], cbfB], [ptbB[hb]])
                        cp(P, "dve" if j % 2 == 0 else "act", XeT[b][:, 4 * j:4 * j + 4, st * 128:(st + 1) * 128], ptb[hb],
                           [ptbB[hb]], [XeTB[b]])
                for hp in range(2):
                    for hl in range(2):
                        hc = 2 * hp + hl
                        mm(P, [dict(out=pb[0 + hp][:, hl * CAP:(hl + 1) * CAP], lhsT=w1s[wb_][:, kc, hc * 128:(hc + 1) * 128],
                                    rhs=XeT[b][:, kc, :], start=(kc == 0), stop=(kc == 15)) for kc in range(16)],
                           [w1B[wb_], XeTB[b]], [pbB[0 + hp]])
                        mm(P, [dict(out=pb[2 + hp][:, hl * CAP:(hl + 1) * CAP], lhsT=w3s[wb_][:, kc, hc * 128:(hc + 1) * 128],
                                    rhs=XeT[b][:, kc, :], start=(kc == 0), stop=(kc == 15)) for kc in range(16)],
                           [w3B[wb_], XeTB[b]], [pbB[2 + hp]])
                    act(P, sl_[hp].rearrange("p a b -> p (a b)"), pb[0 + hp][:, :], AF.Silu, [pbB[0 + hp]], [slB[hp]])
                    tt(P, "dve", hT[b][:, 2 * hp:2 * hp + 2, :].rearrange("p a b -> p (a b)"), sl_[hp].rearrange("p a b -> p (a b)"),
                       pb[2 + hp][:, :], ALU.mult, [slB[hp], pbB[2 + hp]], [hTB[b]])
                for st in range(2):
                    yk = yi % NY
                    yi += 1
                    for cg in range(4):
                        bank = 4 + (cg % 2)
                        mm(P, [dict(out=pb[bank][:, :], lhsT=hT[b][:, hc, st * 128:(st + 1) * 128],
                                    rhs=w2s[wb_][:, hc, cg * 512:(cg + 1) * 512], start=(hc == 0), stop=(hc == 3)) for hc in range(4)],
                           [hTB[b], w2B[wb_]], [pbB[bank]])
                        if cg % 2 == 0:
                            act(P, ys[yk][:, cg * 512:(cg + 1) * 512], pb[bank][:, :], AF.Copy, [pbB[bank], igB], [ysB[yk]],
                                scale=gw_all[:, e_, st:st + 1])
                        else:
                            ts(P, "dve", ys[yk][:, cg * 512:(cg + 1) * 512], pb[bank][:, :], gw_all[:, e_, st:st + 1], None,
                               ALU.mult, None, [pbB[bank], igB], [ysB[yk]])
                    dma(P, "sp", s_ysc, ysc_d[e_ * CAP + st * 128:e_ * CAP + (st + 1) * 128, :], ys[yk], [ysB[yk]], [yscB])
            P.barrier()

        if lvl >= 7:
            R1 = Region(arena, oX, oK - oX)
            R2 = Region(arena, oW, NW_ARENA - oW)
            ws1s = R1.alloc([128, 16, 512], BF16)
            ws3s = R1.alloc([128, 16, 512], BF16)
            wsB_ = [Buf() for _ in range(3)]
            for h in range(2):
                dma(P, "pool", s_w[0], ws1s[:, 8 * h:8 * h + 8, :], ws1_v[:, 8 * h:8 * h + 8, :], [], [wsB_[0]])
                dma(P, "pool", s_w[1], ws3s[:, 8 * h:8 * h + 8, :], ws3_v[:, 8 * h:8 * h + 8, :], [], [wsB_[1]])
            hsT = R2.alloc([128, 4, T], BF16)
            hsB = Buf("hsT")
            sl2 = [R2.alloc([128, 512], F32) for i in range(2)]
            sl2B = [Buf() for _ in range(2)]
            it = 0
            for half in range(2):
                tok = slice(512 * half, 512 * half + 512)
                for hc in range(4):
                    k = it % 2
                    it += 1
                    mm(P, [dict(out=pb[0 + k][:, :], lhsT=ws1s[:, kc, hc * 128:(hc + 1) * 128], rhs=x1T[:, kc, tok],
                                start=(kc == 0), stop=(kc == 15)) for kc in range(16)], [wsB_[0]] + x1TB, [pbB[0 + k]])
                    mm(P, [dict(out=pb[2 + k][:, :], lhsT=ws3s[:, kc, hc * 128:(hc + 1) * 128], rhs=x1T[:, kc, tok],
                                start=(kc == 0), stop=(kc == 15)) for kc in range(16)], [wsB_[1]] + x1TB, [pbB[2 + k]])
                    act(P, sl2[k], pb[0 + k][:, :], AF.Silu, [pbB[0 + k]], [sl2B[k]])
                    tt(P, "dve", hsT[:, hc, tok], sl2[k], pb[2 + k][:, :], ALU.mult, [sl2B[k], pbB[2 + k]], [hsB])
            P.barrier()

            R1.reset()
            Yg = R1.alloc([128, 8, D], F32)
            YgB = [Buf() for _ in range(8)]
            ws2s = R1.alloc([128, 4, D], BF16)
            for h in range(2):
                dma(P, "pool", s_w[2], ws2s[:, 2 * h:2 * h + 2, :], ws2_v[:, 2 * h:2 * h + 2, :], [], [wsB_[2]])
            lnv = R2.alloc([128, 2, D], F32)
            lnvB = Buf("lnv2")
            dma(P, "sp", cs(), lnv[:, 0, :], lnv_d[2], [], [lnvB])
            dma(P, "sp", cs(), lnv[:, 1, :], lnv_d[3], [], [lnvB])
            x1r = [R2.alloc([128, D], F32) for i in range(1)]
            x1rB = [Buf() for _ in range(1)]
            acc = [R2.alloc([128, D], F32) for i in range(2)]
            accB = [Buf() for _ in range(2)]
            accP = [R2.alloc([128, D], F32) for i in range(1)]
            accPB = [Buf() for _ in range(1)]
            st4 = [R2.alloc([128, 4, 6], F32) for i in range(2)]
            mv2 = [R2.alloc([128, 2], F32) for i in range(2)]
            rs2 = [R2.alloc([128, 1], F32) for i in range(2)]
            ln2B = [Buf() for _ in range(2)]

            for i in range(8):
                k = i % 2
                for kk in range(8):
                    gather(P, s_yg[kk], Yg[:, kk, :], ysc_d[:, :], addr8i[:, i, kk:kk + 1], [posB, yscB], [YgB[kk]])
                dma(P, "sp", s_x[0], x1r[0], x1f_d[i * 128:(i + 1) * 128, :], x1fB, [x1rB[0]])
                for cg in range(4):
                    bank = 4 + (cg % 2)
                    mm(P, [dict(out=pb[bank][:, :], lhsT=hsT[:, hc, i * 128:(i + 1) * 128], rhs=ws2s[:, hc, cg * 512:(cg + 1) * 512],
                                start=(hc == 0), stop=(hc == 3)) for hc in range(4)], [hsB, wsB_[2]], [pbB[bank]])
                    stt(P, "dve", acc[k][:, cg * 512:(cg + 1) * 512], x1r[0][:, cg * 512:(cg + 1) * 512], ALPHA, pb[bank][:, :],
                        ALU.mult, ALU.add, [x1rB[0], pbB[bank]], [accB[k]])
                tt(P, "pool", accP[0], Yg[:, 0, :], Yg[:, 1, :], ALU.add, [YgB[0], YgB[1]], [accPB[0]])
                tt(P, "pool", accP[0], accP[0], Yg[:, 2, :], ALU.add, [YgB[2], accPB[0]], [accPB[0]])
                tt(P, "pool", accP[0], accP[0], Yg[:, 3, :], ALU.add, [YgB[3], accPB[0]], [accPB[0]])
                for kk in range(4, 8):
                    tt(P, "dve", acc[k], acc[k], Yg[:, kk, :], ALU.add, [YgB[kk], accB[k]], [accB[k]])
                tt(P, "dve", acc[k], acc[k], accP[0], ALU.add, [accPB[0], accB[k]], [accB[k]])
                src = acc[k]
                for a_ in range(4):
                    P.op("dve", lambda e, o=st4[k], s_=src, a_=a_: e.bn_stats(out=o[:, a_, :], in_=s_[:, a_ * 512:(a_ + 1) * 512]), [accB[k]], [ln2B[k]])
                P.op("dve", lambda e, o=mv2[k], s_=st4[k]: e.bn_aggr(out=o, in_=s_.rearrange("p a b -> p (a b)")), [ln2B[k]], [ln2B[k]])
                act(P, rs2[k], mv2[k][:, 1:2], AF.Sqrt, [ln2B[k]], [ln2B[k]], bias=EPS)
                P.op("dve", lambda e, o=rs2[k]: e.reciprocal(out=o, in_=o), [ln2B[k]], [ln2B[k]])
                ts(P, "dve", src, src, mv2[k][:, 0:1], rs2[k][:, 0:1], ALU.subtract, ALU.mult, [accB[k], ln2B[k]], [accB[k]])
                tt(P, "pool", src, src, lnv[:, 0, :], ALU.mult, [accB[k], lnvB], [accB[k]])
                tt(P, "pool", src, src, lnv[:, 1, :], ALU.add, [accB[k], lnvB], [accB[k]])
                dma(P, "sp", s_out, out_d[i * 128:(i + 1) * 128, :], src, [accB[k]], [outB])
        P.wait_all("sp", [outB, dbgB])
        P.emit()
    return nc, list(dbg.keys())


def _pack(parts):
    off = {}
    cols = []
    o = 0
    for name, arr in parts:
        arr = np.ascontiguousarray(arr, dtype=np.float32).reshape(128, -1)
        off[name] = o
        o += arr.shape[1]
        cols.append(arr)
    return off, np.concatenate(cols, axis=1)


def _const_P(b_in=None, sinks=None, rbias=None, core=0):
    p = np.arange(128)
    ident = np.eye(128, dtype=np.float32)
    psw = np.zeros((128, 128), np.float32)
    for m in range(128):
        d = m % 64
        if d < 8:
            psw[m + 8, m] = 1.0
        elif d < 16:
            psw[m - 8, m] = 1.0
    trilT = (p[None, :] >= p[:, None]).astype(np.float32)
    if b_in is None:
        b_in = np.zeros(IN_W, np.float32)
        sinks = np.zeros(32, np.float32)
        rbias = np.zeros(64, np.float32)
    bias_pm = b_in.reshape(68, 128).T
    bk = b_in[COL_K:COL_K + 256].reshape(4, 64)
    bias_k = np.concatenate([bk.T, bk.T], axis=0)
    sk = sinks.reshape(16, 2)
    sinks_pm = np.repeat(sk.T, 64, axis=0)
    rb = np.broadcast_to(rbias[None, :], (128, 64))
    iota_cap = np.broadcast_to(np.arange(CAP, dtype=np.float32)[None, :], (128, CAP))
    ecap1 = np.broadcast_to((np.arange(64, dtype=np.float32) * CAP + 1.0)[None, :], (128, 64))
    tok = (np.arange(8)[None, :] * 128 + p[:, None])
    tokhl = np.stack([tok // 32, tok % 32], axis=2).astype(np.float32).reshape(128, 16)
    return _pack([("ident", ident), ("psw", psw), ("trilT", trilT),
                  ("bias_pm", bias_pm), ("bias_k", bias_k), ("sinks_pm", sinks_pm), ("rbias", rb),
                  ("iota_cap", iota_cap), ("ecap1", ecap1), ("tokhl", tokhl)])


def _const_bf(core):
    p = np.arange(128)
    ident = np.eye(128, dtype=np.float32)
    ones = np.ones((128, 128), np.float32)
    lstrict = (p[:, None] < p[None, :]).astype(np.float32)
    kk = p[:, None]
    qq = p[None, :]
    m_prev = np.where(kk > qq, 0.0, NEG).astype(np.float32)
    m_cur = np.where(kk <= qq, 0.0, NEG).astype(np.float32)
    m_prev0 = m_prev if core % 4 != 0 else np.full((128, 128), NEG, np.float32)
    masks = np.concatenate([np.tile(m, (1, 4)) for m in (m_prev0, m_prev, m_cur)], axis=1)
    return np.ascontiguousarray(np.concatenate([ident, ones, lstrict, np.zeros((128, 128), np.float32), masks], axis=1))


CPO, _cp0 = _const_P()
CP_W = _cp0.shape[1]
CA_W = 2 * T + 2 * TH
CB_W = 3072


def _rope_tables(core):
    j = core % 4
    pos = np.arange(j * 1024 - 128, j * 1024 + 1024).astype(np.float32)
    inv = (500000.0 ** (-np.arange(0, 16, 2, dtype=np.float32) / 16.0)).astype(np.float32)
    ang = pos[:, None] * inv[None, :]
    cos = np.cos(ang).astype(np.float32)
    sin = np.sin(ang).astype(np.float32)
    C = np.ones((128, TH), np.float32)
    S = np.zeros((128, TH), np.float32)
    for p in range(128):
        d = p % 64
        if d < 8:
            C[p] = cos[:, d]
            S[p] = -sin[:, d]
        elif d < 16:
            C[p] = cos[:, d - 8]
            S[p] = sin[:, d - 8]
    cq = (C[:, 128:] * 0.125).astype(np.float32)
    sq = (S[:, 128:] * 0.125).astype(np.float32)
    return np.ascontiguousarray(np.concatenate([cq, sq, C, S], axis=1))


_CACHE = {}


def _get_prog(debug=()):
    key = tuple(debug)
    if key not in _CACHE:
        _CACHE[key] = build(debug)
    return _CACHE[key]


def kernel(x, w_in, b_in, sinks, sgu_ln_g, sgu_ln_b, w_spatial, b_spatial,
           w_branch_attn, w_branch_sgu, w_out, ln1_g, ln1_b, w_router, router_bias,
           w1, w3, w2, ws1, ws3, ws2, ln2_g, ln2_b, _debug=(), _cores=None):
    f = lambda a: np.ascontiguousarray(np.asarray(a, dtype=np.float32))
    x = f(x)
    nc, dbg_names = _get_prog(_debug)
    small = any(d_.startswith("lvl") and float(d_[3:]) < 7 for d_ in _debug)
    bc = lambda v, n: np.ascontiguousarray(np.broadcast_to(f(v).reshape(1, n), (128, n)))
    shared = {
        "w_in": f(w_in)[0], "w_a": f(w_branch_attn)[0], "w_b": f(w_branch_sgu)[0], "w_o": f(w_out)[0],
        "w_r": f(w_router)[0], "w1": f(w1)[0][:1] if small else f(w1)[0], "w3": f(w3)[0][:1] if small else f(w3)[0],
        "w2": f(w2)[0][:1] if small else f(w2)[0],
        "ws1": f(ws1)[0], "ws3": f(ws3)[0], "ws2": f(ws2)[0],
        "cB": np.ascontiguousarray(np.concatenate([bc(sgu_ln_g, 1024), bc(sgu_ln_b, 1024), bc(b_spatial, 1024)], axis=1)),
        "wsT": np.ascontiguousarray(f(w_spatial)[0].transpose(2, 0, 1)),
        "brow": np.ascontiguousarray(np.concatenate([f(b_in)[0, COL_V:COL_V + 256], f(b_in)[0, COL_VG:COL_VG + 1024]])[None, :]),
        "lnv": np.ascontiguousarray(np.stack([bc(ln1_g, D), bc(ln1_b, D), bc(ln2_g, D), bc(ln2_b, D)])),
    }
    in_maps = []
    for c in range(NCORES):
        b, j = c // 4, c % 4
        t0 = j * 1024
        xin = np.zeros((TH, D), np.float32)
        xin[128:] = x[b, t0:t0 + 1024]
        if j > 0:
            xin[:128] = x[b, t0 - 128:t0]
        _, cPv = _const_P(f(b_in)[0], f(sinks)[0], f(router_bias)[0], c)
        m = dict(shared)
        m["xin"] = xin
        m["cP"] = cPv
        m["cbf"] = _const_bf(c)
        m["cA"] = _rope_tables(c)
        in_maps.append(m)
    cores = list(range(NCORES)) if _cores is None else _cores
    res = run_bass_kernel_spmd(nc, [in_maps[c] for c in cores], core_ids=list(range(len(cores))))
    if _debug:
        return res.results
    out = np.zeros((2, 4096, D), np.float32)
    for c in range(NCORES):
        b, j = c // 4, c % 4
        out[b, j * 1024:(j + 1) * 1024] = res.results[c]["out"]
    return out
```

```python
import numpy as np
from contextlib import ExitStack
import concourse.bass as bass
import concourse.mybir as mybir
from concourse.bass_utils import run_bass_kernel_spmd

F32 = mybir.dt.float32
BF16 = mybir.dt.bfloat16
I32 = mybir.dt.int32
AF = mybir.ActivationFunctionType
ALU = mybir.AluOpType

NCORES = 8
D = 2048
T = 1024
TH = 1152
NT = 8
IN_W = 8704
NE = 64
CAP = 256
ALPHA = 2.0 ** 0.25
EPS = 1e-5
NEG = -30000.0
COL_K, COL_V, COL_U, COL_VG, COL_GA, COL_GB = 2048, 2304, 2560, 3584, 4608, 6656

ENGS = ("pe", "act", "dve", "pool", "sp")
SAME_ENGINE_SYNC = True


class Buf:
    __slots__ = ("name", "ws", "r", "pr")

    def __init__(self, name=""):
        self.name = name
        self.ws = []
        self.r = []
        self.pr = []


class DmaSem:
    def __init__(self, sem):
        self.sem = sem
        self.count = 0


class Prog:
    def __init__(self, nc, stack):
        self.nc = nc
        self.stack = stack
        self.ops = {e: [] for e in ENGS}
        self.seen = {e: {} for e in ENGS}
        self.esem = {e: stack.enter_context(nc.semaphore("es_" + e)) for e in ENGS}
        self.ecnt = {e: 0 for e in ENGS}
        self.dsems = []

    def dma_sem(self, name):
        d = DmaSem(self.stack.enter_context(self.nc.semaphore(name)))
        self.dsems.append(d)
        return d

    def _waits(self, eng, reads, writes):
        need = {}

        def add(ev):
            if ev is None:
                return
            sem, val, src = ev
            if src == eng and (eng == "pe" or not SAME_ENGINE_SYNC):
                return
            k = id(sem)
            if self.seen[eng].get(k, 0) >= val:
                return
            if k not in need or need[k][1] < val:
                need[k] = (sem, val)

        for b in reads:
            for ev in b.ws:
                add(ev)
        for b in writes:
            for ev in (b.r if b.r else b.pr):
                add(ev)
        for k, (sem, val) in need.items():
            self.seen[eng][k] = val
        return list(need.values())

    def _post(self, ev, reads, writes):
        for b in reads:
            b.r.append(ev)
            if len(b.r) > 64:
                best = {}
                for e2 in b.r:
                    k = id(e2[0])
                    if k not in best or best[k][1] < e2[1]:
                        best[k] = e2
                b.r = list(best.values())
        for b in writes:
            if b.r:
                b.ws = [ev]
                b.pr = b.r
                b.r = []
            else:
                b.ws.append(ev)
                if len(b.ws) > 32:
                    best = {}
                    for e2 in b.ws:
                        k = id(e2[0])
                        if k not in best or best[k][1] < e2[1]:
                            best[k] = e2
                    b.ws = list(best.values())

    def op(self, eng, emit, reads=(), writes=()):
        waits = self._waits(eng, reads, writes)
        self.ecnt[eng] += 1
        ev = (self.esem[eng], self.ecnt[eng], eng)
        self.ops[eng].append((waits, emit, (self.esem[eng], 1)))
        self._post(ev, reads, writes)
        return ev

    def dma(self, eng, dsem, emit, reads=(), writes=()):
        waits = self._waits(eng, reads, writes)
        dsem.count += 16
        ev = (dsem.sem, dsem.count, "dma")
        self.ops[eng].append((waits, emit, (dsem.sem, 16)))
        self._post(ev, reads, writes)
        return ev

    def barrier(self):
        for e in ENGS:
            waits = []
            for o in ENGS:
                if (o != e or (e != "pe" and SAME_ENGINE_SYNC)) and self.ecnt[o] > self.seen[e].get(id(self.esem[o]), 0):
                    waits.append((self.esem[o], self.ecnt[o]))
                    self.seen[e][id(self.esem[o])] = self.ecnt[o]
            for d in self.dsems:
                if d.count > self.seen[e].get(id(d.sem), 0):
                    waits.append((d.sem, d.count))
                    self.seen[e][id(d.sem)] = d.count
            self.ops[e].append((waits, None, None))

    def wait_all(self, eng, bufs):
        waits = self._waits(eng, bufs, ())
        self.ops[eng].append((waits, None, None))

    def emit(self):
        nc = self.nc
        with nc.Block() as block:
            def run(engname):
                def f(e):
                    for waits, emit, inc in self.ops[engname]:
                        for sem, val in waits:
                            e.wait_ge(sem, val)
                        if emit is not None:
                            ins = emit(e)
                            ins.then_inc(inc[0], inc[1])
                return f
            block.tensor(run("pe"))
            block.scalar(run("act"))
            block.vector(run("dve"))
            block.gpsimd(run("pool"))
            block.sync(run("sp"))


def mm(P, mms, reads, writes):
    def f(e, mms=mms):
        r = None
        for m in mms:
            r = e.matmul(**m)
        return r
    return P.op("pe", f, reads, writes)


def tr(P, trs, reads, writes):
    def f(e, trs=trs):
        r = None
        for t in trs:
            r = e.transpose(**t)
        return r
    return P.op("pe", f, reads, writes)


def act(P, out, in_, func, reads, writes, bias=None, scale=None):
    kw = {}
    if bias is not None:
        kw["bias"] = bias
    if scale is not None:
        kw["scale"] = scale
    return P.op("act", lambda e: e.activation(out=out, in_=in_, func=func, **kw), reads, writes)


def tt(P, eng, out, in0, in1, op, reads, writes):
    return P.op(eng, lambda e: e.tensor_tensor(out=out, in0=in0, in1=in1, op=op), reads, writes)


def ts(P, eng, out, in0, s1, s2, op0, op1, reads, writes):
    if op1 is None:
        return P.op(eng, lambda e: e.tensor_scalar(out=out, in0=in0, scalar1=s1, scalar2=None, op0=op0), reads, writes)
    return P.op(eng, lambda e: e.tensor_scalar(out=out, in0=in0, scalar1=s1, scalar2=s2, op0=op0, op1=op1), reads, writes)


def stt(P, eng, out, in0, scalar, in1, op0, op1, reads, writes):
    return P.op(eng, lambda e: e.scalar_tensor_tensor(out=out, in0=in0, scalar=scalar, in1=in1, op0=op0, op1=op1), reads, writes)


def cp(P, eng, out, in_, reads, writes):
    if eng == "act":
        return P.op("act", lambda e: e.copy(out=out, in_=in_), reads, writes)
    return P.op(eng, lambda e: e.tensor_copy(out=out, in_=in_), reads, writes)


def dma(P, eng, dsem, out, in_, reads, writes):
    return P.dma(eng, dsem, lambda e: e.dma_start(out=out, in_=in_), reads, writes)


def gather(P, dsem, out, src, idx_ap, reads, writes):
    return P.dma("pool", dsem, lambda e: e.indirect_dma_start(
        out=out, out_offset=None, in_=src,
        in_offset=bass.IndirectOffsetOnAxis(ap=idx_ap, axis=0)), reads, writes)


class Region:
    def __init__(self, arena, base, size):
        self.arena = arena
        self.base = base
        self.size = size
        self.off = 0

    def reset(self):
        self.off = 0

    def alloc(self, shape, dt, parts=128):
        n = 1
        for d in shape[1:]:
            n *= d
        words = n if dt in (F32, I32) else (n + 1) // 2
        assert self.off + words <= self.size, (self.off, words, self.size)
        v = self.arena[0:parts, self.base + self.off:self.base + self.off + words]
        self.off += words
        if dt != F32:
            v = v.bitcast(dt)
        if len(shape) == 3:
            v = v.rearrange("p (a b) -> p a b", a=shape[1])
        elif len(shape) == 4:
            v = v.rearrange("p (a b c) -> p a b c", a=shape[1], b=shape[2])
        return v


NW_ARENA = 52800


def build(debug=()):
    nc = bass.Bass("TRN2", target_bir_lowering=False)
    dbg = {}
    lvl = 8
    for d_ in debug:
        if d_.startswith("lvl"):
            lvl = float(d_[3:])

    def din(name, shape, dt=F32):
        return nc.dram_tensor(name, list(shape), dt, kind="ExternalInput").ap()

    xin = din("xin", [TH, D])
    w_in = din("w_in", [D, IN_W])
    w_a = din("w_a", [D, D])
    w_b = din("w_b", [1024, D])
    w_o = din("w_o", [D, D])
    w_r = din("w_r", [D, NE])
    ne_decl = NE if lvl >= 7 else 1
    w1 = din("w1", [ne_decl, D, 512])
    w3 = din("w3", [ne_decl, D, 512])
    w2 = din("w2", [ne_decl, 512, D])
    ws1 = din("ws1", [D, 512])
    ws3 = din("ws3", [D, 512])
    ws2 = din("ws2", [512, D])
    cP_d = din("cP", [128, CP_W])
    cbf_d = din("cbf", [128, 2048])
    cA_d = din("cA", [128, CA_W])
    cB_d = din("cB", [128, CB_W])
    wsT_d = din("wsT", [128, 8, 128])
    brow_d = din("brow", [1, 1280])
    lnv_d = din("lnv", [4, 128, D])
    out_d = nc.dram_tensor("out", [T, D], F32, kind="ExternalOutput").ap()

    x1bf_d = nc.dram_tensor("x1bf_scr", [T, D], BF16).ap()
    x1f_d = nc.dram_tensor("x1f_scr", [T, D], F32).ap()
    ysc_d = nc.dram_tensor("y_scr", [NE * CAP, D], F32).ap()

    def dbg_out(name, shape, dt=F32):
        if name in debug:
            dbg[name] = nc.dram_tensor("dbg_" + name, list(shape), dt, kind="ExternalOutput").ap()
            return dbg[name]
        return None

    w_in_v = w_in.rearrange("(c p) n -> p c n", p=128)
    w_a_v = w_a.rearrange("(c p) n -> p c n", p=128)
    w_b_v = w_b.rearrange("(c p) n -> p c n", p=128)
    w_o_v = w_o.rearrange("(c p) n -> p c n", p=128)
    w_r_v = w_r.rearrange("(c p) n -> p c n", p=128)
    ws1_v = ws1.rearrange("(c p) n -> p c n", p=128)
    ws3_v = ws3.rearrange("(c p) n -> p c n", p=128)
    ws2_v = ws2.rearrange("(c p) n -> p c n", p=128)

    with ExitStack() as top:
        P = Prog(nc, top)
        arena_t = top.enter_context(nc.sbuf_tensor("arena", [128, NW_ARENA], F32))
        arena = arena_t[:, :]
        oP, oX = 0, 5504
        oQ = oX + 9216
        oU = oQ + 8192
        oK = oU + 4096
        oW = oK + 8192
        oT = oW + 12288
        R_P = Region(arena, oP, oX)
        R_X = Region(arena, oX, 9216)
        R_Q = Region(arena, oQ, 8192)
        R_U = Region(arena, oU, 4096)
        R_K = Region(arena, oK, 8192)
        R_W = Region(arena, oW, 12288)
        R_T = Region(arena, oT, NW_ARENA - oT)
        R_XQU = Region(arena, oX, oK - oX)
        R_WT = Region(arena, oW, NW_ARENA - oW)

        pb = [top.enter_context(nc.psum_tensor(f"pb{i}", [128, 512], F32)) for i in range(8)]
        pbB = [Buf(f"pb{i}") for i in range(8)]
        ptb = [pb[6 + i][:, 0:256].bitcast(BF16).rearrange("p (a b) -> p a b", a=4) for i in range(2)]
        ptbB = [pbB[6], pbB[7]]

        ncs = [0]

        def cs():
            ncs[0] += 1
            return P.dma_sem(f"s_c{ncs[0]}")
        s_w = [P.dma_sem(f"s_w{i}") for i in range(6)]
        s_xp = [P.dma_sem(f"s_xp{i}") for i in range(2)]
        s_x = [P.dma_sem(f"s_x{i}") for i in range(2)]
        s_xbf = P.dma_sem("s_xbf")
        s_xf = P.dma_sem("s_xf")
        s_ysc = [P.dma_sem(f"s_ysc{i}") for i in range(2)]
        s_g = [P.dma_sem(f"s_g{i}") for i in range(2)]
        s_yg = [P.dma_sem(f"s_yg{i}") for i in range(8)]
        s_out = [P.dma_sem(f"s_out{i}") for i in range(2)]
        s_dbg = P.dma_sem("s_dbg")
        dbgB = Buf("dbg")
        outB = Buf("out")

        cP = R_P.alloc([128, CP_W], F32)
        cPB = Buf("cP")
        dma(P, "sp", cs(), cP, cP_d, [], [cPB])

        def cpv(name, n):
            return cP[:, CPO[name]:CPO[name] + n]
        ident_f = cpv("ident", 128)
        psw_f = cpv("psw", 128)
        trilT = cpv("trilT", 128)
        bias_pm = cpv("bias_pm", 68)
        bias_k = cpv("bias_k", 4)
        sinks_pm = cpv("sinks_pm", 16)
        rbias = cpv("rbias", 64)
        iota_cap = cpv("iota_cap", CAP)
        ecap1 = cpv("ecap1", 64)
        tokhl = cpv("tokhl", 16)

        cbf = R_P.alloc([128, 2048], BF16)
        cbfB = Buf("cbf")
        dma(P, "pool", cs(), cbf, cbf_d, [], [cbfB])
        ident_bf = cbf[:, 0:128]
        ones_bf = cbf[:, 128:256]
        lstrict_bf = cbf[:, 256:384]
        masks_bf = cbf[:, 512:512 + 1536].rearrange("p (m n) -> p m n", m=3)
        esink = R_P.alloc([128, 16], F32)
        esinkB = Buf("esink")
        act(P, esink, sinks_pm, AF.Exp, [cPB], [esinkB])
        Gall = R_P.alloc([128, 8, 64], F32)
        sel = R_P.alloc([128, 8, 64], F32)
        selb = R_P.alloc([128, 8, 64], BF16)
        pos = R_P.alloc([128, 8, 64], F32)
        GI5 = R_P.alloc([128, 8, 64, 5], BF16)
        addr8 = R_P.alloc([128, 8, 8], F32)
        addr8i = R_P.alloc([128, 8, 8], I32)
        idx_all = R_P.alloc([128, 64, 2], I32)
        gw_all = R_P.alloc([128, 64, 2], F32)

        xT = R_X.alloc([128, 16, TH], BF16)
        xTB = [Buf(f"xT{i}") for i in range(9)]
        qT = R_Q.alloc([128, 16, T], BF16)
        qTB = [[Buf(f"qT{g}_{i}") for i in range(8)] for g in range(4)]
        uT = R_U.alloc([128, 8, T], BF16)
        uTB = [[Buf(f"uT{gb}_{i}") for i in range(8)] for gb in range(2)]
        kT = R_K.alloc([128, 4, TH], BF16)
        kTB = [Buf(f"kT{g}") for g in range(4)]
        Vs = R_K.alloc([128, 9, 256], BF16)
        VB = [Buf(f"V{i}") for i in range(9)]
        vn = R_K.alloc([128, 8, 1024], BF16)
        vnB = [Buf(f"vn{i}") for i in range(8)]

        NSL = 3
        slab = [R_W.alloc([128, 8192], BF16) for i in range(NSL)]
        slabB = [Buf(f"slab{i}") for i in range(NSL)]
        slab_rr = [0]

        def next_slab():
            i = slab_rr[0] % NSL
            slab_rr[0] += 1
            return i

        def load_slab(src_ap, kc, ncols, pieces=2):
            i = next_slab()
            v = slab[i][:, 0:kc * ncols].rearrange("p (c n) -> p c n", c=kc)
            step = kc // pieces
            for h in range(pieces):
                dma(P, "pool", s_w[i], v[:, h * step:(h + 1) * step, :], src_ap[:, h * step:(h + 1) * step, :], [], [slabB[i]])
            return v, slabB[i]

        if lvl >= 1:
            Ru = Region(arena, oU, 4096)
            cqs = Ru.alloc([128, 2 * T], F32)
            R_T.reset()
            cks = R_T.alloc([128, 2 * TH], F32)
            cAB = Buf("cA")
            dma(P, "sp", cs(), cqs, cA_d[:, 0:2 * T], [], [cAB])
            dma(P, "sp", cs(), cks, cA_d[:, 2 * T:2 * T + 2 * TH], [], [cAB])
            cosq = cqs[:, 0:T]
            sinq = cqs[:, T:2 * T]
            cosk = cks[:, 0:TH]
            sink_ = cks[:, TH:2 * TH]
            Rk = Region(arena, oK + 2304, 8192 - 2304)
            xbf = [Rk.alloc([128, D], BF16) for i in range(2)]
            xbfB = [Buf(f"xbf{i}") for i in range(2)]
            qf = [Rk.alloc([128, 512], F32) for i in range(2)]
            qfB = [Buf(f"qf{i}") for i in range(2)]
            t1 = [Rk.alloc([128, 512], F32) for i in range(2)]
            t1B = [Buf(f"t1_{i}") for i in range(2)]
            t2 = [Rk.alloc([128, 512], F32) for i in range(2)]
            t2B = [Buf(f"t2_{i}") for i in range(2)]

            for i in range(9):
                b = i % 2
                dma(P, "pool", s_xp[b], xbf[b], xin[i * 128:(i + 1) * 128, :], [], [xbfB[b]])
                for j in range(4):
                    hb = (i * 4 + j) % 2
                    tr(P, [dict(out=ptb[hb][:, jj, :], in_=xbf[b][:, (4 * j + jj) * 128:(4 * j + jj + 1) * 128], identity=ident_bf)
                           for jj in range(4)], [xbfB[b], cbfB], [ptbB[hb]])
                    cp(P, "dve" if j % 2 == 0 else "act", xT[:, 4 * j:4 * j + 4, i * 128:(i + 1) * 128], ptb[hb], [ptbB[hb]], [xTB[i]])

            it = [0]

            def rope_chunk(bank, bankB, bias_ap, cos_ap, sin_ap, out_ap, n, wr):
                k = it[0] % 2
                it[0] += 1
                act(P, qf[k][:, 0:n], bank[:, 0:n], AF.Identity, [bankB, cPB], [qfB[k]], bias=bias_ap)
                pbk = 4 + k
                mm(P, [dict(out=pb[pbk][:, 0:n], lhsT=psw_f, rhs=qf[k][:, 0:n], start=True, stop=True)], [qfB[k], cPB], [pbB[pbk]])
                tt(P, "pool", t1[k][:, 0:n], qf[k][:, 0:n], cos_ap, ALU.mult, [qfB[k], cAB], [t1B[k]])
                tt(P, "dve", t2[k][:, 0:n], pb[pbk][:, 0:n], sin_ap, ALU.mult, [pbB[pbk], cAB], [t2B[k]])
                tt(P, "dve", out_ap, t1[k][:, 0:n], t2[k][:, 0:n], ALU.add, [t1B[k], t2B[k]], wr)

            bk = 0
            for s in range(4):
                wv, wB = load_slab(w_in_v[:, :, 512 * s:512 * s + 512], 16, 512)
                for cc in range(4):
                    c = 4 * s + cc
                    for half in range(2):
                        tok = slice(128 + 512 * half, 128 + 512 * half + 512)
                        otok = slice(512 * half, 512 * half + 512)
                        bank = bk % 4
                        bk += 1
                        mm(P, [dict(out=pb[bank][:, :], lhsT=wv[:, kc, cc * 128:(cc + 1) * 128], rhs=xT[:, kc, tok],
                                    start=(kc == 0), stop=(kc == 15)) for kc in range(16)],
                           [wB] + xTB[1 + 4 * half:5 + 4 * half], [pbB[bank]])
                        rope_chunk(pb[bank], pbB[bank], bias_pm[:, c:c + 1], cosq[:, otok], sinq[:, otok],
                                   qT[:, c, otok], 512, [qTB[c // 4][4 * half + ii] for ii in range(4)])
            wv, wB = load_slab(w_in_v[:, :, COL_K:COL_K + 256], 16, 256)
            iw = next_slab()
            wkd = slab[iw][:, :].rearrange("p (c g d) -> p c g d", c=16, g=4)
            wkdB = slabB[iw]
            wv4 = wv.rearrange("p c (g d) -> p c g d", g=4)
            cp(P, "pool", wkd[:, :, :, 0:64], wv4, [wB], [wkdB])
            cp(P, "pool", wkd[:, :, :, 64:128], wv4, [wB], [wkdB])
            for g in range(4):
                for (t0_, n) in ((0, 512), (512, 512), (1024, 128)):
                    bank = bk % 4
                    bk += 1
                    mm(P, [dict(out=pb[bank][:, 0:n], lhsT=wkd[:, kc, g, :], rhs=xT[:, kc, t0_:t0_ + n],
                                start=(kc == 0), stop=(kc == 15)) for kc in range(16)],
                       [wkdB] + xTB, [pbB[bank]])
                    rope_chunk(pb[bank], pbB[bank], bias_k[:, g:g + 1], cosk[:, t0_:t0_ + n], sink_[:, t0_:t0_ + n],
                               kT[:, g, t0_:t0_ + n], n, [kTB[g]])
            P.barrier()

        if lvl >= 2:
            R_T.reset()
            cB = R_T.alloc([128, 2048], F32)
            cBB = Buf("cB")
            dma(P, "sp", cs(), cB, cB_d[:, 0:2048], [], [cBB])
            lng = cB[:, 0:1024]
            lnb = cB[:, 1024:2048]
            brow = R_T.alloc([1, 1280], BF16, parts=1)
            browB = Buf("brow")
            dma(P, "pool", cs(), brow, brow_d, [], [browB])
            vgt = [R_T.alloc([128, 1024], F32) for i in range(2)]
            vgtB = [Buf(f"vgt{i}") for i in range(2)]
            stt_ = [R_T.alloc([128, 8, 6], F32) for i in range(2)]
            mv = [R_T.alloc([128, 8, 2], F32) for i in range(2)]
            rstd = [R_T.alloc([128, 8], F32) for i in range(2)]
            lnB = [Buf(f"ln{i}") for i in range(2)]

            wv, wB = load_slab(w_in_v[:, :, COL_V:COL_V + 256], 16, 256)
            for i in range(9 if lvl >= 1.2 else 0):
                bank = i % 4
                mms = [dict(out=pb[bank][:, 0:256], lhsT=xT[:, kc, i * 128:(i + 1) * 128], rhs=wv[:, kc, :],
                            start=(kc == 0), stop=False) for kc in range(16)]
                mms.append(dict(out=pb[bank][:, 0:256], lhsT=ones_bf[0:1, :], rhs=brow[0:1, 0:256], start=False, stop=True))
                mm(P, mms, [wB, xTB[i], cbfB, browB], [pbB[bank]])
                cp(P, "act", Vs[:, i, :], pb[bank][:, 0:256], [pbB[bank]], [VB[i]])
            bk = 0
            for s in range(2 if lvl >= 1.3 else 0):
                wv, wB = load_slab(w_in_v[:, :, COL_U + 512 * s:COL_U + 512 * s + 512], 16, 512)
                for cc in range(4):
                    c = 4 * s + cc
                    for half in range(2):
                        tok = slice(128 + 512 * half, 128 + 512 * half + 512)
                        otok = slice(512 * half, 512 * half + 512)
                        bank = bk % 4
                        bk += 1
                        mm(P, [dict(out=pb[bank][:, :], lhsT=wv[:, kc, cc * 128:(cc + 1) * 128], rhs=xT[:, kc, tok],
                                    start=(kc == 0), stop=(kc == 15)) for kc in range(16)],
                           [wB] + xTB[1 + 4 * half:5 + 4 * half], [pbB[bank]])
                        act(P, uT[:, c, otok], pb[bank][:, :], AF.Gelu_apprx_tanh, [pbB[bank], cPB],
                            [uTB[c // 4][4 * half + ii] for ii in range(4)], bias=bias_pm[:, 20 + c:21 + c])
            wvs = []
            for s in range(2):
                wvs.append(load_slab(w_in_v[:, :, COL_VG + 512 * s:COL_VG + 512 * s + 512], 16, 512))
            for i in range(8 if lvl >= 1.4 else 0):
                k = i % 2
                for s in range(2):
                    bank = 4 + s
                    wv, wB = wvs[s]
                    mms = [dict(out=pb[bank][:, :], lhsT=xT[:, kc, (i + 1) * 128:(i + 2) * 128], rhs=wv[:, kc, :],
                                start=(kc == 0), stop=False) for kc in range(16)]
                    mms.append(dict(out=pb[bank][:, :], lhsT=ones_bf[0:1, :], rhs=brow[0:1, 256 + 512 * s:256 + 512 * s + 512],
                                    start=False, stop=True))
                    mm(P, mms, [wB, xTB[i + 1], cbfB, browB], [pbB[bank]])
                    act(P, vgt[k][:, 512 * s:512 * s + 512], pb[bank][:, :], AF.Gelu_apprx_tanh, [pbB[bank]], [vgtB[k]])
                v3 = vgt[k].rearrange("p (g c) -> p g c", g=8)
                if lvl < 1.5:
                    continue
                for g_ in range(8):
                    P.op("dve", lambda e, o=stt_[k], v=v3, g_=g_: e.bn_stats(out=o[:, g_, :], in_=v[:, g_, :]), [vgtB[k]], [lnB[k]])
                    P.op("dve", lambda e, o=mv[k], s_=stt_[k], g_=g_: e.bn_aggr(out=o[:, g_, :], in_=s_[:, g_, :]), [lnB[k]], [lnB[k]])
                if lvl < 1.6:
                    continue
                act(P, rstd[k], mv[k][:, :, 1], AF.Sqrt, [lnB[k]], [lnB[k]], bias=EPS)
                P.op("dve", lambda e, o=rstd[k]: e.reciprocal(out=o, in_=o), [lnB[k]], [lnB[k]])
                if lvl < 1.7:
                    continue
                tt(P, "dve", v3, v3, mv[k][:, :, 0:1].broadcast_to([128, 8, 128]), ALU.subtract, [vgtB[k], lnB[k]], [vgtB[k]])
                tt(P, "dve", v3, v3, rstd[k].unsqueeze(2).broadcast_to([128, 8, 128]), ALU.mult, [vgtB[k], lnB[k]], [vgtB[k]])
                tt(P, "pool", vgt[k], vgt[k], lng, ALU.mult, [vgtB[k], cBB], [vgtB[k]])
                tt(P, "pool", vn[:, i, :], vgt[k], lnb, ALU.add, [vgtB[k], cBB], [vnB[i]])
            if dbg_out("vn", [128, 8, 1024], BF16) is not None:
                dma(P, "sp", s_dbg, dbg["vn"], vn, vnB, [dbgB])
            if dbg_out("qT", [128, 16, T], BF16) is not None:
                dma(P, "sp", s_dbg, dbg["qT"], qT, [b for r in qTB for b in r], [dbgB])
            if dbg_out("kT", [128, 4, TH], BF16) is not None:
                dma(P, "sp", s_dbg, dbg["kT"], kT, kTB, [dbgB])
            if dbg_out("Vs", [128, 9, 256], BF16) is not None:
                dma(P, "sp", s_dbg, dbg["Vs"], Vs, VB, [dbgB])
            if dbg_out("uT", [128, 8, T], BF16) is not None:
                dma(P, "sp", s_dbg, dbg["uT"], uT, [b for r in uTB for b in r], [dbgB])
            P.barrier()

        if lvl > 2:
            Rw = Region(arena, oW, 12288)
            PT = [Rw.alloc([128, 2, 8, 128], BF16) for i in range(2)]
            PTB = [Buf(f"PT{i}") for i in range(2)]
            dn = [Rw.alloc([128, 512], F32) for i in range(2)]
            dnB = [Buf(f"dn{i}") for i in range(2)]
            wsT_f = Rw.alloc([128, 8, 128], F32)
            wsT_b = Rw.alloc([128, 8, 128], BF16)
            wsB = Buf("wsT")
            bs_bc = Rw.alloc([128, 1024], F32)
            cBB = Buf("cB2")
            dma(P, "sp", cs(), bs_bc, cB_d[:, 2048:3072], [], [cBB])
            dma(P, "sp", cs(), wsT_f, wsT_d, [], [wsB])
            tt(P, "dve", wsT_b, wsT_f, trilT.unsqueeze(1).broadcast_to([128, 8, 128]), ALU.mult, [wsB, cPB], [wsB])
            sgt = [Rw.alloc([128, 512], F32) for i in range(2)]
            sgtB = [Buf(f"sgt{i}") for i in range(2)]
            kz = [Rw.alloc([128, 4, TH], BF16) for r_ in range(2)]
            kzB = Buf("kz")
            P.op("pool", lambda e: e.memset(kz[0][64:128, :, :], 0.0), [], [kzB])
            P.op("pool", lambda e: e.memset(kz[1][0:64, :, :], 0.0), [], [kzB])
            cp(P, "pool", kz[0][0:64, :, :], kT[0:64, :, :], kTB, [kzB])
            cp(P, "pool", kz[1][64:128, :, :], kT[64:128, :, :], kTB, [kzB])

            it = 0
            for i in range(8):
                for g in range(4 if lvl >= 2.2 else 0):
                    k = it % 2
                    it += 1
                    for h in range(2):
                        kt = i + h
                        mk = 2 if h == 1 else (0 if i == 0 else 1)
                        for hb in range(2):
                            bank = 2 * h + hb
                            mms = [dict(out=pb[bank][:, :], lhsT=ident_bf, rhs=masks_bf[:, mk, :], start=True, stop=False)]
                            for sl in range(4):
                                h8 = 4 * hb + sl
                                c = 4 * g + h8 // 2
                                r = h8 % 2
                                mms.append(dict(out=pb[bank][:, sl * 128:(sl + 1) * 128],
                                                lhsT=kz[r][:, g, kt * 128:(kt + 1) * 128],
                                                rhs=qT[:, c, i * 128:(i + 1) * 128],
                                                start=False, stop=(sl == 3)))
                            mm(P, mms, [cbfB, kzB, qTB[g][i]], [pbB[bank]])
                            act(P, PT[k][:, h, 4 * hb:4 * hb + 4, :], pb[bank][:, :].rearrange("p (a b) -> p a b", a=4),
                                AF.Exp, [pbB[bank]], [PTB[k]])
                    if lvl < 2.3:
                        continue
                    mms = []
                    for cl in range(4):
                        for r in range(2):
                            hs = 2 * cl + r
                            tp = dict(tile_position=(0, 64)) if r == 1 else {}
                            for h in range(2):
                                kt = i + h
                                mms.append(dict(out=pb[4][r * 64:(r + 1) * 64, cl * 128:(cl + 1) * 128],
                                                lhsT=Vs[:, kt, g * 64:(g + 1) * 64], rhs=PT[k][:, h, hs, :],
                                                start=(h == 0), stop=(h == 1), **tp))
                            for h in range(2):
                                mms.append(dict(out=pb[5][r * 64:(r + 1) * 64, cl * 128:(cl + 1) * 128],
                                                lhsT=ones_bf[:, 0:64], rhs=PT[k][:, h, hs, :],
                                                start=(h == 0), stop=(h == 1), **tp))
                    mm(P, mms, [PTB[k], VB[i], VB[i + 1], cbfB], [pbB[4], pbB[5]])
                    if lvl < 2.4:
                        continue
                    d3 = dn[k].rearrange("p (a b) -> p a b", a=4)
                    tt(P, "dve", d3, pb[5][:, :].rearrange("p (a b) -> p a b", a=4),
                       esink[:, 4 * g:4 * g + 4].unsqueeze(2).broadcast_to([128, 4, 128]), ALU.add, [pbB[5], esinkB], [dnB[k]])
                    P.op("dve", lambda e, o=dn[k]: e.reciprocal(out=o, in_=o), [dnB[k]], [dnB[k]])
                    tt(P, "dve", qT[:, 4 * g:4 * g + 4, i * 128:(i + 1) * 128], pb[4][:, :].rearrange("p (a b) -> p a b", a=4),
                       d3, ALU.mult, [pbB[4], dnB[k]], [qTB[g][i]])
                for gb in range(2 if (lvl >= 3 or lvl == 2.1) else 0):
                    k2 = (2 * i + gb) % 2
                    mms = []
                    for gl in range(4):
                        g8 = 4 * gb + gl
                        mms.append(dict(out=pb[6][:, gl * 128:(gl + 1) * 128], lhsT=vn[:, i, g8 * 128:(g8 + 1) * 128],
                                        rhs=wsT_b[:, g8, :], start=True, stop=True))
                    mm(P, mms, [vnB[i], wsB], [pbB[6]])
                    tt(P, "dve", sgt[k2], pb[6][:, :], bs_bc[:, 512 * gb:512 * gb + 512], ALU.add, [pbB[6], cBB], [sgtB[k2]])
                    uv = uT[:, 4 * gb:4 * gb + 4, i * 128:(i + 1) * 128]
                    tt(P, "pool", uv, sgt[k2].rearrange("p (a b) -> p a b", a=4), uv, ALU.mult, [sgtB[k2], uTB[gb][i]], [uTB[gb][i]])
            if dbg_out("attnT", [128, 16, T], BF16) is not None:
                dma(P, "sp", s_dbg, dbg["attnT"], qT, [b for r in qTB for b in r], [dbgB])
            if dbg_out("sguT", [128, 8, T], BF16) is not None:
                dma(P, "sp", s_dbg, dbg["sguT"], uT, [b for r in uTB for b in r], [dbgB])
            P.barrier()
        attnT = qT
        sguT = uT
        attnB = [b for r in qTB for b in r]
        sguB = [b for r in uTB for b in r]

        mT = Region(arena, oK, 8192).alloc([128, 16, T], BF16)
        mTB = [Buf(f"mT{i}") for i in range(2)]
        if lvl >= 4:
            R_T.reset()
            sa = [R_T.alloc([128, 512], F32) for i in range(2)]
            sg = [R_T.alloc([128, 512], F32) for i in range(2)]
            ta = [R_T.alloc([128, 512], F32) for i in range(2)]
            tb_ = [R_T.alloc([128, 512], F32) for i in range(2)]
            saB = [Buf() for _ in range(2)]
            sgB = [Buf() for _ in range(2)]
            taB = [Buf() for _ in range(2)]
            tbB = [Buf() for _ in range(2)]
            it = 0
            for op_ in range(8):
                wa, waB = load_slab(w_a_v[:, :, 256 * op_:256 * op_ + 256], 16, 256)
                wga, wgaB = load_slab(w_in_v[:, :, COL_GA + 256 * op_:COL_GA + 256 * op_ + 256], 16, 256)
                i = next_slab()
                wbv = slab[i][:, 0:2048].rearrange("p (c n) -> p c n", c=8)
                wgb = slab[i][:, 2048:2048 + 4096].rearrange("p (c n) -> p c n", c=16)
                dma(P, "pool", s_w[i], wbv, w_b_v[:, :, 256 * op_:256 * op_ + 256], [], [slabB[i]])
                dma(P, "pool", s_w[i], wgb, w_in_v[:, :, COL_GB + 256 * op_:COL_GB + 256 * op_ + 256], [], [slabB[i]])
                wbB = slabB[i]
                for cc in range(2):
                    c = 2 * op_ + cc
                    csl = slice(cc * 128, (cc + 1) * 128)
                    for half in range(2):
                        k = it % 2
                        it += 1
                        otok = slice(512 * half, 512 * half + 512)
                        xtok = slice(128 + 512 * half, 128 + 512 * half + 512)
                        mm(P, [dict(out=pb[0][:, :], lhsT=wa[:, kc, csl], rhs=attnT[:, kc, otok], start=(kc == 0), stop=(kc == 15))
                               for kc in range(16)], [waB] + attnB, [pbB[0]])
                        mm(P, [dict(out=pb[1][:, :], lhsT=wga[:, kc, csl], rhs=xT[:, kc, xtok], start=(kc == 0), stop=(kc == 15))
                               for kc in range(16)], [wgaB] + xTB, [pbB[1]])
                        mm(P, [dict(out=pb[2][:, :], lhsT=wbv[:, kc, csl], rhs=sguT[:, kc, otok], start=(kc == 0), stop=(kc == 7))
                               for kc in range(8)], [wbB] + sguB, [pbB[2]])
                        mm(P, [dict(out=pb[3][:, :], lhsT=wgb[:, kc, csl], rhs=xT[:, kc, xtok], start=(kc == 0), stop=(kc == 15))
                               for kc in range(16)], [wbB] + xTB, [pbB[3]])
                        act(P, sa[k], pb[1][:, :], AF.Sigmoid, [pbB[1], cPB], [saB[k]], bias=bias_pm[:, 36 + c:37 + c])
                        act(P, sg[k], pb[3][:, :], AF.Sigmoid, [pbB[3], cPB], [sgB[k]], bias=bias_pm[:, 52 + c:53 + c])
                        tt(P, "dve", ta[k], pb[0][:, :], sa[k], ALU.mult, [pbB[0], saB[k]], [taB[k]])
                        tt(P, "dve", tb_[k], pb[2][:, :], sg[k], ALU.mult, [pbB[2], sgB[k]], [tbB[k]])
                        tt(P, "pool", mT[:, c, otok], ta[k], tb_[k], ALU.add, [taB[k], tbB[k]], [mTB[half]])
            if dbg_out("mT", [128, 16, T], BF16) is not None:
                dma(P, "sp", s_dbg, dbg["mT"], mT, mTB, [dbgB])
            P.barrier()

        R_XQU.reset()
        r = R_XQU.alloc([128, 8, D], F32)
        rB = [Buf(f"r{i}") for i in range(8)]
        if lvl >= 5:
            Rw = Region(arena, oW, 12288)
            NS2 = 2
            slab2 = [Rw.alloc([128, 16, 512], BF16) for i in range(NS2)]
            slab2B = [Buf() for _ in range(NS2)]
            R_T.reset()
            xt = [R_T.alloc([128, 512], F32) for i in range(2)]
            xtB = [Buf() for _ in range(2)]
            xin_own = xin[128:, :]
            it = 0
            for s in range(4):
                b = s % NS2
                for h in range(2):
                    dma(P, "pool", s_w[b], slab2[b][:, 8 * h:8 * h + 8, :], w_o_v[:, 8 * h:8 * h + 8, 512 * s:512 * s + 512], [], [slab2B[b]])
                for i in range(8):
                    k = it % 2
                    it += 1
                    bank = it % 4
                    dma(P, "sp", s_x[k], xt[k], xin_own[i * 128:(i + 1) * 128, 512 * s:512 * s + 512], [], [xtB[k]])
                    mm(P, [dict(out=pb[bank][:, :], lhsT=mT[:, kc, i * 128:(i + 1) * 128], rhs=slab2[b][:, kc, :],
                                start=(kc == 0), stop=(kc == 15)) for kc in range(16)], [slab2B[b]] + mTB, [pbB[bank]])
                    stt(P, "dve", r[:, i, 512 * s:512 * s + 512], xt[k], ALPHA, pb[bank][:, :], ALU.mult, ALU.add,
                        [xtB[k], pbB[bank]], [rB[i]])
            P.barrier()

        x1T = Region(arena, oK, 8192).alloc([128, 16, T], BF16)
        x1TB = [Buf(f"x1T{i}") for i in range(8)]
        rtB = [Buf(f"rt{i}") for i in range(8)]
        posB = Buf("pos")
        igB = Buf("ig")
        x1bfB = [Buf(f"x1bf_d{i}") for i in range(8)]
        x1fB = [Buf(f"x1f_d{i}") for i in range(8)]
        if lvl > 5:
            Rw = Region(arena, oW, 12288)
            lnv = Rw.alloc([128, 2, D], F32)
            lnvB = Buf("lnv")
            dma(P, "sp", cs(), lnv[:, 0, :], lnv_d[0], [], [lnvB])
            dma(P, "sp", cs(), lnv[:, 1, :], lnv_d[1], [], [lnvB])
            wr_f = Rw.alloc([128, 16, 64], F32)
            wr_hi = Rw.alloc([128, 16, 64], BF16)
            wr_lo = Rw.alloc([128, 16, 64], BF16)
            wrB = Buf("wr")
            dma(P, "sp", cs(), wr_f, w_r_v, [], [wrB])
            cp(P, "dve", wr_hi, wr_f, [wrB], [wrB])
            tt(P, "dve", wr_lo, wr_f, wr_hi, ALU.subtract, [wrB], [wrB])
            x1b = [Rw.alloc([128, D], BF16) for i in range(1)]
            x1bB = [Buf() for _ in range(1)]
            x1lo = [Rw.alloc([128, D], BF16) for i in range(1)]
            x1loB = [Buf() for _ in range(1)]
            x1Tlo = [Rw.alloc([128, 16, 128], BF16) for i in range(2)]
            x1TloB = [Buf() for _ in range(2)]
            R_T.reset()
            st4 = [R_T.alloc([128, 4, 6], F32) for i in range(2)]
            mv2 = [R_T.alloc([128, 2], F32) for i in range(2)]
            rs2 = [R_T.alloc([128, 1], F32) for i in range(2)]
            ln2B = [Buf() for _ in range(2)]
            sc = [R_T.alloc([128, 64], F32) for i in range(2)]
            bi = [R_T.alloc([128, 64], F32) for i in range(2)]
            mx = [R_T.alloc([128, 8, 8], F32) for i in range(2)]
            gs = [R_T.alloc([128, 8], F32) for i in range(2)]
            gm = [R_T.alloc([128, 8], F32) for i in range(2)]
            m8 = [R_T.alloc([128, 8], F32) for i in range(2)]
            mk_ = [R_T.alloc([128, 64], F32) for i in range(2)]
            wsum = [R_T.alloc([128, 1], F32) for i in range(2)]
            rtsB = [Buf() for _ in range(2)]

            def layer_norm(eng2, src, srcB, g_ap, b_ap, gbB, k):
                for a_ in range(4):
                    P.op("dve", lambda e, a_=a_, o=st4[k], s_=src: e.bn_stats(out=o[:, a_, :], in_=s_[:, a_ * 512:(a_ + 1) * 512]), [srcB], [ln2B[k]])
                P.op("dve", lambda e, o=mv2[k], s_=st4[k]: e.bn_aggr(out=o, in_=s_.rearrange("p a b -> p (a b)")), [ln2B[k]], [ln2B[k]])
                act(P, rs2[k], mv2[k][:, 1:2], AF.Sqrt, [ln2B[k]], [ln2B[k]], bias=EPS)
                P.op("dve", lambda e, o=rs2[k]: e.reciprocal(out=o, in_=o), [ln2B[k]], [ln2B[k]])
                ts(P, "dve", src, src, mv2[k][:, 0:1], rs2[k][:, 0:1], ALU.subtract, ALU.mult, [srcB, ln2B[k]], [srcB])
                tt(P, eng2, src, src, g_ap, ALU.mult, [srcB, gbB], [srcB])
                tt(P, eng2, src, src, b_ap, ALU.add, [srcB, gbB], [srcB])

            for i in range(8):
                k = i % 2
                layer_norm("pool", r[:, i, :], rB[i], lnv[:, 0, :], lnv[:, 1, :], lnvB, k)
                cp(P, "act", x1b[0], r[:, i, :], [rB[i]], [x1bB[0]])
                tt(P, "dve", x1lo[0], r[:, i, :], x1b[0], ALU.subtract, [rB[i], x1bB[0]], [x1loB[0]])
                dma(P, "sp", s_xbf, x1bf_d[i * 128:(i + 1) * 128, :], x1b[0], [x1bB[0]], [x1bfB[i]])
                dma(P, "sp", s_xf, x1f_d[i * 128:(i + 1) * 128, :], r[:, i, :], [rB[i]], [x1fB[i]])
                for j in range(4):
                    hb = j % 2
                    tr(P, [dict(out=ptb[hb][:, jj, :], in_=x1b[0][:, (4 * j + jj) * 128:(4 * j + jj + 1) * 128], identity=ident_bf)
                           for jj in range(4)], [x1bB[0], cbfB], [ptbB[hb]])
                    cp(P, "dve" if j % 2 == 0 else "act", x1T[:, 4 * j:4 * j + 4, i * 128:(i + 1) * 128], ptb[hb], [ptbB[hb]], [x1TB[i]])
                for j in range(4):
                    hb = j % 2
                    tr(P, [dict(out=ptb[hb][:, jj, :], in_=x1lo[0][:, (4 * j + jj) * 128:(4 * j + jj + 1) * 128], identity=ident_bf)
                           for jj in range(4)], [x1loB[0], cbfB], [ptbB[hb]])
                    cp(P, "act" if j % 2 == 0 else "dve", x1Tlo[k][:, 4 * j:4 * j + 4, :], ptb[hb], [ptbB[hb]], [x1TloB[k]])
                mms = []
                for kc in range(16):
                    xh = x1T[:, kc, i * 128:(i + 1) * 128]
                    mms.append(dict(out=pb[5][:, 0:64], lhsT=xh, rhs=wr_hi[:, kc, :], start=(kc == 0), stop=False))
                    mms.append(dict(out=pb[5][:, 0:64], lhsT=xh, rhs=wr_lo[:, kc, :], start=False, stop=False))
                    mms.append(dict(out=pb[5][:, 0:64], lhsT=x1Tlo[k][:, kc, :], rhs=wr_hi[:, kc, :], start=False, stop=(kc == 15)))
                mm(P, mms, [x1TB[i], x1TloB[k], wrB], [pbB[5]])
                if lvl < 5.2:
                    continue
                RB = rtsB[k]
                act(P, sc[k], pb[5][:, 0:64], AF.Sigmoid, [pbB[5]], [RB])
                tt(P, "dve", bi[k], sc[k], rbias, ALU.add, [RB, cPB], [RB])
                for g in range(8):
                    P.op("dve", lambda e, o=mx[k][:, g, :], v=bi[k][:, g * 8:(g + 1) * 8]: e.max(out=o, in_=v), [RB], [RB])
                tt(P, "dve", gs[k], mx[k][:, :, 0], mx[k][:, :, 1], ALU.add, [RB], [RB])
                P.op("dve", lambda e, o=m8[k], v=gs[k]: e.max(out=o, in_=v), [RB], [RB])
                ts(P, "dve", gm[k], gs[k], m8[k][:, 3:4], None, ALU.is_ge, None, [RB], [RB])
                stt(P, "dve", mk_[k].rearrange("p (g e) -> p g e", g=8), bi[k].rearrange("p (g e) -> p g e", g=8), 2.0,
                    gm[k].unsqueeze(2).broadcast_to([128, 8, 8]), ALU.add, ALU.mult, [RB], [RB])
                P.op("dve", lambda e, o=m8[k], v=mk_[k]: e.max(out=o, in_=v), [RB], [RB])
                ts(P, "dve", sel[:, i, :], mk_[k], m8[k][:, 7:8], None, ALU.is_ge, None, [RB], [rtB[i]])
                tt(P, "dve", sc[k], sc[k], sel[:, i, :], ALU.mult, [RB, rtB[i]], [RB])
                P.op("dve", lambda e, o=wsum[k], v=sc[k]: e.reduce_sum(out=o, in_=v, axis=mybir.AxisListType.X), [RB], [RB])
                ts(P, "dve", wsum[k], wsum[k], 1e-20, 1.0 / 2.5, ALU.add, ALU.mult, [RB], [RB])
                P.op("dve", lambda e, o=wsum[k]: e.reciprocal(out=o, in_=o), [RB], [RB])
                ts(P, "dve", Gall[:, i, :], sc[k], wsum[k][:, 0:1], None, ALU.mult, None, [RB], [rtB[i]])
                cp(P, "dve", selb[:, i, :], sel[:, i, :], [rtB[i]], [rtB[i]])
            for i in range(8 if lvl >= 5.3 else 0):
                mms = [dict(out=pb[4][:, i * 64:(i + 1) * 64], lhsT=lstrict_bf, rhs=selb[:, i, :], start=True, stop=(i == 0))]
                for j in range(i):
                    mms.append(dict(out=pb[4][:, i * 64:(i + 1) * 64], lhsT=ones_bf, rhs=selb[:, j, :], start=False, stop=(j == i - 1)))
                mm(P, mms, rtB[:i + 1] + [cbfB], [pbB[4]])
            cp(P, "dve", pos.rearrange("p a b -> p (a b)"), pb[4][:, :], [pbB[4]], [posB])
            for i in range(8 if lvl >= 5.3 else 0):
                k = i % 2
                tt(P, "dve", mk_[k], pos[:, i, :], ecap1, ALU.add, [posB, cPB], [rtsB[k]])
                tt(P, "dve", mk_[k], mk_[k], sel[:, i, :], ALU.mult, [rtsB[k], rtB[i]], [rtsB[k]])
                P.op("dve", lambda e, o=addr8[:, i, :], v=mk_[k]: e.max(out=o, in_=v), [rtsB[k]], [posB])
            ts(P, "dve", addr8, addr8, -1.0, None, ALU.add, None, [posB], [posB])
            cp(P, "dve", addr8i, addr8, [posB], [posB])
            P.barrier()
            Rw = Region(arena, oW, 12288)
            gtmp = Rw.alloc([128, 8, 64], F32)
            gB = Buf("gtmp")
            thl = tokhl.rearrange("p (a b) -> p a b", a=8)
            cp(P, "pool", GI5[:, :, :, 0], thl[:, :, 0:1].broadcast_to([128, 8, 64]), [cPB], [igB])
            cp(P, "pool", GI5[:, :, :, 1], thl[:, :, 1:2].broadcast_to([128, 8, 64]), [cPB], [igB])
            cp(P, "dve", GI5[:, :, :, 2], Gall, rtB, [igB])
            tt(P, "dve", gtmp, Gall, GI5[:, :, :, 2], ALU.subtract, rtB + [igB], [gB])
            cp(P, "dve", GI5[:, :, :, 3], gtmp, [gB], [igB])
            tt(P, "dve", gtmp, gtmp, GI5[:, :, :, 3], ALU.subtract, [gB, igB], [gB])
            cp(P, "dve", GI5[:, :, :, 4], gtmp, [gB], [igB])
            S1 = [Rw.alloc([128, 8, CAP], BF16) for i in range(4)]
            S1B = [Buf() for _ in range(4)]
            it = 0
            for e_ in range(NE if lvl >= 5.4 else 0):
                k = e_ % 4
                bank = 2 + e_ // 32
                for i in range(8):
                    it += 1
                    ts(P, "dve" if it % 2 == 0 else "pool", S1[k][:, i, :], iota_cap, pos[:, i, e_:e_ + 1], sel[:, i, e_:e_ + 1],
                       ALU.is_equal, ALU.mult, [posB, rtB[i], cPB], [S1B[k]])
                c0 = ((e_ % 32) * 2) * 5
                mm(P, [dict(out=pb[bank][:, c0 + st * 5:c0 + st * 5 + 5], lhsT=S1[k][:, i, st * 128:(st + 1) * 128],
                            rhs=GI5[:, i, e_, :], start=(i == 0), stop=(i == 7)) for st in range(2) for i in range(8)],
                   [S1B[k], igB], [pbB[bank]])
            igs = Rw.alloc([128, 2, 320], F32)
            igsB = Buf("igs")
            idxf = Rw.alloc([128, 64, 2], F32)
            cp(P, "dve", igs[:, 0, :], pb[2][:, 0:320], [pbB[2]], [igsB])
            cp(P, "act", igs[:, 1, :], pb[3][:, 0:320], [pbB[3]], [igsB])
            for hb_ in range(2):
                v5 = igs[:, hb_, :].rearrange("p (e s c) -> p e s c", e=32, s=2)
                es = slice(32 * hb_, 32 * hb_ + 32)
                stt(P, "dve", idxf[:, es, :], v5[:, :, :, 0], 32.0, v5[:, :, :, 1], ALU.mult, ALU.add, [igsB], [igB])
                tt(P, "dve", gw_all[:, es, :], v5[:, :, :, 2], v5[:, :, :, 3], ALU.add, [igsB], [igB])
                tt(P, "dve", gw_all[:, es, :], gw_all[:, es, :], v5[:, :, :, 4], ALU.add, [igsB, igB], [igB])
            cp(P, "dve", idx_all, idxf, [igB], [igB])
            if dbg_out("x1", [128, 8, D]) is not None:
                dma(P, "sp", s_dbg, dbg["x1"], r, rB, [dbgB])
            if dbg_out("Gall", [128, 8, 64]) is not None:
                dma(P, "sp", s_dbg, dbg["Gall"], Gall, rtB, [dbgB])
            if dbg_out("pos", [128, 8, 64]) is not None:
                dma(P, "sp", s_dbg, dbg["pos"], pos, [posB], [dbgB])
            if dbg_out("idx_all", [128, 64, 2], I32) is not None:
                dma(P, "sp", s_dbg, dbg["idx_all"], idx_all, [igB], [dbgB])
            if dbg_out("gw_all", [128, 64, 2]) is not None:
                dma(P, "sp", s_dbg, dbg["gw_all"], gw_all, [igB], [dbgB])
            if dbg_out("addr8i", [128, 8, 8], I32) is not None:
                dma(P, "sp", s_dbg, dbg["addr8i"], addr8i, [posB], [dbgB])
            P.barrier()

        yscB = Buf("ysc")
        if lvl >= 7:
            R1 = Region(arena, oX, oK - oX)
            R2 = Region(arena, oW, NW_ARENA - oW)
            NW = 2
            w1s = [R1.alloc([128, 16, 512], BF16) for i in range(NW)]
            w3s = [R1.alloc([128, 16, 512], BF16) for i in range(NW)]
            w2s = [R1.alloc([128, 4, D], BF16), R2.alloc([128, 4, D], BF16)]
            hT = [R1.alloc([128, 4, CAP], BF16) for i in range(2)]
            w1B = [Buf() for _ in range(NW)]
            w3B = [Buf() for _ in range(NW)]
            w2B = [Buf() for _ in range(NW)]
            Xg = [R2.alloc([128, 2, D], BF16) for i in range(2)]
            XgB = [Buf() for _ in range(2)]
            XeT = [R2.alloc([128, 16, CAP], BF16) for i in range(2)]
            XeTB = [Buf() for _ in range(2)]
            hTB = [Buf() for _ in range(2)]
            sl_ = [R2.alloc([128, 2, CAP], F32) for i in range(2)]
            slB = [Buf() for _ in range(2)]
            NY = 2
            ys = [R2.alloc([128, D], F32) for i in range(NY)]
            ysB = [Buf() for _ in range(NY)]

            def load_expert(e_):
                b = e_ % NW
                for h in range(2):
                    dma(P, "pool", s_w[b], w1s[b][:, 8 * h:8 * h + 8, :],
                        w1[e_].rearrange("(c p) n -> p c n", p=128)[:, 8 * h:8 * h + 8, :], [], [w1B[b]])
                for h in range(2):
                    dma(P, "pool", s_w[2 + b], w3s[b][:, 8 * h:8 * h + 8, :],
                        w3[e_].rearrange("(c p) n -> p c n", p=128)[:, 8 * h:8 * h + 8, :], [], [w3B[b]])
                for h in range(2):
                    dma(P, "pool", s_w[4 + b], w2s[b][:, 2 * h:2 * h + 2, :],
                        w2[e_].rearrange("(c p) n -> p c n", p=128)[:, 2 * h:2 * h + 2, :], [], [w2B[b]])

            def load_tokens(e_):
                b = e_ % 2
                for st in range(2):
                    gather(P, s_g[b], Xg[b][:, st, :], x1bf_d[:, :], idx_all[:, e_, st:st + 1], [igB] + x1bfB, [XgB[b]])

            load_tokens(0)
            load_expert(0)
            yi = 0
            for e_ in range(NE):
                b = e_ % 2
                wb_ = e_ % NW
                if e_ + 1 < NE:
                    load_tokens(e_ + 1)
                    load_expert(e_ + 1)
                for st in range(2):
                    for j in range(4):
                        hb = j % 2
                        tr(P, [dict(out=ptb[hb][:, jj, :], in_=Xg[b][:, st, (4 * j + jj) * 128:(4 * j + jj + 1) * 128], identity=ident_bf)
                               for jj in range(4)], [XgB[b], cbfB], [ptbB[hb]])
                        cp(P, "dve" if j % 2 == 0 else "act", XeT[b][:, 4 * j:4 * j + 4, st * 128:(st + 1) * 128], ptb[hb],
                           [ptbB[hb]], [XeTB[b]])
                for hp in range(2):
                    for hl in range(2):
                        hc = 2 * hp + hl
                        mm(P, [dict(out=pb[0 + hp][:, hl * CAP:(hl + 1) * CAP], lhsT=w1s[wb_][:, kc, hc * 128:(hc + 1) * 128],
                                    rhs=XeT[b][:, kc, :], start=(kc == 0), stop=(kc == 15)) for kc in range(16)],
                           [w1B[wb_], XeTB[b]], [pbB[0 + hp]])
                        mm(P, [dict(out=pb[2 + hp][:, hl * CAP:(hl + 1) * CAP], lhsT=w3s[wb_][:, kc, hc * 128:(hc + 1) * 128],
                                    rhs=XeT[b][:, kc, :], start=(kc == 0), stop=(kc == 15)) for kc in range(16)],
                           [w3B[wb_], XeTB[b]], [pbB[2 + hp]])
                    act(P, sl_[hp].rearrange("p a b -> p (a b)"), pb[0 + hp][:, :], AF.Silu, [pbB[0 + hp]], [slB[hp]])
                    tt(P, "dve", hT[b][:, 2 * hp:2 * hp + 2, :].rearrange("p a b -> p (a b)"), sl_[hp].rearrange("p a b -> p (a b)"),
                       pb[2 + hp][:, :], ALU.mult, [slB[hp], pbB[2 + hp]], [hTB[b]])
                for st in range(2):
                    yk = yi % NY
                    yi += 1
                    for cg in range(4):
                        bank = 4 + (cg % 2)
                        mm(P, [dict(out=pb[bank][:, :], lhsT=hT[b][:, hc, st * 128:(st + 1) * 128],
                                    rhs=w2s[wb_][:, hc, cg * 512:(cg + 1) * 512], start=(hc == 0), stop=(hc == 3)) for hc in range(4)],
                           [hTB[b], w2B[wb_]], [pbB[bank]])
                        if cg % 2 == 0:
                            act(P, ys[yk][:, cg * 512:(cg + 1) * 512], pb[bank][:, :], AF.Copy, [pbB[bank], igB], [ysB[yk]],
                                scale=gw_all[:, e_, st:st + 1])
                        else:
                            ts(P, "dve", ys[yk][:, cg * 512:(cg + 1) * 512], pb[bank][:, :], gw_all[:, e_, st:st + 1], None,
                               ALU.mult, None, [pbB[bank], igB], [ysB[yk]])
                    dma(P, "sp", s_ysc[yk], ysc_d[e_ * CAP + st * 128:e_ * CAP + (st + 1) * 128, :], ys[yk], [ysB[yk]], [yscB])
            P.barrier()

        if lvl >= 7:
            R1 = Region(arena, oX, oK - oX)
            R2 = Region(arena, oW, NW_ARENA - oW)
            ws1s = R1.alloc([128, 16, 512], BF16)
            ws3s = R1.alloc([128, 16, 512], BF16)
            wsB_ = [Buf() for _ in range(3)]
            for h in range(2):
                dma(P, "pool", s_w[0], ws1s[:, 8 * h:8 * h + 8, :], ws1_v[:, 8 * h:8 * h + 8, :], [], [wsB_[0]])
                dma(P, "pool", s_w[1], ws3s[:, 8 * h:8 * h + 8, :], ws3_v[:, 8 * h:8 * h + 8, :], [], [wsB_[1]])
            hsT = R2.alloc([128, 4, T], BF16)
            hsB = Buf("hsT")
            sl2 = [R2.alloc([128, 512], F32) for i in range(2)]
            sl2B = [Buf() for _ in range(2)]
            it = 0
            for half in range(2):
                tok = slice(512 * half, 512 * half + 512)
                for hc in range(4):
                    k = it % 2
                    it += 1
                    mm(P, [dict(out=pb[0 + k][:, :], lhsT=ws1s[:, kc, hc * 128:(hc + 1) * 128], rhs=x1T[:, kc, tok],
                                start=(kc == 0), stop=(kc == 15)) for kc in range(16)], [wsB_[0]] + x1TB, [pbB[0 + k]])
                    mm(P, [dict(out=pb[2 + k][:, :], lhsT=ws3s[:, kc, hc * 128:(hc + 1) * 128], rhs=x1T[:, kc, tok],
                                start=(kc == 0), stop=(kc == 15)) for kc in range(16)], [wsB_[1]] + x1TB, [pbB[2 + k]])
                    act(P, sl2[k], pb[0 + k][:, :], AF.Silu, [pbB[0 + k]], [sl2B[k]])
                    tt(P, "dve", hsT[:, hc, tok], sl2[k], pb[2 + k][:, :], ALU.mult, [sl2B[k], pbB[2 + k]], [hsB])
            P.barrier()

            R1.reset()
            Yg = R1.alloc([128, 8, D], F32)
            YgB = [Buf() for _ in range(8)]
            ws2s = R1.alloc([128, 4, D], BF16)
            for h in range(2):
                dma(P, "pool", s_w[2], ws2s[:, 2 * h:2 * h + 2, :], ws2_v[:, 2 * h:2 * h + 2, :], [], [wsB_[2]])
            lnv = R2.alloc([128, 2, D], F32)
            lnvB = Buf("lnv2")
            dma(P, "sp", cs(), lnv[:, 0, :], lnv_d[2], [], [lnvB])
            dma(P, "sp", cs(), lnv[:, 1, :], lnv_d[3], [], [lnvB])
            x1r = [R2.alloc([128, D], F32) for i in range(1)]
            x1rB = [Buf() for _ in range(1)]
            acc = [R2.alloc([128, D], F32) for i in range(2)]
            accB = [Buf() for _ in range(2)]
            accP = [R2.alloc([128, D], F32) for i in range(1)]
            accPB = [Buf() for _ in range(1)]
            st4 = [R2.alloc([128, 4, 6], F32) for i in range(2)]
            mv2 = [R2.alloc([128, 2], F32) for i in range(2)]
            rs2 = [R2.alloc([128, 1], F32) for i in range(2)]
            ln2B = [Buf() for _ in range(2)]

            for i in range(8):
                k = i % 2
                for kk in range(8):
                    gather(P, s_yg[kk], Yg[:, kk, :], ysc_d[:, :], addr8i[:, i, kk:kk + 1], [posB, yscB], [YgB[kk]])
                dma(P, "sp", s_x[0], x1r[0], x1f_d[i * 128:(i + 1) * 128, :], x1fB, [x1rB[0]])
                for cg in range(4):
                    bank = 4 + (cg % 2)
                    mm(P, [dict(out=pb[bank][:, :], lhsT=hsT[:, hc, i * 128:(i + 1) * 128], rhs=ws2s[:, hc, cg * 512:(cg + 1) * 512],
                                start=(hc == 0), stop=(hc == 3)) for hc in range(4)], [hsB, wsB_[2]], [pbB[bank]])
                    stt(P, "dve", acc[k][:, cg * 512:(cg + 1) * 512], x1r[0][:, cg * 512:(cg + 1) * 512], ALPHA, pb[bank][:, :],
                        ALU.mult, ALU.add, [x1rB[0], pbB[bank]], [accB[k]])
                tt(P, "pool", accP[0], Yg[:, 0, :], Yg[:, 1, :], ALU.add, [YgB[0], YgB[1]], [accPB[0]])
                tt(P, "pool", accP[0], accP[0], Yg[:, 2, :], ALU.add, [YgB[2], accPB[0]], [accPB[0]])
                tt(P, "pool", accP[0], accP[0], Yg[:, 3, :], ALU.add, [YgB[3], accPB[0]], [accPB[0]])
                for kk in range(4, 8):
                    tt(P, "dve", acc[k], acc[k], Yg[:, kk, :], ALU.add, [YgB[kk], accB[k]], [accB[k]])
                tt(P, "dve", acc[k], acc[k], accP[0], ALU.add, [accPB[0], accB[k]], [accB[k]])
                src = acc[k]
                for a_ in range(4):
                    P.op("dve", lambda e, o=st4[k], s_=src, a_=a_: e.bn_stats(out=o[:, a_, :], in_=s_[:, a_ * 512:(a_ + 1) * 512]), [accB[k]], [ln2B[k]])
                P.op("dve", lambda e, o=mv2[k], s_=st4[k]: e.bn_aggr(out=o, in_=s_.rearrange("p a b -> p (a b)")), [ln2B[k]], [ln2B[k]])
                act(P, rs2[k], mv2[k][:, 1:2], AF.Sqrt, [ln2B[k]], [ln2B[k]], bias=EPS)
                P.op("dve", lambda e, o=rs2[k]: e.reciprocal(out=o, in_=o), [ln2B[k]], [ln2B[k]])
                ts(P, "dve", src, src, mv2[k][:, 0:1], rs2[k][:, 0:1], ALU.subtract, ALU.mult, [accB[k], ln2B[k]], [accB[k]])
                tt(P, "pool", src, src, lnv[:, 0, :], ALU.mult, [accB[k], lnvB], [accB[k]])
                tt(P, "pool", src, src, lnv[:, 1, :], ALU.add, [accB[k], lnvB], [accB[k]])
                dma(P, "sp", s_out[k], out_d[i * 128:(i + 1) * 128, :], src, [accB[k]], [outB])
        P.wait_all("sp", [outB, dbgB])
        P.emit()
    return nc, list(dbg.keys())


def _pack(parts):
    off = {}
    cols = []
    o = 0
    for name, arr in parts:
        arr = np.ascontiguousarray(arr, dtype=np.float32).reshape(128, -1)
        off[name] = o
        o += arr.shape[1]
        cols.append(arr)
    return off, np.concatenate(cols, axis=1)


def _const_P(b_in=None, sinks=None, rbias=None, core=0):
    p = np.arange(128)
    ident = np.eye(128, dtype=np.float32)
    psw = np.zeros((128, 128), np.float32)
    for m in range(128):
        d = m % 64
        if d < 8:
            psw[m + 8, m] = 1.0
        elif d < 16:
            psw[m - 8, m] = 1.0
    trilT = (p[None, :] >= p[:, None]).astype(np.float32)
    if b_in is None:
        b_in = np.zeros(IN_W, np.float32)
        sinks = np.zeros(32, np.float32)
        rbias = np.zeros(64, np.float32)
    bias_pm = b_in.reshape(68, 128).T
    bk = b_in[COL_K:COL_K + 256].reshape(4, 64)
    bias_k = np.concatenate([bk.T, bk.T], axis=0)
    sk = sinks.reshape(16, 2)
    sinks_pm = np.repeat(sk.T, 64, axis=0)
    rb = np.broadcast_to(rbias[None, :], (128, 64))
    iota_cap = np.broadcast_to(np.arange(CAP, dtype=np.float32)[None, :], (128, CAP))
    ecap1 = np.broadcast_to((np.arange(64, dtype=np.float32) * CAP + 1.0)[None, :], (128, 64))
    tok = (np.arange(8)[None, :] * 128 + p[:, None])
    tokhl = np.stack([tok // 32, tok % 32], axis=2).astype(np.float32).reshape(128, 16)
    return _pack([("ident", ident), ("psw", psw), ("trilT", trilT),
                  ("bias_pm", bias_pm), ("bias_k", bias_k), ("sinks_pm", sinks_pm), ("rbias", rb),
                  ("iota_cap", iota_cap), ("ecap1", ecap1), ("tokhl", tokhl)])


def _const_bf(core):
    p = np.arange(128)
    ident = np.eye(128, dtype=np.float32)
    ones = np.ones((128, 128), np.float32)
    lstrict = (p[:, None] < p[None, :]).astype(np.float32)
    kk = p[:, None]
    qq = p[None, :]
    m_prev = np.where(kk > qq, 0.0, NEG).astype(np.float32)
    m_cur = np.where(kk <= qq, 0.0, NEG).astype(np.float32)
    m_prev0 = m_prev if core % 4 != 0 else np.full((128, 128), NEG, np.float32)
    masks = np.concatenate([np.tile(m, (1, 4)) for m in (m_prev0, m_prev, m_cur)], axis=1)
    return np.ascontiguousarray(np.concatenate([ident, ones, lstrict, np.zeros((128, 128), np.float32), masks], axis=1))


CPO, _cp0 = _const_P()
CP_W = _cp0.shape[1]
CA_W = 2 * T + 2 * TH
CB_W = 3072


def _rope_tables(core):
    j = core % 4
    pos = np.arange(j * 1024 - 128, j * 1024 + 1024).astype(np.float32)
    inv = (500000.0 ** (-np.arange(0, 16, 2, dtype=np.float32) / 16.0)).astype(np.float32)
    ang = pos[:, None] * inv[None, :]
    cos = np.cos(ang).astype(np.float32)
    sin = np.sin(ang).astype(np.float32)
    C = np.ones((128, TH), np.float32)
    S = np.zeros((128, TH), np.float32)
    for p in range(128):
        d = p % 64
        if d < 8:
            C[p] = cos[:, d]
            S[p] = -sin[:, d]
        elif d < 16:
            C[p] = cos[:, d - 8]
            S[p] = sin[:, d - 8]
    cq = (C[:, 128:] * 0.125).astype(np.float32)
    sq = (S[:, 128:] * 0.125).astype(np.float32)
    return np.ascontiguousarray(np.concatenate([cq, sq, C, S], axis=1))


_CACHE = {}


def _get_prog(debug=()):
    key = tuple(debug)
    if key not in _CACHE:
        _CACHE[key] = build(debug)
    return _CACHE[key]


def kernel(x, w_in, b_in, sinks, sgu_ln_g, sgu_ln_b, w_spatial, b_spatial,
           w_branch_attn, w_branch_sgu, w_out, ln1_g, ln1_b, w_router, router_bias,
           w1, w3, w2, ws1, ws3, ws2, ln2_g, ln2_b, _debug=(), _cores=None):
    f = lambda a: np.ascontiguousarray(np.asarray(a, dtype=np.float32))
    x = f(x)
    nc, dbg_names = _get_prog(_debug)
    small = any(d_.startswith("lvl") and float(d_[3:]) < 7 for d_ in _debug)
    bc = lambda v, n: np.ascontiguousarray(np.broadcast_to(f(v).reshape(1, n), (128, n)))
    shared = {
        "w_in": f(w_in)[0], "w_a": f(w_branch_attn)[0], "w_b": f(w_branch_sgu)[0], "w_o": f(w_out)[0],
        "w_r": f(w_router)[0], "w1": f(w1)[0][:1] if small else f(w1)[0], "w3": f(w3)[0][:1] if small else f(w3)[0],
        "w2": f(w2)[0][:1] if small else f(w2)[0],
        "ws1": f(ws1)[0], "ws3": f(ws3)[0], "ws2": f(ws2)[0],
        "cB": np.ascontiguousarray(np.concatenate([bc(sgu_ln_g, 1024), bc(sgu_ln_b, 1024), bc(b_spatial, 1024)], axis=1)),
        "wsT": np.ascontiguousarray(f(w_spatial)[0].transpose(2, 0, 1)),
        "brow": np.ascontiguousarray(np.concatenate([f(b_in)[0, COL_V:COL_V + 256], f(b_in)[0, COL_VG:COL_VG + 1024]])[None, :]),
        "lnv": np.ascontiguousarray(np.stack([bc(ln1_g, D), bc(ln1_b, D), bc(ln2_g, D), bc(ln2_b, D)])),
    }
    in_maps = []
    for c in range(NCORES):
        b, j = c // 4, c % 4
        t0 = j * 1024
        xin = np.zeros((TH, D), np.float32)
        xin[128:] = x[b, t0:t0 + 1024]
        if j > 0:
            xin[:128] = x[b, t0 - 128:t0]
        _, cPv = _const_P(f(b_in)[0], f(sinks)[0], f(router_bias)[0], c)
        m = dict(shared)
        m["xin"] = xin
        m["cP"] = cPv
        m["cbf"] = _const_bf(c)
        m["cA"] = _rope_tables(c)
        in_maps.append(m)
    cores = list(range(NCORES)) if _cores is None else _cores
    res = run_bass_kernel_spmd(nc, [in_maps[c] for c in cores], core_ids=list(range(len(cores))))
    if _debug:
        print("EXEC_NS", res.exec_time_ns)
        return res.results
    out = np.zeros((2, 4096, D), np.float32)
    for c in range(NCORES):
        b, j = c // 4, c % 4
        out[b, j * 1024:(j + 1) * 1024] = res.results[c]["out"]
    return out
```

```python
import numpy as np
from contextlib import ExitStack
import concourse.bass as bass
import concourse.mybir as mybir
from concourse.bass_utils import run_bass_kernel_spmd

F32 = mybir.dt.float32
BF16 = mybir.dt.bfloat16
I32 = mybir.dt.int32
AF = mybir.ActivationFunctionType
ALU = mybir.AluOpType

NCORES = 8
D = 2048
T = 1024
TH = 1152
NT = 8
IN_W = 8704
NE = 64
CAP = 256
ALPHA = 2.0 ** 0.25
EPS = 1e-5
NEG = -30000.0
COL_K, COL_V, COL_U, COL_VG, COL_GA, COL_GB = 2048, 2304, 2560, 3584, 4608, 6656

ENGS = ("pe", "act", "dve", "pool", "sp")
SAME_ENGINE_SYNC = True


class Buf:
    __slots__ = ("name", "ws", "r", "pr")

    def __init__(self, name=""):
        self.name = name
        self.ws = []
        self.r = []
        self.pr = []


class DmaSem:
    def __init__(self, sem):
        self.sem = sem
        self.count = 0


class Prog:
    def __init__(self, nc, stack):
        self.nc = nc
        self.stack = stack
        self.ops = {e: [] for e in ENGS}
        self.seen = {e: {} for e in ENGS}
        self.esem = {e: stack.enter_context(nc.semaphore("es_" + e)) for e in ENGS}
        self.ecnt = {e: 0 for e in ENGS}
        self.dsems = []

    def dma_sem(self, name):
        d = DmaSem(self.stack.enter_context(self.nc.semaphore(name)))
        self.dsems.append(d)
        return d

    def _waits(self, eng, reads, writes):
        need = {}

        def add(ev):
            if ev is None:
                return
            sem, val, src = ev
            if src == eng and (eng == "pe" or not SAME_ENGINE_SYNC):
                return
            k = id(sem)
            if self.seen[eng].get(k, 0) >= val:
                return
            if k not in need or need[k][1] < val:
                need[k] = (sem, val)

        for b in reads:
            for ev in b.ws:
                add(ev)
        for b in writes:
            for ev in (b.r if b.r else b.pr):
                add(ev)
        for k, (sem, val) in need.items():
            self.seen[eng][k] = val
        return list(need.values())

    def _post(self, ev, reads, writes):
        for b in reads:
            b.r.append(ev)
            if len(b.r) > 64:
                best = {}
                for e2 in b.r:
                    k = id(e2[0])
                    if k not in best or best[k][1] < e2[1]:
                        best[k] = e2
                b.r = list(best.values())
        for b in writes:
            if b.r:
                b.ws = [ev]
                b.pr = b.r
                b.r = []
            else:
                b.ws.append(ev)
                if len(b.ws) > 32:
                    best = {}
                    for e2 in b.ws:
                        k = id(e2[0])
                        if k not in best or best[k][1] < e2[1]:
                            best[k] = e2
                    b.ws = list(best.values())

    def op(self, eng, emit, reads=(), writes=()):
        waits = self._waits(eng, reads, writes)
        self.ecnt[eng] += 1
        ev = (self.esem[eng], self.ecnt[eng], eng)
        self.ops[eng].append((waits, emit, (self.esem[eng], 1)))
        self._post(ev, reads, writes)
        return ev

    def dma(self, eng, dsem, emit, reads=(), writes=()):
        waits = self._waits(eng, reads, writes)
        dsem.count += 16
        ev = (dsem.sem, dsem.count, "dma")
        self.ops[eng].append((waits, emit, (dsem.sem, 16)))
        self._post(ev, reads, writes)
        return ev

    def barrier(self):
        for e in ENGS:
            waits = []
            for o in ENGS:
                if (o != e or (e != "pe" and SAME_ENGINE_SYNC)) and self.ecnt[o] > self.seen[e].get(id(self.esem[o]), 0):
                    waits.append((self.esem[o], self.ecnt[o]))
                    self.seen[e][id(self.esem[o])] = self.ecnt[o]
            for d in self.dsems:
                if d.count > self.seen[e].get(id(d.sem), 0):
                    waits.append((d.sem, d.count))
                    self.seen[e][id(d.sem)] = d.count
            self.ops[e].append((waits, None, None))

    def wait_all(self, eng, bufs):
        waits = self._waits(eng, bufs, ())
        self.ops[eng].append((waits, None, None))

    def emit(self):
        nc = self.nc
        with nc.Block() as block:
            def run(engname):
                def f(e):
                    for waits, emit, inc in self.ops[engname]:
                        for sem, val in waits:
                            e.wait_ge(sem, val)
                        if emit is not None:
                            ins = emit(e)
                            ins.then_inc(inc[0], inc[1])
                return f
            block.tensor(run("pe"))
            block.scalar(run("act"))
            block.vector(run("dve"))
            block.gpsimd(run("pool"))
            block.sync(run("sp"))


def mm(P, mms, reads, writes):
    def f(e, mms=mms):
        r = None
        for m in mms:
            r = e.matmul(**m)
        return r
    return P.op("pe", f, reads, writes)


def tr(P, trs, reads, writes):
    def f(e, trs=trs):
        r = None
        for t in trs:
            r = e.transpose(**t)
        return r
    return P.op("pe", f, reads, writes)


def act(P, out, in_, func, reads, writes, bias=None, scale=None):
    kw = {}
    if bias is not None:
        kw["bias"] = bias
    if scale is not None:
        kw["scale"] = scale
    return P.op("act", lambda e: e.activation(out=out, in_=in_, func=func, **kw), reads, writes)


def tt(P, eng, out, in0, in1, op, reads, writes):
    return P.op(eng, lambda e: e.tensor_tensor(out=out, in0=in0, in1=in1, op=op), reads, writes)


def ts(P, eng, out, in0, s1, s2, op0, op1, reads, writes):
    if op1 is None:
        return P.op(eng, lambda e: e.tensor_scalar(out=out, in0=in0, scalar1=s1, scalar2=None, op0=op0), reads, writes)
    return P.op(eng, lambda e: e.tensor_scalar(out=out, in0=in0, scalar1=s1, scalar2=s2, op0=op0, op1=op1), reads, writes)


def stt(P, eng, out, in0, scalar, in1, op0, op1, reads, writes):
    return P.op(eng, lambda e: e.scalar_tensor_tensor(out=out, in0=in0, scalar=scalar, in1=in1, op0=op0, op1=op1), reads, writes)


def cp(P, eng, out, in_, reads, writes):
    if eng == "act":
        return P.op("act", lambda e: e.copy(out=out, in_=in_), reads, writes)
    return P.op(eng, lambda e: e.tensor_copy(out=out, in_=in_), reads, writes)


def dma(P, eng, dsem, out, in_, reads, writes):
    return P.dma(eng, dsem, lambda e: e.dma_start(out=out, in_=in_), reads, writes)


def gather(P, dsem, out, src, idx_ap, reads, writes):
    return P.dma("pool", dsem, lambda e: e.indirect_dma_start(
        out=out, out_offset=None, in_=src,
        in_offset=bass.IndirectOffsetOnAxis(ap=idx_ap, axis=0)), reads, writes)


class Region:
    def __init__(self, arena, base, size):
        self.arena = arena
        self.base = base
        self.size = size
        self.off = 0

    def reset(self):
        self.off = 0

    def alloc(self, shape, dt, parts=128):
        n = 1
        for d in shape[1:]:
            n *= d
        words = n if dt in (F32, I32) else (n + 1) // 2
        assert self.off + words <= self.size, (self.off, words, self.size)
        v = self.arena[0:parts, self.base + self.off:self.base + self.off + words]
        self.off += words
        if dt != F32:
            v = v.bitcast(dt)
        if len(shape) == 3:
            v = v.rearrange("p (a b) -> p a b", a=shape[1])
        elif len(shape) == 4:
            v = v.rearrange("p (a b c) -> p a b c", a=shape[1], b=shape[2])
        return v


NW_ARENA = 52800


def build(debug=()):
    nc = bass.Bass("TRN2", target_bir_lowering=False)
    dbg = {}
    lvl = 8
    for d_ in debug:
        if d_.startswith("lvl"):
            lvl = float(d_[3:])

    def din(name, shape, dt=F32):
        return nc.dram_tensor(name, list(shape), dt, kind="ExternalInput").ap()

    xin = din("xin", [TH, D])
    w_in = din("w_in", [D, IN_W])
    w_a = din("w_a", [D, D])
    w_b = din("w_b", [1024, D])
    w_o = din("w_o", [D, D])
    w_r = din("w_r", [D, NE])
    ne_decl = NE if lvl >= 7 else 1
    w1 = din("w1", [ne_decl, D, 512])
    w3 = din("w3", [ne_decl, D, 512])
    w2 = din("w2", [ne_decl, 512, D])
    ws1 = din("ws1", [D, 512])
    ws3 = din("ws3", [D, 512])
    ws2 = din("ws2", [512, D])
    cP_d = din("cP", [128, CP_W])
    cbf_d = din("cbf", [128, 2048])
    cA_d = din("cA", [128, CA_W])
    cB_d = din("cB", [128, CB_W])
    wsT_d = din("wsT", [128, 8, 128])
    brow_d = din("brow", [1, 1280])
    lnv_d = din("lnv", [4, 128, D])
    out_d = nc.dram_tensor("out", [T, D], F32, kind="ExternalOutput").ap()

    x1bf_d = nc.dram_tensor("x1bf_scr", [T, D], BF16).ap()
    x1f_d = nc.dram_tensor("x1f_scr", [T, D], F32).ap()
    ysc_d = nc.dram_tensor("y_scr", [NE * CAP, D], F32).ap()

    def dbg_out(name, shape, dt=F32):
        if name in debug:
            dbg[name] = nc.dram_tensor("dbg_" + name, list(shape), dt, kind="ExternalOutput").ap()
            return dbg[name]
        return None

    w_in_v = w_in.rearrange("(c p) n -> p c n", p=128)
    w_a_v = w_a.rearrange("(c p) n -> p c n", p=128)
    w_b_v = w_b.rearrange("(c p) n -> p c n", p=128)
    w_o_v = w_o.rearrange("(c p) n -> p c n", p=128)
    w_r_v = w_r.rearrange("(c p) n -> p c n", p=128)
    ws1_v = ws1.rearrange("(c p) n -> p c n", p=128)
    ws3_v = ws3.rearrange("(c p) n -> p c n", p=128)
    ws2_v = ws2.rearrange("(c p) n -> p c n", p=128)

    with ExitStack() as top:
        P = Prog(nc, top)
        arena_t = top.enter_context(nc.sbuf_tensor("arena", [128, NW_ARENA], F32))
        arena = arena_t[:, :]
        oP, oX = 0, 5504
        oQ = oX + 9216
        oU = oQ + 8192
        oK = oU + 4096
        oW = oK + 8192
        oT = oW + 12288
        R_P = Region(arena, oP, oX)
        R_X = Region(arena, oX, 9216)
        R_Q = Region(arena, oQ, 8192)
        R_U = Region(arena, oU, 4096)
        R_K = Region(arena, oK, 8192)
        R_W = Region(arena, oW, 12288)
        R_T = Region(arena, oT, NW_ARENA - oT)
        R_XQU = Region(arena, oX, oK - oX)
        R_WT = Region(arena, oW, NW_ARENA - oW)

        pb = [top.enter_context(nc.psum_tensor(f"pb{i}", [128, 512], F32)) for i in range(8)]
        pbB = [Buf(f"pb{i}") for i in range(8)]
        ptb = [pb[6 + i][:, 0:256].bitcast(BF16).rearrange("p (a b) -> p a b", a=4) for i in range(2)]
        ptbB = [pbB[6], pbB[7]]

        ncs = [0]

        def cs():
            ncs[0] += 1
            return P.dma_sem(f"s_c{ncs[0]}")
        s_w = [P.dma_sem(f"s_w{i}") for i in range(6)]
        s_xp = [P.dma_sem(f"s_xp{i}") for i in range(2)]
        s_x = [P.dma_sem(f"s_x{i}") for i in range(2)]
        s_xbf = P.dma_sem("s_xbf")
        s_xf = P.dma_sem("s_xf")
        s_ysc = [P.dma_sem(f"s_ysc{i}") for i in range(2)]
        s_g = [P.dma_sem(f"s_g{i}") for i in range(2)]
        s_yg = [P.dma_sem(f"s_yg{i}") for i in range(8)]
        s_out = [P.dma_sem(f"s_out{i}") for i in range(2)]
        s_dbg = P.dma_sem("s_dbg")
        dbgB = Buf("dbg")
        outB = Buf("out")

        cP = R_P.alloc([128, CP_W], F32)
        cPB = Buf("cP")
        dma(P, "sp", cs(), cP, cP_d, [], [cPB])

        def cpv(name, n):
            return cP[:, CPO[name]:CPO[name] + n]
        ident_f = cpv("ident", 128)
        psw_f = cpv("psw", 128)
        trilT = cpv("trilT", 128)
        bias_pm = cpv("bias_pm", 68)
        bias_k = cpv("bias_k", 4)
        sinks_pm = cpv("sinks_pm", 16)
        rbias = cpv("rbias", 64)
        iota_cap = cpv("iota_cap", CAP)
        ecap1 = cpv("ecap1", 64)
        tokhl = cpv("tokhl", 16)

        cbf = R_P.alloc([128, 2048], BF16)
        cbfB = Buf("cbf")
        dma(P, "pool", cs(), cbf, cbf_d, [], [cbfB])
        ident_bf = cbf[:, 0:128]
        ones_bf = cbf[:, 128:256]
        lstrict_bf = cbf[:, 256:384]
        masks_bf = cbf[:, 512:512 + 1536].rearrange("p (m n) -> p m n", m=3)
        esink = R_P.alloc([128, 16], F32)
        esinkB = Buf("esink")
        act(P, esink, sinks_pm, AF.Exp, [cPB], [esinkB])
        Gall = R_P.alloc([128, 8, 64], F32)
        sel = R_P.alloc([128, 8, 64], F32)
        selb = R_P.alloc([128, 8, 64], BF16)
        pos = R_P.alloc([128, 8, 64], F32)
        GI5 = R_P.alloc([128, 8, 64, 5], BF16)
        addr8 = R_P.alloc([128, 8, 8], F32)
        addr8i = R_P.alloc([128, 8, 8], I32)
        idx_all = R_P.alloc([128, 64, 2], I32)
        gw_all = R_P.alloc([128, 64, 2], F32)

        xT = R_X.alloc([128, 16, TH], BF16)
        xTB = [Buf(f"xT{i}") for i in range(9)]
        qT = R_Q.alloc([128, 16, T], BF16)
        qTB = [[Buf(f"qT{g}_{i}") for i in range(8)] for g in range(4)]
        uT = R_U.alloc([128, 8, T], BF16)
        uTB = [[Buf(f"uT{gb}_{i}") for i in range(8)] for gb in range(2)]
        kT = R_K.alloc([128, 4, TH], BF16)
        kTB = [Buf(f"kT{g}") for g in range(4)]
        Vs = R_K.alloc([128, 9, 256], BF16)
        VB = [Buf(f"V{i}") for i in range(9)]
        vn = R_K.alloc([128, 8, 1024], BF16)
        vnB = [Buf(f"vn{i}") for i in range(8)]

        NSL = 3
        slab = [R_W.alloc([128, 8192], BF16) for i in range(NSL)]
        slabB = [Buf(f"slab{i}") for i in range(NSL)]
        slab_rr = [0]

        def next_slab():
            i = slab_rr[0] % NSL
            slab_rr[0] += 1
            return i

        def load_slab(src_ap, kc, ncols, pieces=2):
            i = next_slab()
            v = slab[i][:, 0:kc * ncols].rearrange("p (c n) -> p c n", c=kc)
            step = kc // pieces
            for h in range(pieces):
                dma(P, "pool", s_w[i], v[:, h * step:(h + 1) * step, :], src_ap[:, h * step:(h + 1) * step, :], [], [slabB[i]])
            return v, slabB[i]

        if lvl >= 1:
            Ru = Region(arena, oU, 4096)
            cqs = Ru.alloc([128, 2 * T], F32)
            R_T.reset()
            cks = R_T.alloc([128, 2 * TH], F32)
            cAB = Buf("cA")
            dma(P, "sp", cs(), cqs, cA_d[:, 0:2 * T], [], [cAB])
            dma(P, "sp", cs(), cks, cA_d[:, 2 * T:2 * T + 2 * TH], [], [cAB])
            cosq = cqs[:, 0:T]
            sinq = cqs[:, T:2 * T]
            cosk = cks[:, 0:TH]
            sink_ = cks[:, TH:2 * TH]
            Rk = Region(arena, oK + 2304, 8192 - 2304)
            xbf = [Rk.alloc([128, D], BF16) for i in range(2)]
            xbfB = [Buf(f"xbf{i}") for i in range(2)]
            qf = [Rk.alloc([128, 512], F32) for i in range(2)]
            qfB = [Buf(f"qf{i}") for i in range(2)]
            t1 = [Rk.alloc([128, 512], F32) for i in range(2)]
            t1B = [Buf(f"t1_{i}") for i in range(2)]
            t2 = [Rk.alloc([128, 512], F32) for i in range(2)]
            t2B = [Buf(f"t2_{i}") for i in range(2)]

            for i in range(9):
                b = i % 2
                dma(P, "pool", s_xp[b], xbf[b], xin[i * 128:(i + 1) * 128, :], [], [xbfB[b]])
                for j in range(4):
                    hb = (i * 4 + j) % 2
                    tr(P, [dict(out=ptb[hb][:, jj, :], in_=xbf[b][:, (4 * j + jj) * 128:(4 * j + jj + 1) * 128], identity=ident_bf)
                           for jj in range(4)], [xbfB[b], cbfB], [ptbB[hb]])
                    cp(P, "dve" if j % 2 == 0 else "act", xT[:, 4 * j:4 * j + 4, i * 128:(i + 1) * 128], ptb[hb], [ptbB[hb]], [xTB[i]])

            it = [0]

            def rope_chunk(bank, bankB, bias_ap, cos_ap, sin_ap, out_ap, n, wr):
                k = it[0] % 2
                it[0] += 1
                act(P, qf[k][:, 0:n], bank[:, 0:n], AF.Identity, [bankB, cPB], [qfB[k]], bias=bias_ap)
                pbk = 4 + k
                mm(P, [dict(out=pb[pbk][:, 0:n], lhsT=psw_f, rhs=qf[k][:, 0:n], start=True, stop=True)], [qfB[k], cPB], [pbB[pbk]])
                tt(P, "pool", t1[k][:, 0:n], qf[k][:, 0:n], cos_ap, ALU.mult, [qfB[k], cAB], [t1B[k]])
                tt(P, "dve", t2[k][:, 0:n], pb[pbk][:, 0:n], sin_ap, ALU.mult, [pbB[pbk], cAB], [t2B[k]])
                tt(P, "dve", out_ap, t1[k][:, 0:n], t2[k][:, 0:n], ALU.add, [t1B[k], t2B[k]], wr)

            bk = 0
            for s in range(4):
                wv, wB = load_slab(w_in_v[:, :, 512 * s:512 * s + 512], 16, 512)
                for cc in range(4):
                    c = 4 * s + cc
                    for half in range(2):
                        tok = slice(128 + 512 * half, 128 + 512 * half + 512)
                        otok = slice(512 * half, 512 * half + 512)
                        bank = bk % 4
                        bk += 1
                        mm(P, [dict(out=pb[bank][:, :], lhsT=wv[:, kc, cc * 128:(cc + 1) * 128], rhs=xT[:, kc, tok],
                                    start=(kc == 0), stop=(kc == 15)) for kc in range(16)],
                           [wB] + xTB[1 + 4 * half:5 + 4 * half], [pbB[bank]])
                        rope_chunk(pb[bank], pbB[bank], bias_pm[:, c:c + 1], cosq[:, otok], sinq[:, otok],
                                   qT[:, c, otok], 512, [qTB[c // 4][4 * half + ii] for ii in range(4)])
            wv, wB = load_slab(w_in_v[:, :, COL_K:COL_K + 256], 16, 256)
            iw = next_slab()
            wkd = slab[iw][:, :].rearrange("p (c g d) -> p c g d", c=16, g=4)
            wkdB = slabB[iw]
            wv4 = wv.rearrange("p c (g d) -> p c g d", g=4)
            cp(P, "pool", wkd[:, :, :, 0:64], wv4, [wB], [wkdB])
            cp(P, "pool", wkd[:, :, :, 64:128], wv4, [wB], [wkdB])
            for g in range(4):
                for (t0_, n) in ((0, 512), (512, 512), (1024, 128)):
                    bank = bk % 4
                    bk += 1
                    mm(P, [dict(out=pb[bank][:, 0:n], lhsT=wkd[:, kc, g, :], rhs=xT[:, kc, t0_:t0_ + n],
                                start=(kc == 0), stop=(kc == 15)) for kc in range(16)],
                       [wkdB] + xTB, [pbB[bank]])
                    rope_chunk(pb[bank], pbB[bank], bias_k[:, g:g + 1], cosk[:, t0_:t0_ + n], sink_[:, t0_:t0_ + n],
                               kT[:, g, t0_:t0_ + n], n, [kTB[g]])
            P.barrier()

        if lvl >= 2:
            R_T.reset()
            cB = R_T.alloc([128, 2048], F32)
            cBB = Buf("cB")
            dma(P, "sp", cs(), cB, cB_d[:, 0:2048], [], [cBB])
            lng = cB[:, 0:1024]
            lnb = cB[:, 1024:2048]
            brow = R_T.alloc([1, 1280], BF16, parts=1)
            browB = Buf("brow")
            dma(P, "pool", cs(), brow, brow_d, [], [browB])
            vgt = [R_T.alloc([128, 1024], F32) for i in range(2)]
            vgtB = [Buf(f"vgt{i}") for i in range(2)]
            stt_ = [R_T.alloc([128, 8, 6], F32) for i in range(2)]
            mv = [R_T.alloc([128, 8, 2], F32) for i in range(2)]
            rstd = [R_T.alloc([128, 8], F32) for i in range(2)]
            lnB = [Buf(f"ln{i}") for i in range(2)]

            wv, wB = load_slab(w_in_v[:, :, COL_V:COL_V + 256], 16, 256)
            for i in range(9 if lvl >= 1.2 else 0):
                bank = i % 4
                mms = [dict(out=pb[bank][:, 0:256], lhsT=xT[:, kc, i * 128:(i + 1) * 128], rhs=wv[:, kc, :],
                            start=(kc == 0), stop=False) for kc in range(16)]
                mms.append(dict(out=pb[bank][:, 0:256], lhsT=ones_bf[0:1, :], rhs=brow[0:1, 0:256], start=False, stop=True))
                mm(P, mms, [wB, xTB[i], cbfB, browB], [pbB[bank]])
                cp(P, "act", Vs[:, i, :], pb[bank][:, 0:256], [pbB[bank]], [VB[i]])
            bk = 0
            for s in range(2 if lvl >= 1.3 else 0):
                wv, wB = load_slab(w_in_v[:, :, COL_U + 512 * s:COL_U + 512 * s + 512], 16, 512)
                for cc in range(4):
                    c = 4 * s + cc
                    for half in range(2):
                        tok = slice(128 + 512 * half, 128 + 512 * half + 512)
                        otok = slice(512 * half, 512 * half + 512)
                        bank = bk % 4
                        bk += 1
                        mm(P, [dict(out=pb[bank][:, :], lhsT=wv[:, kc, cc * 128:(cc + 1) * 128], rhs=xT[:, kc, tok],
                                    start=(kc == 0), stop=(kc == 15)) for kc in range(16)],
                           [wB] + xTB[1 + 4 * half:5 + 4 * half], [pbB[bank]])
                        act(P, uT[:, c, otok], pb[bank][:, :], AF.Gelu_apprx_tanh, [pbB[bank], cPB],
                            [uTB[c // 4][4 * half + ii] for ii in range(4)], bias=bias_pm[:, 20 + c:21 + c])
            wvs = []
            for s in range(2):
                wvs.append(load_slab(w_in_v[:, :, COL_VG + 512 * s:COL_VG + 512 * s + 512], 16, 512))
            for i in range(8 if lvl >= 1.4 else 0):
                k = i % 2
                for s in range(2):
                    bank = 4 + s
                    wv, wB = wvs[s]
                    mms = [dict(out=pb[bank][:, :], lhsT=xT[:, kc, (i + 1) * 128:(i + 2) * 128], rhs=wv[:, kc, :],
                                start=(kc == 0), stop=False) for kc in range(16)]
                    mms.append(dict(out=pb[bank][:, :], lhsT=ones_bf[0:1, :], rhs=brow[0:1, 256 + 512 * s:256 + 512 * s + 512],
                                    start=False, stop=True))
                    mm(P, mms, [wB, xTB[i + 1], cbfB, browB], [pbB[bank]])
                    act(P, vgt[k][:, 512 * s:512 * s + 512], pb[bank][:, :], AF.Gelu_apprx_tanh, [pbB[bank]], [vgtB[k]])
                v3 = vgt[k].rearrange("p (g c) -> p g c", g=8)
                if lvl < 1.5:
                    continue
                for g_ in range(8):
                    P.op("dve", lambda e, o=stt_[k], v=v3, g_=g_: e.bn_stats(out=o[:, g_, :], in_=v[:, g_, :]), [vgtB[k]], [lnB[k]])
                    P.op("dve", lambda e, o=mv[k], s_=stt_[k], g_=g_: e.bn_aggr(out=o[:, g_, :], in_=s_[:, g_, :]), [lnB[k]], [lnB[k]])
                if lvl < 1.6:
                    continue
                act(P, rstd[k], mv[k][:, :, 1], AF.Sqrt, [lnB[k]], [lnB[k]], bias=EPS)
                P.op("dve", lambda e, o=rstd[k]: e.reciprocal(out=o, in_=o), [lnB[k]], [lnB[k]])
                if lvl < 1.7:
                    continue
                tt(P, "dve", v3, v3, mv[k][:, :, 0:1].broadcast_to([128, 8, 128]), ALU.subtract, [vgtB[k], lnB[k]], [vgtB[k]])
                tt(P, "dve", v3, v3, rstd[k].unsqueeze(2).broadcast_to([128, 8, 128]), ALU.mult, [vgtB[k], lnB[k]], [vgtB[k]])
                tt(P, "pool", vgt[k], vgt[k], lng, ALU.mult, [vgtB[k], cBB], [vgtB[k]])
                tt(P, "pool", vn[:, i, :], vgt[k], lnb, ALU.add, [vgtB[k], cBB], [vnB[i]])
            if dbg_out("vn", [128, 8, 1024], BF16) is not None:
                dma(P, "sp", s_dbg, dbg["vn"], vn, vnB, [dbgB])
            if dbg_out("qT", [128, 16, T], BF16) is not None:
                dma(P, "sp", s_dbg, dbg["qT"], qT, [b for r in qTB for b in r], [dbgB])
            if dbg_out("kT", [128, 4, TH], BF16) is not None:
                dma(P, "sp", s_dbg, dbg["kT"], kT, kTB, [dbgB])
            if dbg_out("Vs", [128, 9, 256], BF16) is not None:
                dma(P, "sp", s_dbg, dbg["Vs"], Vs, VB, [dbgB])
            if dbg_out("uT", [128, 8, T], BF16) is not None:
                dma(P, "sp", s_dbg, dbg["uT"], uT, [b for r in uTB for b in r], [dbgB])
            P.barrier()

        if lvl > 2:
            Rw = Region(arena, oW, 12288)
            PT = [Rw.alloc([128, 2, 8, 128], BF16) for i in range(2)]
            PTB = [Buf(f"PT{i}") for i in range(2)]
            dn = [Rw.alloc([128, 512], F32) for i in range(2)]
            dnB = [Buf(f"dn{i}") for i in range(2)]
            wsT_f = Rw.alloc([128, 8, 128], F32)
            wsT_b = Rw.alloc([128, 8, 128], BF16)
            wsB = Buf("wsT")
            bs_bc = Rw.alloc([128, 1024], F32)
            cBB = Buf("cB2")
            dma(P, "sp", cs(), bs_bc, cB_d[:, 2048:3072], [], [cBB])
            dma(P, "sp", cs(), wsT_f, wsT_d, [], [wsB])
            tt(P, "dve", wsT_b, wsT_f, trilT.unsqueeze(1).broadcast_to([128, 8, 128]), ALU.mult, [wsB, cPB], [wsB])
            sgt = [Rw.alloc([128, 512], F32) for i in range(2)]
            sgtB = [Buf(f"sgt{i}") for i in range(2)]
            kz = [Rw.alloc([128, 4, TH], BF16) for r_ in range(2)]
            kzB = Buf("kz")
            P.op("pool", lambda e: e.memset(kz[0][64:128, :, :], 0.0), [], [kzB])
            P.op("pool", lambda e: e.memset(kz[1][0:64, :, :], 0.0), [], [kzB])
            cp(P, "pool", kz[0][0:64, :, :], kT[0:64, :, :], kTB, [kzB])
            cp(P, "pool", kz[1][64:128, :, :], kT[64:128, :, :], kTB, [kzB])

            it = 0
            for i in range(8):
                for g in range(4 if lvl >= 2.2 else 0):
                    k = it % 2
                    it += 1
                    for h in range(2):
                        kt = i + h
                        mk = 2 if h == 1 else (0 if i == 0 else 1)
                        for hb in range(2):
                            bank = 2 * h + hb
                            mms = [dict(out=pb[bank][:, :], lhsT=ident_bf, rhs=masks_bf[:, mk, :], start=True, stop=False)]
                            for sl in range(4):
                                h8 = 4 * hb + sl
                                c = 4 * g + h8 // 2
                                r = h8 % 2
                                mms.append(dict(out=pb[bank][:, sl * 128:(sl + 1) * 128],
                                                lhsT=kz[r][:, g, kt * 128:(kt + 1) * 128],
                                                rhs=qT[:, c, i * 128:(i + 1) * 128],
                                                start=False, stop=(sl == 3)))
                            mm(P, mms, [cbfB, kzB, qTB[g][i]], [pbB[bank]])
                            act(P, PT[k][:, h, 4 * hb:4 * hb + 4, :], pb[bank][:, :].rearrange("p (a b) -> p a b", a=4),
                                AF.Exp, [pbB[bank]], [PTB[k]])
                    if lvl < 2.3:
                        continue
                    mms = []
                    for cl in range(4):
                        for r in range(2):
                            hs = 2 * cl + r
                            tp = dict(tile_position=(0, 64)) if r == 1 else {}
                            for h in range(2):
                                kt = i + h
                                mms.append(dict(out=pb[4][r * 64:(r + 1) * 64, cl * 128:(cl + 1) * 128],
                                                lhsT=Vs[:, kt, g * 64:(g + 1) * 64], rhs=PT[k][:, h, hs, :],
                                                start=(h == 0), stop=(h == 1), **tp))
                            for h in range(2):
                                mms.append(dict(out=pb[5][r * 64:(r + 1) * 64, cl * 128:(cl + 1) * 128],
                                                lhsT=ones_bf[:, 0:64], rhs=PT[k][:, h, hs, :],
                                                start=(h == 0), stop=(h == 1), **tp))
                    mm(P, mms, [PTB[k], VB[i], VB[i + 1], cbfB], [pbB[4], pbB[5]])
                    if lvl < 2.4:
                        continue
                    d3 = dn[k].rearrange("p (a b) -> p a b", a=4)
                    tt(P, "dve", d3, pb[5][:, :].rearrange("p (a b) -> p a b", a=4),
                       esink[:, 4 * g:4 * g + 4].unsqueeze(2).broadcast_to([128, 4, 128]), ALU.add, [pbB[5], esinkB], [dnB[k]])
                    P.op("dve", lambda e, o=dn[k]: e.reciprocal(out=o, in_=o), [dnB[k]], [dnB[k]])
                    tt(P, "dve", qT[:, 4 * g:4 * g + 4, i * 128:(i + 1) * 128], pb[4][:, :].rearrange("p (a b) -> p a b", a=4),
                       d3, ALU.mult, [pbB[4], dnB[k]], [qTB[g][i]])
                for gb in range(2 if (lvl >= 3 or lvl == 2.1) else 0):
                    k2 = (2 * i + gb) % 2
                    mms = []
                    for gl in range(4):
                        g8 = 4 * gb + gl
                        mms.append(dict(out=pb[6][:, gl * 128:(gl + 1) * 128], lhsT=vn[:, i, g8 * 128:(g8 + 1) * 128],
                                        rhs=wsT_b[:, g8, :], start=True, stop=True))
                    mm(P, mms, [vnB[i], wsB], [pbB[6]])
                    tt(P, "dve", sgt[k2], pb[6][:, :], bs_bc[:, 512 * gb:512 * gb + 512], ALU.add, [pbB[6], cBB], [sgtB[k2]])
                    uv = uT[:, 4 * gb:4 * gb + 4, i * 128:(i + 1) * 128]
                    tt(P, "pool", uv, sgt[k2].rearrange("p (a b) -> p a b", a=4), uv, ALU.mult, [sgtB[k2], uTB[gb][i]], [uTB[gb][i]])
            if dbg_out("attnT", [128, 16, T], BF16) is not None:
                dma(P, "sp", s_dbg, dbg["attnT"], qT, [b for r in qTB for b in r], [dbgB])
            if dbg_out("sguT", [128, 8, T], BF16) is not None:
                dma(P, "sp", s_dbg, dbg["sguT"], uT, [b for r in uTB for b in r], [dbgB])
            P.barrier()
        attnT = qT
        sguT = uT
        attnB = [b for r in qTB for b in r]
        sguB = [b for r in uTB for b in r]

        mT = Region(arena, oK, 8192).alloc([128, 16, T], BF16)
        mTB = [Buf(f"mT{i}") for i in range(2)]
        if lvl >= 4:
            R_T.reset()
            sa = [R_T.alloc([128, 512], F32) for i in range(2)]
            sg = [R_T.alloc([128, 512], F32) for i in range(2)]
            ta = [R_T.alloc([128, 512], F32) for i in range(2)]
            tb_ = [R_T.alloc([128, 512], F32) for i in range(2)]
            saB = [Buf() for _ in range(2)]
            sgB = [Buf() for _ in range(2)]
            taB = [Buf() for _ in range(2)]
            tbB = [Buf() for _ in range(2)]
            it = 0
            for op_ in range(8):
                wa, waB = load_slab(w_a_v[:, :, 256 * op_:256 * op_ + 256], 16, 256)
                wga, wgaB = load_slab(w_in_v[:, :, COL_GA + 256 * op_:COL_GA + 256 * op_ + 256], 16, 256)
                i = next_slab()
                wbv = slab[i][:, 0:2048].rearrange("p (c n) -> p c n", c=8)
                wgb = slab[i][:, 2048:2048 + 4096].rearrange("p (c n) -> p c n", c=16)
                dma(P, "pool", s_w[i], wbv, w_b_v[:, :, 256 * op_:256 * op_ + 256], [], [slabB[i]])
                dma(P, "pool", s_w[i], wgb, w_in_v[:, :, COL_GB + 256 * op_:COL_GB + 256 * op_ + 256], [], [slabB[i]])
                wbB = slabB[i]
                for cc in range(2):
                    c = 2 * op_ + cc
                    csl = slice(cc * 128, (cc + 1) * 128)
                    for half in range(2):
                        k = it % 2
                        it += 1
                        otok = slice(512 * half, 512 * half + 512)
                        xtok = slice(128 + 512 * half, 128 + 512 * half + 512)
                        mm(P, [dict(out=pb[0][:, :], lhsT=wa[:, kc, csl], rhs=attnT[:, kc, otok], start=(kc == 0), stop=(kc == 15))
                               for kc in range(16)], [waB] + attnB, [pbB[0]])
                        mm(P, [dict(out=pb[1][:, :], lhsT=wga[:, kc, csl], rhs=xT[:, kc, xtok], start=(kc == 0), stop=(kc == 15))
                               for kc in range(16)], [wgaB] + xTB, [pbB[1]])
                        mm(P, [dict(out=pb[2][:, :], lhsT=wbv[:, kc, csl], rhs=sguT[:, kc, otok], start=(kc == 0), stop=(kc == 7))
                               for kc in range(8)], [wbB] + sguB, [pbB[2]])
                        mm(P, [dict(out=pb[3][:, :], lhsT=wgb[:, kc, csl], rhs=xT[:, kc, xtok], start=(kc == 0), stop=(kc == 15))
                               for kc in range(16)], [wbB] + xTB, [pbB[3]])
                        act(P, sa[k], pb[1][:, :], AF.Sigmoid, [pbB[1], cPB], [saB[k]], bias=bias_pm[:, 36 + c:37 + c])
                        act(P, sg[k], pb[3][:, :], AF.Sigmoid, [pbB[3], cPB], [sgB[k]], bias=bias_pm[:, 52 + c:53 + c])
                        tt(P, "dve", ta[k], pb[0][:, :], sa[k], ALU.mult, [pbB[0], saB[k]], [taB[k]])
                        tt(P, "dve", tb_[k], pb[2][:, :], sg[k], ALU.mult, [pbB[2], sgB[k]], [tbB[k]])
                        tt(P, "pool", mT[:, c, otok], ta[k], tb_[k], ALU.add, [taB[k], tbB[k]], [mTB[half]])
            if dbg_out("mT", [128, 16, T], BF16) is not None:
                dma(P, "sp", s_dbg, dbg["mT"], mT, mTB, [dbgB])
            P.barrier()

        R_XQU.reset()
        r = R_XQU.alloc([128, 8, D], F32)
        rB = [Buf(f"r{i}") for i in range(8)]
        if lvl >= 5:
            Rw = Region(arena, oW, 12288)
            NS2 = 2
            slab2 = [Rw.alloc([128, 16, 512], BF16) for i in range(NS2)]
            slab2B = [Buf() for _ in range(NS2)]
            R_T.reset()
            xt = [R_T.alloc([128, 512], F32) for i in range(2)]
            xtB = [Buf() for _ in range(2)]
            xin_own = xin[128:, :]
            it = 0
            for s in range(4):
                b = s % NS2
                for h in range(2):
                    dma(P, "pool", s_w[b], slab2[b][:, 8 * h:8 * h + 8, :], w_o_v[:, 8 * h:8 * h + 8, 512 * s:512 * s + 512], [], [slab2B[b]])
                for i in range(8):
                    k = it % 2
                    it += 1
                    bank = it % 4
                    dma(P, "sp", s_x[k], xt[k], xin_own[i * 128:(i + 1) * 128, 512 * s:512 * s + 512], [], [xtB[k]])
                    mm(P, [dict(out=pb[bank][:, :], lhsT=mT[:, kc, i * 128:(i + 1) * 128], rhs=slab2[b][:, kc, :],
                                start=(kc == 0), stop=(kc == 15)) for kc in range(16)], [slab2B[b]] + mTB, [pbB[bank]])
                    stt(P, "dve", r[:, i, 512 * s:512 * s + 512], xt[k], ALPHA, pb[bank][:, :], ALU.mult, ALU.add,
                        [xtB[k], pbB[bank]], [rB[i]])
            P.barrier()

        x1T = Region(arena, oK, 8192).alloc([128, 16, T], BF16)
        x1TB = [Buf(f"x1T{i}") for i in range(8)]
        rtB = [Buf(f"rt{i}") for i in range(8)]
        posB = Buf("pos")
        igB = Buf("ig")
        x1bfB = [Buf(f"x1bf_d{i}") for i in range(8)]
        x1fB = [Buf(f"x1f_d{i}") for i in range(8)]
        if lvl > 5:
            Rw = Region(arena, oW, 12288)
            lnv = Rw.alloc([128, 2, D], F32)
            lnvB = Buf("lnv")
            dma(P, "sp", cs(), lnv[:, 0, :], lnv_d[0], [], [lnvB])
            dma(P, "sp", cs(), lnv[:, 1, :], lnv_d[1], [], [lnvB])
            wr_f = Rw.alloc([128, 16, 64], F32)
            wr_hi = Rw.alloc([128, 16, 64], BF16)
            wr_lo = Rw.alloc([128, 16, 64], BF16)
            wrB = Buf("wr")
            dma(P, "sp", cs(), wr_f, w_r_v, [], [wrB])
            cp(P, "dve", wr_hi, wr_f, [wrB], [wrB])
            tt(P, "dve", wr_lo, wr_f, wr_hi, ALU.subtract, [wrB], [wrB])
            x1b = [Rw.alloc([128, D], BF16) for i in range(1)]
            x1bB = [Buf() for _ in range(1)]
            x1lo = [Rw.alloc([128, D], BF16) for i in range(1)]
            x1loB = [Buf() for _ in range(1)]
            x1Tlo = [Rw.alloc([128, 16, 128], BF16) for i in range(2)]
            x1TloB = [Buf() for _ in range(2)]
            R_T.reset()
            st4 = [R_T.alloc([128, 4, 6], F32) for i in range(2)]
            mv2 = [R_T.alloc([128, 2], F32) for i in range(2)]
            rs2 = [R_T.alloc([128, 1], F32) for i in range(2)]
            ln2B = [Buf() for _ in range(2)]
            sc = [R_T.alloc([128, 64], F32) for i in range(2)]
            bi = [R_T.alloc([128, 64], F32) for i in range(2)]
            mx = [R_T.alloc([128, 8, 8], F32) for i in range(2)]
            gs = [R_T.alloc([128, 8], F32) for i in range(2)]
            gm = [R_T.alloc([128, 8], F32) for i in range(2)]
            m8 = [R_T.alloc([128, 8], F32) for i in range(2)]
            mk_ = [R_T.alloc([128, 64], F32) for i in range(2)]
            wsum = [R_T.alloc([128, 1], F32) for i in range(2)]
            rtsB = [Buf() for _ in range(2)]

            def layer_norm(eng2, src, srcB, g_ap, b_ap, gbB, k):
                for a_ in range(4):
                    P.op("dve", lambda e, a_=a_, o=st4[k], s_=src: e.bn_stats(out=o[:, a_, :], in_=s_[:, a_ * 512:(a_ + 1) * 512]), [srcB], [ln2B[k]])
                P.op("dve", lambda e, o=mv2[k], s_=st4[k]: e.bn_aggr(out=o, in_=s_.rearrange("p a b -> p (a b)")), [ln2B[k]], [ln2B[k]])
                act(P, rs2[k], mv2[k][:, 1:2], AF.Sqrt, [ln2B[k]], [ln2B[k]], bias=EPS)
                P.op("dve", lambda e, o=rs2[k]: e.reciprocal(out=o, in_=o), [ln2B[k]], [ln2B[k]])
                ts(P, "dve", src, src, mv2[k][:, 0:1], rs2[k][:, 0:1], ALU.subtract, ALU.mult, [srcB, ln2B[k]], [srcB])
                tt(P, eng2, src, src, g_ap, ALU.mult, [srcB, gbB], [srcB])
                tt(P, eng2, src, src, b_ap, ALU.add, [srcB, gbB], [srcB])

            for i in range(8):
                k = i % 2
                layer_norm("pool", r[:, i, :], rB[i], lnv[:, 0, :], lnv[:, 1, :], lnvB, k)
                cp(P, "act", x1b[0], r[:, i, :], [rB[i]], [x1bB[0]])
                tt(P, "dve", x1lo[0], r[:, i, :], x1b[0], ALU.subtract, [rB[i], x1bB[0]], [x1loB[0]])
                dma(P, "sp", s_xbf, x1bf_d[i * 128:(i + 1) * 128, :], x1b[0], [x1bB[0]], [x1bfB[i]])
                dma(P, "sp", s_xf, x1f_d[i * 128:(i + 1) * 128, :], r[:, i, :], [rB[i]], [x1fB[i]])
                for j in range(4):
                    hb = j % 2
                    tr(P, [dict(out=ptb[hb][:, jj, :], in_=x1b[0][:, (4 * j + jj) * 128:(4 * j + jj + 1) * 128], identity=ident_bf)
                           for jj in range(4)], [x1bB[0], cbfB], [ptbB[hb]])
                    cp(P, "dve" if j % 2 == 0 else "act", x1T[:, 4 * j:4 * j + 4, i * 128:(i + 1) * 128], ptb[hb], [ptbB[hb]], [x1TB[i]])
                for j in range(4):
                    hb = j % 2
                    tr(P, [dict(out=ptb[hb][:, jj, :], in_=x1lo[0][:, (4 * j + jj) * 128:(4 * j + jj + 1) * 128], identity=ident_bf)
                           for jj in range(4)], [x1loB[0], cbfB], [ptbB[hb]])
                    cp(P, "act" if j % 2 == 0 else "dve", x1Tlo[k][:, 4 * j:4 * j + 4, :], ptb[hb], [ptbB[hb]], [x1TloB[k]])
                mms = []
                for kc in range(16):
                    xh = x1T[:, kc, i * 128:(i + 1) * 128]
                    mms.append(dict(out=pb[5][:, 0:64], lhsT=xh, rhs=wr_hi[:, kc, :], start=(kc == 0), stop=False))
                    mms.append(dict(out=pb[5][:, 0:64], lhsT=xh, rhs=wr_lo[:, kc, :], start=False, stop=False))
                    mms.append(dict(out=pb[5][:, 0:64], lhsT=x1Tlo[k][:, kc, :], rhs=wr_hi[:, kc, :], start=False, stop=(kc == 15)))
                mm(P, mms, [x1TB[i], x1TloB[k], wrB], [pbB[5]])
                if lvl < 5.2:
                    continue
                RB = rtsB[k]
                act(P, sc[k], pb[5][:, 0:64], AF.Sigmoid, [pbB[5]], [RB])
                tt(P, "dve", bi[k], sc[k], rbias, ALU.add, [RB, cPB], [RB])
                for g in range(8):
                    P.op("dve", lambda e, o=mx[k][:, g, :], v=bi[k][:, g * 8:(g + 1) * 8]: e.max(out=o, in_=v), [RB], [RB])
                tt(P, "dve", gs[k], mx[k][:, :, 0], mx[k][:, :, 1], ALU.add, [RB], [RB])
                P.op("dve", lambda e, o=m8[k], v=gs[k]: e.max(out=o, in_=v), [RB], [RB])
                ts(P, "dve", gm[k], gs[k], m8[k][:, 3:4], None, ALU.is_ge, None, [RB], [RB])
                stt(P, "dve", mk_[k].rearrange("p (g e) -> p g e", g=8), bi[k].rearrange("p (g e) -> p g e", g=8), 2.0,
                    gm[k].unsqueeze(2).broadcast_to([128, 8, 8]), ALU.add, ALU.mult, [RB], [RB])
                P.op("dve", lambda e, o=m8[k], v=mk_[k]: e.max(out=o, in_=v), [RB], [RB])
                ts(P, "dve", sel[:, i, :], mk_[k], m8[k][:, 7:8], None, ALU.is_ge, None, [RB], [rtB[i]])
                tt(P, "dve", sc[k], sc[k], sel[:, i, :], ALU.mult, [RB, rtB[i]], [RB])
                P.op("dve", lambda e, o=wsum[k], v=sc[k]: e.reduce_sum(out=o, in_=v, axis=mybir.AxisListType.X), [RB], [RB])
                ts(P, "dve", wsum[k], wsum[k], 1e-20, 1.0 / 2.5, ALU.add, ALU.mult, [RB], [RB])
                P.op("dve", lambda e, o=wsum[k]: e.reciprocal(out=o, in_=o), [RB], [RB])
                ts(P, "dve", Gall[:, i, :], sc[k], wsum[k][:, 0:1], None, ALU.mult, None, [RB], [rtB[i]])
                cp(P, "dve", selb[:, i, :], sel[:, i, :], [rtB[i]], [rtB[i]])
            for i in range(8 if lvl >= 5.3 else 0):
                mms = [dict(out=pb[4][:, i * 64:(i + 1) * 64], lhsT=lstrict_bf, rhs=selb[:, i, :], start=True, stop=(i == 0))]
                for j in range(i):
                    mms.append(dict(out=pb[4][:, i * 64:(i + 1) * 64], lhsT=ones_bf, rhs=selb[:, j, :], start=False, stop=(j == i - 1)))
                mm(P, mms, rtB[:i + 1] + [cbfB], [pbB[4]])
            cp(P, "dve", pos.rearrange("p a b -> p (a b)"), pb[4][:, :], [pbB[4]], [posB])
            for i in range(8 if lvl >= 5.3 else 0):
                k = i % 2
                tt(P, "dve", mk_[k], pos[:, i, :], ecap1, ALU.add, [posB, cPB], [rtsB[k]])
                tt(P, "dve", mk_[k], mk_[k], sel[:, i, :], ALU.mult, [rtsB[k], rtB[i]], [rtsB[k]])
                P.op("dve", lambda e, o=addr8[:, i, :], v=mk_[k]: e.max(out=o, in_=v), [rtsB[k]], [posB])
            ts(P, "dve", addr8, addr8, -1.0, None, ALU.add, None, [posB], [posB])
            cp(P, "dve", addr8i, addr8, [posB], [posB])
            P.barrier()
            Rw = Region(arena, oW, 12288)
            gtmp = Rw.alloc([128, 8, 64], F32)
            gB = Buf("gtmp")
            thl = tokhl.rearrange("p (a b) -> p a b", a=8)
            cp(P, "pool", GI5[:, :, :, 0], thl[:, :, 0:1].broadcast_to([128, 8, 64]), [cPB], [igB])
            cp(P, "pool", GI5[:, :, :, 1], thl[:, :, 1:2].broadcast_to([128, 8, 64]), [cPB], [igB])
            cp(P, "dve", GI5[:, :, :, 2], Gall, rtB, [igB])
            tt(P, "dve", gtmp, Gall, GI5[:, :, :, 2], ALU.subtract, rtB + [igB], [gB])
            cp(P, "dve", GI5[:, :, :, 3], gtmp, [gB], [igB])
            tt(P, "dve", gtmp, gtmp, GI5[:, :, :, 3], ALU.subtract, [gB, igB], [gB])
            cp(P, "dve", GI5[:, :, :, 4], gtmp, [gB], [igB])
            S1 = [Rw.alloc([128, 8, CAP], BF16) for i in range(4)]
            S1B = [Buf() for _ in range(4)]
            posm = Rw.alloc([128, 8, 64], F32)
            posmB = Buf("posm")
            stt(P, "dve", posm, pos, 1.0, sel, ALU.add, ALU.mult, [posB] + rtB, [posmB])
            ts(P, "dve", posm, posm, -1.0, None, ALU.add, None, [posmB], [posmB])
            for e_ in range(NE if lvl >= 5.4 else 0):
                k = e_ % 4
                bank = 2 + e_ // 32
                tt(P, "dve", S1[k], iota_cap.unsqueeze(1).broadcast_to([128, 8, CAP]),
                   posm[:, :, e_:e_ + 1].broadcast_to([128, 8, CAP]), ALU.is_equal, [posmB, cPB], [S1B[k]])
                c0 = ((e_ % 32) * 2) * 5
                mm(P, [dict(out=pb[bank][:, c0 + st * 5:c0 + st * 5 + 5], lhsT=S1[k][:, i, st * 128:(st + 1) * 128],
                            rhs=GI5[:, i, e_, :], start=(i == 0), stop=(i == 7)) for st in range(2) for i in range(8)],
                   [S1B[k], igB], [pbB[bank]])
            igs = Rw.alloc([128, 2, 320], F32)
            igsB = Buf("igs")
            idxf = Rw.alloc([128, 64, 2], F32)
            cp(P, "dve", igs[:, 0, :], pb[2][:, 0:320], [pbB[2]], [igsB])
            cp(P, "act", igs[:, 1, :], pb[3][:, 0:320], [pbB[3]], [igsB])
            for hb_ in range(2):
                v5 = igs[:, hb_, :].rearrange("p (e s c) -> p e s c", e=32, s=2)
                es = slice(32 * hb_, 32 * hb_ + 32)
                stt(P, "dve", idxf[:, es, :], v5[:, :, :, 0], 32.0, v5[:, :, :, 1], ALU.mult, ALU.add, [igsB], [igB])
                tt(P, "dve", gw_all[:, es, :], v5[:, :, :, 2], v5[:, :, :, 3], ALU.add, [igsB], [igB])
                tt(P, "dve", gw_all[:, es, :], gw_all[:, es, :], v5[:, :, :, 4], ALU.add, [igsB, igB], [igB])
            cp(P, "dve", idx_all, idxf, [igB], [igB])
            if dbg_out("x1", [128, 8, D]) is not None:
                dma(P, "sp", s_dbg, dbg["x1"], r, rB, [dbgB])
            if dbg_out("Gall", [128, 8, 64]) is not None:
                dma(P, "sp", s_dbg, dbg["Gall"], Gall, rtB, [dbgB])
            if dbg_out("pos", [128, 8, 64]) is not None:
                dma(P, "sp", s_dbg, dbg["pos"], pos, [posB], [dbgB])
            if dbg_out("idx_all", [128, 64, 2], I32) is not None:
                dma(P, "sp", s_dbg, dbg["idx_all"], idx_all, [igB], [dbgB])
            if dbg_out("gw_all", [128, 64, 2]) is not None:
                dma(P, "sp", s_dbg, dbg["gw_all"], gw_all, [igB], [dbgB])
            if dbg_out("addr8i", [128, 8, 8], I32) is not None:
                dma(P, "sp", s_dbg, dbg["addr8i"], addr8i, [posB], [dbgB])
            P.barrier()

        yscB = Buf("ysc")
        if lvl >= 7:
            R1 = Region(arena, oX, oK - oX)
            R2 = Region(arena, oW, NW_ARENA - oW)
            NW = 2
            w1s = [R1.alloc([128, 16, 512], BF16) for i in range(NW)]
            w3s = [R1.alloc([128, 16, 512], BF16) for i in range(NW)]
            w2s = [R1.alloc([128, 4, D], BF16), R2.alloc([128, 4, D], BF16)]
            hT = [R1.alloc([128, 4, CAP], BF16) for i in range(2)]
            w1B = [Buf() for _ in range(NW)]
            w3B = [Buf() for _ in range(NW)]
            w2B = [Buf() for _ in range(NW)]
            Xg = [R2.alloc([128, 2, D], BF16) for i in range(2)]
            XgB = [Buf() for _ in range(2)]
            XeT = [R2.alloc([128, 16, CAP], BF16) for i in range(2)]
            XeTB = [Buf() for _ in range(2)]
            hTB = [Buf() for _ in range(2)]
            sl_ = [R2.alloc([128, 2, CAP], F32) for i in range(2)]
            slB = [Buf() for _ in range(2)]
            NY = 2
            ys = [R2.alloc([128, D], F32) for i in range(NY)]
            ysB = [Buf() for _ in range(NY)]

            def load_expert(e_):
                b = e_ % NW
                for h in range(2):
                    dma(P, "pool", s_w[b], w1s[b][:, 8 * h:8 * h + 8, :],
                        w1[e_].rearrange("(c p) n -> p c n", p=128)[:, 8 * h:8 * h + 8, :], [], [w1B[b]])
                for h in range(2):
                    dma(P, "pool", s_w[2 + b], w3s[b][:, 8 * h:8 * h + 8, :],
                        w3[e_].rearrange("(c p) n -> p c n", p=128)[:, 8 * h:8 * h + 8, :], [], [w3B[b]])
                for h in range(2):
                    dma(P, "pool", s_w[4 + b], w2s[b][:, 2 * h:2 * h + 2, :],
                        w2[e_].rearrange("(c p) n -> p c n", p=128)[:, 2 * h:2 * h + 2, :], [], [w2B[b]])

            def load_tokens(e_):
                b = e_ % 2
                for st in range(2):
                    gather(P, s_g[b], Xg[b][:, st, :], x1bf_d[:, :], idx_all[:, e_, st:st + 1], [igB] + x1bfB, [XgB[b]])

            load_tokens(0)
            load_expert(0)
            yi = 0
            for e_ in range(NE):
                b = e_ % 2
                wb_ = e_ % NW
                if e_ + 1 < NE:
                    load_tokens(e_ + 1)
                    load_expert(e_ + 1)
                for st in range(2):
                    for j in range(4):
                        hb = j % 2
                        tr(P, [dict(out=ptb[hb][:, jj, :], in_=Xg[b][:, st, (4 * j + jj) * 128:(4 * j + jj + 1) * 128], identity=ident_bf)
                               for jj in range(4)], [XgB[b], cbfB], [ptbB[hb]])
                        cp(P, "dve" if j % 2 == 0 else "act", XeT[b][:, 4 * j:4 * j + 4, st * 128:(st + 1) * 128], ptb[hb],
                           [ptbB[hb]], [XeTB[b]])
                for hp in range(2):
                    for hl in range(2):
                        hc = 2 * hp + hl
                        mm(P, [dict(out=pb[0 + hp][:, hl * CAP:(hl + 1) * CAP], lhsT=w1s[wb_][:, kc, hc * 128:(hc + 1) * 128],
                                    rhs=XeT[b][:, kc, :], start=(kc == 0), stop=(kc == 15)) for kc in range(16)],
                           [w1B[wb_], XeTB[b]], [pbB[0 + hp]])
                        mm(P, [dict(out=pb[2 + hp][:, hl * CAP:(hl + 1) * CAP], lhsT=w3s[wb_][:, kc, hc * 128:(hc + 1) * 128],
                                    rhs=XeT[b][:, kc, :], start=(kc == 0), stop=(kc == 15)) for kc in range(16)],
                           [w3B[wb_], XeTB[b]], [pbB[2 + hp]])
                    act(P, sl_[hp].rearrange("p a b -> p (a b)"), pb[0 + hp][:, :], AF.Silu, [pbB[0 + hp]], [slB[hp]])
                    tt(P, "dve", hT[b][:, 2 * hp:2 * hp + 2, :].rearrange("p a b -> p (a b)"), sl_[hp].rearrange("p a b -> p (a b)"),
                       pb[2 + hp][:, :], ALU.mult, [slB[hp], pbB[2 + hp]], [hTB[b]])
                for st in range(2):
                    yk = yi % NY
                    yi += 1
                    for cg in range(4):
                        bank = 4 + (cg % 2)
                        mm(P, [dict(out=pb[bank][:, :], lhsT=hT[b][:, hc, st * 128:(st + 1) * 128],
                                    rhs=w2s[wb_][:, hc, cg * 512:(cg + 1) * 512], start=(hc == 0), stop=(hc == 3)) for hc in range(4)],
                           [hTB[b], w2B[wb_]], [pbB[bank]])
                        if cg % 2 == 0:
                            act(P, ys[yk][:, cg * 512:(cg + 1) * 512], pb[bank][:, :], AF.Copy, [pbB[bank], igB], [ysB[yk]],
                                scale=gw_all[:, e_, st:st + 1])
                        else:
                            ts(P, "dve", ys[yk][:, cg * 512:(cg + 1) * 512], pb[bank][:, :], gw_all[:, e_, st:st + 1], None,
                               ALU.mult, None, [pbB[bank], igB], [ysB[yk]])
                    dma(P, "sp", s_ysc[yk], ysc_d[e_ * CAP + st * 128:e_ * CAP + (st + 1) * 128, :], ys[yk], [ysB[yk]], [yscB])
            P.barrier()

        if lvl >= 7:
            R1 = Region(arena, oX, oK - oX)
            R2 = Region(arena, oW, NW_ARENA - oW)
            ws1s = R1.alloc([128, 16, 512], BF16)
            ws3s = R1.alloc([128, 16, 512], BF16)
            wsB_ = [Buf() for _ in range(3)]
            for h in range(2):
                dma(P, "pool", s_w[0], ws1s[:, 8 * h:8 * h + 8, :], ws1_v[:, 8 * h:8 * h + 8, :], [], [wsB_[0]])
                dma(P, "pool", s_w[1], ws3s[:, 8 * h:8 * h + 8, :], ws3_v[:, 8 * h:8 * h + 8, :], [], [wsB_[1]])
            hsT = R2.alloc([128, 4, T], BF16)
            hsB = Buf("hsT")
            sl2 = [R2.alloc([128, 512], F32) for i in range(2)]
            sl2B = [Buf() for _ in range(2)]
            it = 0
            for half in range(2):
                tok = slice(512 * half, 512 * half + 512)
                for hc in range(4):
                    k = it % 2
                    it += 1
                    mm(P, [dict(out=pb[0 + k][:, :], lhsT=ws1s[:, kc, hc * 128:(hc + 1) * 128], rhs=x1T[:, kc, tok],
                                start=(kc == 0), stop=(kc == 15)) for kc in range(16)], [wsB_[0]] + x1TB, [pbB[0 + k]])
                    mm(P, [dict(out=pb[2 + k][:, :], lhsT=ws3s[:, kc, hc * 128:(hc + 1) * 128], rhs=x1T[:, kc, tok],
                                start=(kc == 0), stop=(kc == 15)) for kc in range(16)], [wsB_[1]] + x1TB, [pbB[2 + k]])
                    act(P, sl2[k], pb[0 + k][:, :], AF.Silu, [pbB[0 + k]], [sl2B[k]])
                    tt(P, "dve", hsT[:, hc, tok], sl2[k], pb[2 + k][:, :], ALU.mult, [sl2B[k], pbB[2 + k]], [hsB])
            P.barrier()

            R1.reset()
            Yg = R1.alloc([128, 8, D], F32)
            YgB = [Buf() for _ in range(8)]
            ws2s = R1.alloc([128, 4, D], BF16)
            for h in range(2):
                dma(P, "pool", s_w[2], ws2s[:, 2 * h:2 * h + 2, :], ws2_v[:, 2 * h:2 * h + 2, :], [], [wsB_[2]])
            lnv = R2.alloc([128, 2, D], F32)
            lnvB = Buf("lnv2")
            dma(P, "sp", cs(), lnv[:, 0, :], lnv_d[2], [], [lnvB])
            dma(P, "sp", cs(), lnv[:, 1, :], lnv_d[3], [], [lnvB])
            x1r = [R2.alloc([128, D], F32) for i in range(1)]
            x1rB = [Buf() for _ in range(1)]
            acc = [R2.alloc([128, D], F32) for i in range(2)]
            accB = [Buf() for _ in range(2)]
            accP = [R2.alloc([128, D], F32) for i in range(1)]
            accPB = [Buf() for _ in range(1)]
            st4 = [R2.alloc([128, 4, 6], F32) for i in range(2)]
            mv2 = [R2.alloc([128, 2], F32) for i in range(2)]
            rs2 = [R2.alloc([128, 1], F32) for i in range(2)]
            ln2B = [Buf() for _ in range(2)]

            for i in range(8):
                k = i % 2
                for kk in range(8):
                    gather(P, s_yg[kk], Yg[:, kk, :], ysc_d[:, :], addr8i[:, i, kk:kk + 1], [posB, yscB], [YgB[kk]])
                dma(P, "sp", s_x[0], x1r[0], x1f_d[i * 128:(i + 1) * 128, :], x1fB, [x1rB[0]])
                for cg in range(4):
                    bank = 4 + (cg % 2)
                    mm(P, [dict(out=pb[bank][:, :], lhsT=hsT[:, hc, i * 128:(i + 1) * 128], rhs=ws2s[:, hc, cg * 512:(cg + 1) * 512],
                                start=(hc == 0), stop=(hc == 3)) for hc in range(4)], [hsB, wsB_[2]], [pbB[bank]])
                    stt(P, "dve", acc[k][:, cg * 512:(cg + 1) * 512], x1r[0][:, cg * 512:(cg + 1) * 512], ALPHA, pb[bank][:, :],
                        ALU.mult, ALU.add, [x1rB[0], pbB[bank]], [accB[k]])
                tt(P, "pool", accP[0], Yg[:, 0, :], Yg[:, 1, :], ALU.add, [YgB[0], YgB[1]], [accPB[0]])
                tt(P, "pool", accP[0], accP[0], Yg[:, 2, :], ALU.add, [YgB[2], accPB[0]], [accPB[0]])
                tt(P, "pool", accP[0], accP[0], Yg[:, 3, :], ALU.add, [YgB[3], accPB[0]], [accPB[0]])
                for kk in range(4, 8):
                    tt(P, "dve", acc[k], acc[k], Yg[:, kk, :], ALU.add, [YgB[kk], accB[k]], [accB[k]])
                tt(P, "dve", acc[k], acc[k], accP[0], ALU.add, [accPB[0], accB[k]], [accB[k]])
                src = acc[k]
                for a_ in range(4):
                    P.op("dve", lambda e, o=st4[k], s_=src, a_=a_: e.bn_stats(out=o[:, a_, :], in_=s_[:, a_ * 512:(a_ + 1) * 512]), [accB[k]], [ln2B[k]])
                P.op("dve", lambda e, o=mv2[k], s_=st4[k]: e.bn_aggr(out=o, in_=s_.rearrange("p a b -> p (a b)")), [ln2B[k]], [ln2B[k]])
                act(P, rs2[k], mv2[k][:, 1:2], AF.Sqrt, [ln2B[k]], [ln2B[k]], bias=EPS)
                P.op("dve", lambda e, o=rs2[k]: e.reciprocal(out=o, in_=o), [ln2B[k]], [ln2B[k]])
                ts(P, "dve", src, src, mv2[k][:, 0:1], rs2[k][:, 0:1], ALU.subtract, ALU.mult, [accB[k], ln2B[k]], [accB[k]])
                tt(P, "pool", src, src, lnv[:, 0, :], ALU.mult, [accB[k], lnvB], [accB[k]])
                tt(P, "pool", src, src, lnv[:, 1, :], ALU.add, [accB[k], lnvB], [accB[k]])
                dma(P, "sp", s_out[k], out_d[i * 128:(i + 1) * 128, :], src, [accB[k]], [outB])
        P.wait_all("sp", [outB, dbgB])
        P.emit()
    return nc, list(dbg.keys())


def _pack(parts):
    off = {}
    cols = []
    o = 0
    for name, arr in parts:
        arr = np.ascontiguousarray(arr, dtype=np.float32).reshape(128, -1)
        off[name] = o
        o += arr.shape[1]
        cols.append(arr)
    return off, np.concatenate(cols, axis=1)


def _const_P(b_in=None, sinks=None, rbias=None, core=0):
    p = np.arange(128)
    ident = np.eye(128, dtype=np.float32)
    psw = np.zeros((128, 128), np.float32)
    for m in range(128):
        d = m % 64
        if d < 8:
            psw[m + 8, m] = 1.0
        elif d < 16:
            psw[m - 8, m] = 1.0
    trilT = (p[None, :] >= p[:, None]).astype(np.float32)
    if b_in is None:
        b_in = np.zeros(IN_W, np.float32)
        sinks = np.zeros(32, np.float32)
        rbias = np.zeros(64, np.float32)
    bias_pm = b_in.reshape(68, 128).T
    bk = b_in[COL_K:COL_K + 256].reshape(4, 64)
    bias_k = np.concatenate([bk.T, bk.T], axis=0)
    sk = sinks.reshape(16, 2)
    sinks_pm = np.repeat(sk.T, 64, axis=0)
    rb = np.broadcast_to(rbias[None, :], (128, 64))
    iota_cap = np.broadcast_to(np.arange(CAP, dtype=np.float32)[None, :], (128, CAP))
    ecap1 = np.broadcast_to((np.arange(64, dtype=np.float32) * CAP + 1.0)[None, :], (128, 64))
    tok = (np.arange(8)[None, :] * 128 + p[:, None])
    tokhl = np.stack([tok // 32, tok % 32], axis=2).astype(np.float32).reshape(128, 16)
    return _pack([("ident", ident), ("psw", psw), ("trilT", trilT),
                  ("bias_pm", bias_pm), ("bias_k", bias_k), ("sinks_pm", sinks_pm), ("rbias", rb),
                  ("iota_cap", iota_cap), ("ecap1", ecap1), ("tokhl", tokhl)])


def _const_bf(core):
    p = np.arange(128)
    ident = np.eye(128, dtype=np.float32)
    ones = np.ones((128, 128), np.float32)
    lstrict = (p[:, None] < p[None, :]).astype(np.float32)
    kk = p[:, None]
    qq = p[None, :]
    m_prev = np.where(kk > qq, 0.0, NEG).astype(np.float32)
    m_cur = np.where(kk <= qq, 0.0, NEG).astype(np.float32)
    m_prev0 = m_prev if core % 4 != 0 else np.full((128, 128), NEG, np.float32)
    masks = np.concatenate([np.tile(m, (1, 4)) for m in (m_prev0, m_prev, m_cur)], axis=1)
    return np.ascontiguousarray(np.concatenate([ident, ones, lstrict, np.zeros((128, 128), np.float32), masks], axis=1))


CPO, _cp0 = _const_P()
CP_W = _cp0.shape[1]
CA_W = 2 * T + 2 * TH
CB_W = 3072


def _rope_tables(core):
    j = core % 4
    pos = np.arange(j * 1024 - 128, j * 1024 + 1024).astype(np.float32)
    inv = (500000.0 ** (-np.arange(0, 16, 2, dtype=np.float32) / 16.0)).astype(np.float32)
    ang = pos[:, None] * inv[None, :]
    cos = np.cos(ang).astype(np.float32)
    sin = np.sin(ang).astype(np.float32)
    C = np.ones((128, TH), np.float32)
    S = np.zeros((128, TH), np.float32)
    for p in range(128):
        d = p % 64
        if d < 8:
            C[p] = cos[:, d]
            S[p] = -sin[:, d]
        elif d < 16:
            C[p] = cos[:, d - 8]
            S[p] = sin[:, d - 8]
    cq = (C[:, 128:] * 0.125).astype(np.float32)
    sq = (S[:, 128:] * 0.125).astype(np.float32)
    return np.ascontiguousarray(np.concatenate([cq, sq, C, S], axis=1))


_CACHE = {}


def _get_prog(debug=()):
    key = tuple(debug)
    if key not in _CACHE:
        _CACHE[key] = build(debug)
    return _CACHE[key]


def kernel(x, w_in, b_in, sinks, sgu_ln_g, sgu_ln_b, w_spatial, b_spatial,
           w_branch_attn, w_branch_sgu, w_out, ln1_g, ln1_b, w_router, router_bias,
           w1, w3, w2, ws1, ws3, ws2, ln2_g, ln2_b, _debug=(), _cores=None):
    f = lambda a: np.ascontiguousarray(np.asarray(a, dtype=np.float32))
    x = f(x)
    nc, dbg_names = _get_prog(_debug)
    small = any(d_.startswith("lvl") and float(d_[3:]) < 7 for d_ in _debug)
    bc = lambda v, n: np.ascontiguousarray(np.broadcast_to(f(v).reshape(1, n), (128, n)))
    shared = {
        "w_in": f(w_in)[0], "w_a": f(w_branch_attn)[0], "w_b": f(w_branch_sgu)[0], "w_o": f(w_out)[0],
        "w_r": f(w_router)[0], "w1": f(w1)[0][:1] if small else f(w1)[0], "w3": f(w3)[0][:1] if small else f(w3)[0],
        "w2": f(w2)[0][:1] if small else f(w2)[0],
        "ws1": f(ws1)[0], "ws3": f(ws3)[0], "ws2": f(ws2)[0],
        "cB": np.ascontiguousarray(np.concatenate([bc(sgu_ln_g, 1024), bc(sgu_ln_b, 1024), bc(b_spatial, 1024)], axis=1)),
        "wsT": np.ascontiguousarray(f(w_spatial)[0].transpose(2, 0, 1)),
        "brow": np.ascontiguousarray(np.concatenate([f(b_in)[0, COL_V:COL_V + 256], f(b_in)[0, COL_VG:COL_VG + 1024]])[None, :]),
        "lnv": np.ascontiguousarray(np.stack([bc(ln1_g, D), bc(ln1_b, D), bc(ln2_g, D), bc(ln2_b, D)])),
    }
    in_maps = []
    for c in range(NCORES):
        b, j = c // 4, c % 4
        t0 = j * 1024
        xin = np.zeros((TH, D), np.float32)
        xin[128:] = x[b, t0:t0 + 1024]
        if j > 0:
            xin[:128] = x[b, t0 - 128:t0]
        _, cPv = _const_P(f(b_in)[0], f(sinks)[0], f(router_bias)[0], c)
        m = dict(shared)
        m["xin"] = xin
        m["cP"] = cPv
        m["cbf"] = _const_bf(c)
        m["cA"] = _rope_tables(c)
        in_maps.append(m)
    cores = list(range(NCORES)) if _cores is None else _cores
    res = run_bass_kernel_spmd(nc, [in_maps[c] for c in cores], core_ids=list(range(len(cores))))
    if _debug:
        print("EXEC_NS", res.exec_time_ns)
        return res.results
    out = np.zeros((2, 4096, D), np.float32)
    for c in range(NCORES):
        b, j = c // 4, c % 4
        out[b, j * 1024:(j + 1) * 1024] = res.results[c]["out"]
    return out
```
